# Optimizing a Trainium2 kernel written in Bass

```python
import jax
import jax.numpy as jnp
from jax import lax
import numpy as np

D_MODEL = 1024
BATCH = 16
SEQ = 4096
DEPTH = 1
DEC_BATCH = 128
DEC_SEQ = 1
PAST_LEN = 8192
PAGE_SIZE = 128

MIX_WIDTH = D_MODEL
NSA_HEADS = 8
NSA_HEAD_DIM = 64
NSA_KV_GROUPS = 2
NSA_REP = NSA_HEADS // NSA_KV_GROUPS
NSA_WIDTH = NSA_HEADS * NSA_HEAD_DIM
CMP_BLOCK = 32
CMP_STRIDE = 16
CMP_RATIO = CMP_BLOCK // CMP_STRIDE
SLC_BLOCK = 64
SLC_TOPN = 16
WINDOW = 512
Q_BLOCK = 64
HGRN_HEADS = 4
HGRN_DK = 128
HGRN_DV = 128
HGRN_WIDTH = HGRN_HEADS * HGRN_DV
HGRN_CHUNK = 16
N_GROUPS = 4
EXPERTS_PER_GROUP = 4
N_EXPERTS = N_GROUPS * EXPERTS_PER_GROUP
EXPERT_TOPK = 2
D_EXPERT = 256
NORM_EPS = 1e-6
KV_COLS = NSA_KV_GROUPS * 2 * NSA_HEAD_DIM
NSA_GATE_COLS = 3 * NSA_HEADS
HGRN_QK_COLS = HGRN_HEADS * HGRN_DK
HGRN_V_COLS = HGRN_HEADS * HGRN_DV
IN_COLS = NSA_WIDTH + 3 * KV_COLS + NSA_GATE_COLS + 2 * HGRN_QK_COLS + 2 * HGRN_V_COLS

kernel_name = 'hymba_nsa_hgrn2_hmoe_step'


def rms_norm(x, g):
    xf = x.astype(jnp.float32)
    y = xf * lax.rsqrt(jnp.mean(xf * xf, axis=-1, keepdims=True) + NORM_EPS)
    return (y * g.astype(jnp.float32)).astype(x.dtype)


def alibi_slopes():
    h = jnp.arange(1, NSA_HEADS + 1, dtype=jnp.float32)
    return jnp.exp2(-8.0 * h / NSA_HEADS).reshape(NSA_KV_GROUPS, NSA_REP)


def masked_softmax(logits, mask):
    logits = jnp.where(mask, logits, -jnp.inf)
    m = jnp.max(logits, axis=-1, keepdims=True)
    m = jnp.where(jnp.isfinite(m), m, 0.0)
    e = jnp.exp(logits - m)
    return e / jnp.maximum(jnp.sum(e, axis=-1, keepdims=True), 1e-30)


def adaln(c, w_ada, b_ada):
    a = jax.nn.silu(c) @ w_ada + b_ada
    return jnp.split(a[:, None, :], 6, axis=-1)


def split_projection(proj):
    b, t = proj.shape[0], proj.shape[1]
    sizes = [NSA_WIDTH, KV_COLS, KV_COLS, KV_COLS, NSA_GATE_COLS, HGRN_QK_COLS, HGRN_QK_COLS, HGRN_V_COLS, HGRN_V_COLS]
    parts = jnp.split(proj, np.cumsum(sizes)[:-1].tolist(), axis=-1)
    q = parts[0].reshape(b, t, NSA_KV_GROUPS, NSA_REP, NSA_HEAD_DIM) * (NSA_HEAD_DIM ** -0.5)
    kv_c = parts[1].reshape(b, t, NSA_KV_GROUPS, 2, NSA_HEAD_DIM)
    kv_s = parts[2].reshape(b, t, NSA_KV_GROUPS, 2, NSA_HEAD_DIM)
    kv_w = parts[3].reshape(b, t, NSA_KV_GROUPS, 2, NSA_HEAD_DIM)
    gates = parts[4].reshape(b, t, NSA_HEADS, 3)
    return (q, kv_c, kv_s, kv_w, gates, parts[5], parts[6], parts[7], parts[8])


def compress_kv(kv, w1, b1, w2):
    b, L = kv.shape[0], kv.shape[1]
    n_ch = L // CMP_STRIDE
    n_cmp = n_ch - CMP_RATIO + 1
    ch = kv[:, :n_ch * CMP_STRIDE].reshape(b, n_ch, CMP_STRIDE, NSA_KV_GROUPS, 2, NSA_HEAD_DIM)
    w1r = w1.reshape(2, CMP_RATIO, CMP_STRIDE, NSA_HEAD_DIM, NSA_HEAD_DIM)
    hid = b1[None, None, None]
    for r in range(CMP_RATIO):
        hid = hid + jnp.einsum('bnsgkd,ksde->bngke', ch[:, r:r + n_cmp], w1r[:, r])
    return jnp.einsum('bngke,kef->bngkf', jax.nn.gelu(hid), w2)


def cmp_branch(q, q_pos, kvc, slopes):
    n_cmp = kvc.shape[1]
    e_pos = jnp.arange(n_cmp) * CMP_STRIDE + (CMP_BLOCK - 1)
    dist = (q_pos[:, None] - e_pos[None, :]).astype(jnp.float32)
    logits = jnp.einsum('bqgrd,bngd->bqgrn', q, kvc[..., 0, :]).astype(jnp.float32)
    logits = logits - slopes[None, None, :, :, None] * dist[None, :, None, None, :]
    p = masked_softmax(logits, (dist >= 0)[None, :, None, None, :])
    o = jnp.einsum('bqgrn,bngd->bqgrd', p.astype(kvc.dtype), kvc[..., 1, :])
    return o, jnp.sum(p, axis=3)


def select_positions(p_grp, q_pos, seq_len):
    n_cmp = p_grp.shape[-1]
    n_slc = -(-seq_len // SLC_BLOCK)
    start = jnp.arange(n_cmp)[:, None] * CMP_STRIDE
    j = jnp.arange(n_slc)[None, :]
    overlap = ((start < (j + 1) * SLC_BLOCK) & (start + CMP_BLOCK > j * SLC_BLOCK)).astype(jnp.float32)
    p_slc = jnp.einsum('bqgn,nj->bqgj', p_grp, overlap)
    cur = (q_pos // SLC_BLOCK)[:, None]
    forced = (j == 0) | (j == cur) | (j == cur - 1)
    valid = j * SLC_BLOCK <= q_pos[:, None]
    score = jnp.where(forced[None, :, None, :], jnp.inf, jnp.where(valid[None, :, None, :], p_slc, -jnp.inf))
    k = min(SLC_TOPN, n_slc)
    _, idx = lax.top_k(score, k)
    pos = idx[..., None] * SLC_BLOCK + jnp.arange(SLC_BLOCK)
    return pos.reshape(idx.shape[0], idx.shape[1], idx.shape[2], k * SLC_BLOCK)


def slc_branch(q, q_pos, rows, pos, slopes):
    dist = (q_pos[None, :, None, None] - pos).astype(jnp.float32)
    logits = jnp.einsum('bqgrd,bqgkd->bqgrk', q, rows[..., 0, :]).astype(jnp.float32)
    logits = logits - slopes[None, None, :, :, None] * dist[:, :, :, None, :]
    p = masked_softmax(logits, (dist >= 0)[:, :, :, None, :])
    return jnp.einsum('bqgrk,bqgkd->bqgrd', p.astype(rows.dtype), rows[..., 1, :])


def win_branch(q, q_pos, kvw, k_pos, slopes):
    dist = q_pos[:, None] - k_pos[None, :]
    mask = (dist >= 0) & (dist < WINDOW) & (k_pos[None, :] >= 0)
    logits = jnp.einsum('bqgrd,bsgd->bqgrs', q, kvw[..., 0, :]).astype(jnp.float32)
    logits = logits - slopes[None, None, :, :, None] * dist.astype(jnp.float32)[None, :, None, None, :]
    p = masked_softmax(logits, mask[None, :, None, None, :])
    return jnp.einsum('bqgrs,bsgd->bqgrd', p.astype(kvw.dtype), kvw[..., 1, :])


def nsa_combine(gates, o_c, o_s, o_w):
    b, tq = gates.shape[0], gates.shape[1]
    g = jax.nn.sigmoid(gates.astype(jnp.float32)).reshape(b, tq, NSA_KV_GROUPS, NSA_REP, 3)
    o = g[..., 0:1] * o_c + g[..., 1:2] * o_s + g[..., 2:3] * o_w
    return o.reshape(b, tq, NSA_WIDTH).astype(o_c.dtype)


def nsa_prompt(q, kv_c, kv_s, kv_w, gates, w_phi1, b_phi1, w_phi2):
    b, t = q.shape[0], q.shape[1]
    slopes = alibi_slopes()
    kvc = compress_kv(kv_c, w_phi1, b_phi1, w_phi2)
    kvw_pad = jnp.pad(kv_w, ((0, 0), (WINDOW, 0), (0, 0), (0, 0), (0, 0)))
    b_idx = jnp.arange(b)[:, None, None, None]
    g_idx = jnp.arange(NSA_KV_GROUPS)[None, None, :, None]

    def block(q0):
        qb = lax.dynamic_slice_in_dim(q, q0, Q_BLOCK, axis=1)
        gb = lax.dynamic_slice_in_dim(gates, q0, Q_BLOCK, axis=1)
        q_pos = q0 + jnp.arange(Q_BLOCK)
        o_c, p_grp = cmp_branch(qb, q_pos, kvc, slopes)
        pos = select_positions(p_grp, q_pos, t)
        rows = kv_s[b_idx, jnp.minimum(pos, t - 1), g_idx]
        o_s = slc_branch(qb, q_pos, rows, pos, slopes)
        kw = lax.dynamic_slice_in_dim(kvw_pad, q0, WINDOW + Q_BLOCK, axis=1)
        k_pos = q0 - WINDOW + jnp.arange(WINDOW + Q_BLOCK)
        o_w = win_branch(qb, q_pos, kw, k_pos, slopes)
        return nsa_combine(gb, o_c, o_s, o_w)

    out = lax.map(block, jnp.arange(0, t, Q_BLOCK))
    return out.transpose(1, 0, 2, 3).reshape(b, t, NSA_WIDTH)


def nsa_sample(q, kv_c, kv_s, kv_w, gates, pool_c, pool_s, win_buf, page_table, w_phi1, b_phi1, w_phi2):
    b, t = q.shape[0], q.shape[1]
    past = page_table.shape[1] * PAGE_SIZE
    slopes = alibi_slopes()
    q_pos = past + jnp.arange(t)
    b_idx = jnp.arange(b)[:, None, None, None]
    g_idx = jnp.arange(NSA_KV_GROUPS)[None, None, :, None]
    past_c = pool_c[page_table].reshape(b, past, NSA_KV_GROUPS, 2, NSA_HEAD_DIM)
    kvc = compress_kv(jnp.concatenate([past_c, kv_c.astype(past_c.dtype)], axis=1), w_phi1, b_phi1, w_phi2)
    o_c, p_grp = cmp_branch(q, q_pos, kvc, slopes)
    pos = select_positions(p_grp, q_pos, past + t)
    pc = jnp.minimum(pos, past - 1)
    phys = page_table[b_idx, pc // PAGE_SIZE]
    rows_past = pool_s[phys, pc % PAGE_SIZE, g_idx]
    rows_new = kv_s[b_idx, jnp.clip(pos - past, 0, t - 1), g_idx].astype(rows_past.dtype)
    rows = jnp.where((pos < past)[..., None, None], rows_past, rows_new)
    o_s = slc_branch(q, q_pos, rows, pos, slopes)
    w_len = win_buf.shape[1]
    kw = jnp.concatenate([win_buf, kv_w.astype(win_buf.dtype)], axis=1)
    k_pos = past - w_len + jnp.arange(w_len + t)
    o_w = win_branch(q, q_pos, kw, k_pos, slopes)
    return nsa_combine(gates, o_c, o_s, o_w), kw[:, t:]


def hgrn2_chunked(q, k, v, logf, s0):
    b, t = q.shape[0], q.shape[1]
    n_ch = -(-t // HGRN_CHUNK)
    pad = n_ch * HGRN_CHUNK - t

    def prep(a):
        a = jnp.pad(a, ((0, 0), (0, pad), (0, 0), (0, 0)))
        return a.reshape(b, n_ch, HGRN_CHUNK, a.shape[2], a.shape[3]).transpose(1, 0, 3, 2, 4)

    causal = jnp.tril(jnp.ones((HGRN_CHUNK, HGRN_CHUNK), dtype=bool))

    def step(s, inp):
        qc, kc, vc, lc = inp
        bcum = jnp.cumsum(lc, axis=2)
        b_last = bcum[:, :, -1:, :]
        qe = qc * jnp.exp(bcum)
        ke = kc * jnp.exp(-bcum)
        o = jnp.einsum('bhck,bhkv->bhcv', qe, s)
        a = jnp.where(causal, jnp.einsum('bhck,bhsk->bhcs', qe, ke), 0.0)
        o = o + jnp.einsum('bhcs,bhsv->bhcv', a, vc)
        s_new = jnp.exp(b_last[:, :, 0, :])[..., None] * s + jnp.einsum('bhsk,bhsv->bhkv', kc * jnp.exp(b_last - bcum), vc)
        return s_new, o

    s_fin, os = lax.scan(step, s0, (prep(q), prep(k), prep(v), prep(logf)))
    o = os.transpose(1, 0, 3, 2, 4).reshape(b, n_ch * HGRN_CHUNK, HGRN_HEADS, HGRN_DV)[:, :t]
    return o, s_fin


def hgrn2_mixer(q_raw, f_raw, i_raw, og_raw, lb, g_out, s0):
    b, t = q_raw.shape[0], q_raw.shape[1]
    shp_k = (b, t, HGRN_HEADS, HGRN_DK)
    q = jax.nn.silu(q_raw.astype(jnp.float32)).reshape(shp_k)
    fz = f_raw.astype(jnp.float32)
    logf = jnp.log(lb + (1.0 - lb) * jax.nn.sigmoid(fz)).reshape(shp_k)
    k = ((1.0 - lb) * jax.nn.sigmoid(-fz)).reshape(shp_k)
    v = i_raw.astype(jnp.float32).reshape(b, t, HGRN_HEADS, HGRN_DV)
    o, s_new = hgrn2_chunked(q, k, v, logf, s0)
    o = rms_norm(o, g_out) * jax.nn.silu(og_raw.astype(jnp.float32)).reshape(b, t, HGRN_HEADS, HGRN_DV)
    return o.reshape(b, t, HGRN_WIDTH).astype(q_raw.dtype), s_new


def hier_moe(h, w_rg, b_rg, w_re, b_re, w_gate, w_up, w_down):
    b, t, d = h.shape
    hf = h.reshape(b * t, d)
    pg = jax.nn.softmax((hf @ w_rg).astype(jnp.float32) + b_rg.astype(jnp.float32), axis=-1)
    g_star = jnp.argmax(pg, axis=-1)
    pg_top = jnp.max(pg, axis=-1)
    le = ((hf @ w_re).astype(jnp.float32) + b_re.astype(jnp.float32)).reshape(-1, N_GROUPS, EXPERTS_PER_GROUP)
    le_g = jnp.einsum('nge,ng->ne', le, jax.nn.one_hot(g_star, N_GROUPS, dtype=jnp.float32))
    top_v, top_i = lax.top_k(le_g, EXPERT_TOPK)
    w_top = jax.nn.softmax(top_v, axis=-1) * pg_top[:, None]
    comb = jnp.einsum('nk,nke->ne', w_top, jax.nn.one_hot(g_star[:, None] * EXPERTS_PER_GROUP + top_i, N_EXPERTS, dtype=jnp.float32))
    y = jnp.zeros((b * t, d), jnp.float32)
    for grp in range(N_GROUPS):
        sl = slice(grp * EXPERTS_PER_GROUP, (grp + 1) * EXPERTS_PER_GROUP)
        hg = jnp.einsum('nd,edf->nef', hf, w_gate[sl])
        hu = jnp.einsum('nd,edf->nef', hf, w_up[sl])
        act = (jax.nn.silu(hg) * hu) * comb[:, sl, None]
        y = y + jnp.einsum('nef,efd->nd', act, w_down[sl]).astype(jnp.float32)
    return y.reshape(b, t, d).astype(h.dtype)


def setup_inputs(seed: int = 0) -> dict:
    key = jax.random.key(seed)
    ks = jax.random.split(key, 32)

    def nrm(i, shape, scale):
        return scale * jax.random.normal(ks[i], shape, jnp.float32)

    n_pages = PAST_LEN // PAGE_SIZE
    n_used = DEC_BATCH * n_pages
    n_pool = n_used + max(1, n_used // 4)
    w_buf = min(WINDOW, PAST_LEN)
    page_table = jax.random.permutation(ks[8], n_pool)[:n_used].reshape(DEC_BATCH, n_pages).astype(jnp.int32)
    D = D_MODEL
    return {
        'x_prompt': nrm(0, (BATCH, SEQ, D), 1.0),
        'x_sample': nrm(1, (DEC_BATCH, DEC_SEQ, D), 1.0),
        'c_prompt': nrm(2, (BATCH, D), 1.0),
        'c_sample': nrm(3, (DEC_BATCH, D), 1.0),
        'cache_cmp_kv': nrm(4, (DEPTH, n_pool, PAGE_SIZE, NSA_KV_GROUPS, 2, NSA_HEAD_DIM), 1.0),
        'cache_slc_kv': nrm(5, (DEPTH, n_pool, PAGE_SIZE, NSA_KV_GROUPS, 2, NSA_HEAD_DIM), 1.0),
        'cache_win_kv': nrm(6, (DEPTH, DEC_BATCH, w_buf, NSA_KV_GROUPS, 2, NSA_HEAD_DIM), 1.0),
        'state_hgrn': nrm(7, (DEPTH, DEC_BATCH, HGRN_HEADS, HGRN_DK, HGRN_DV), 0.5),
        'page_table': page_table,
        'w_ada': nrm(9, (DEPTH, D, 6 * D), 0.5 * D ** -0.5),
        'b_ada': nrm(10, (DEPTH, 6 * D), 0.02),
        'g_pre_mix': 1.0 + nrm(11, (DEPTH, D), 0.05),
        'g_post_mix': 1.0 + nrm(12, (DEPTH, D), 0.05),
        'g_pre_ffn': 1.0 + nrm(13, (DEPTH, D), 0.05),
        'g_post_ffn': 1.0 + nrm(14, (DEPTH, D), 0.05),
        'w_in': nrm(15, (DEPTH, D, IN_COLS), D ** -0.5),
        'w_phi1': nrm(16, (DEPTH, 2, CMP_BLOCK, NSA_HEAD_DIM, NSA_HEAD_DIM), (CMP_BLOCK * NSA_HEAD_DIM) ** -0.5),
        'b_phi1': nrm(17, (DEPTH, 2, NSA_HEAD_DIM), 0.02),
        'w_phi2': nrm(18, (DEPTH, 2, NSA_HEAD_DIM, NSA_HEAD_DIM), NSA_HEAD_DIM ** -0.5),
        'g_nsa_out': 1.0 + nrm(19, (DEPTH, NSA_WIDTH), 0.05),
        'hgrn_lb_logits': nrm(20, (DEPTH + 1, HGRN_HEADS * HGRN_DK), 0.1),
        'g_hgrn_out': 1.0 + nrm(21, (DEPTH, HGRN_HEADS, HGRN_DV), 0.05),
        'w_out': nrm(22, (DEPTH, MIX_WIDTH, D), MIX_WIDTH ** -0.5),
        'w_route_group': nrm(23, (DEPTH, D, N_GROUPS), D ** -0.5),
        'b_route_group': nrm(24, (DEPTH, N_GROUPS), 0.01),
        'w_route_expert': nrm(25, (DEPTH, D, N_EXPERTS), D ** -0.5),
        'b_route_expert': nrm(26, (DEPTH, N_EXPERTS), 0.01),
        'w_exp_gate': nrm(27, (DEPTH, N_EXPERTS, D, D_EXPERT), D ** -0.5),
        'w_exp_up': nrm(28, (DEPTH, N_EXPERTS, D, D_EXPERT), D ** -0.5),
        'w_exp_down': nrm(29, (DEPTH, N_EXPERTS, D_EXPERT, D), D_EXPERT ** -0.5),
    }


def reference(x_prompt, x_sample, c_prompt, c_sample, cache_cmp_kv, cache_slc_kv, cache_win_kv, state_hgrn, page_table,
              w_ada, b_ada, g_pre_mix, g_post_mix, g_pre_ffn, g_post_ffn, w_in, w_phi1, b_phi1, w_phi2, g_nsa_out,
              hgrn_lb_logits, g_hgrn_out, w_out, w_route_group, b_route_group, w_route_expert, b_route_expert,
              w_exp_gate, w_exp_up, w_exp_down):
    lower_bounds = jnp.cumsum(jax.nn.softmax(hgrn_lb_logits.astype(jnp.float32), axis=0), axis=0)

    def run_layer(x, c, l, mixer):
        sh_a, sc_a, gt_a, sh_f, sc_f, gt_f = adaln(c, w_ada[l], b_ada[l])
        h = rms_norm(x, g_pre_mix[l]) * (1.0 + sc_a) + sh_a
        parts = split_projection(h @ w_in[l])
        o_nsa, o_hgrn, states = mixer(parts, l)
        mixed = jnp.concatenate([rms_norm(o_nsa, g_nsa_out[l]), o_hgrn], axis=-1) @ w_out[l]
        x = x + gt_a * rms_norm(mixed, g_post_mix[l])
        h = rms_norm(x, g_pre_ffn[l]) * (1.0 + sc_f) + sh_f
        ffn = hier_moe(h, w_route_group[l], b_route_group[l], w_route_expert[l], b_route_expert[l],
                       w_exp_gate[l], w_exp_up[l], w_exp_down[l])
        x = x + gt_f * rms_norm(ffn, g_post_ffn[l])
        return x, states

    def prompt_mixer(parts, l):
        q, kv_c, kv_s, kv_w, gates, hq, hf, hi, hog = parts
        o_nsa = nsa_prompt(q, kv_c, kv_s, kv_w, gates, w_phi1[l], b_phi1[l], w_phi2[l])
        s0 = jnp.zeros((q.shape[0], HGRN_HEADS, HGRN_DK, HGRN_DV), jnp.float32)
        o_h, s_new = hgrn2_mixer(hq, hf, hi, hog, lower_bounds[l], g_hgrn_out[l], s0)
        w_keep = min(WINDOW, kv_w.shape[1])
        return o_nsa, o_h, (kv_c, kv_s, kv_w[:, kv_w.shape[1] - w_keep:], s_new.astype(x_prompt.dtype))

    def sample_mixer(parts, l):
        q, kv_c, kv_s, kv_w, gates, hq, hf, hi, hog = parts
        o_nsa, new_buf = nsa_sample(q, kv_c, kv_s, kv_w, gates, cache_cmp_kv[l], cache_slc_kv[l], cache_win_kv[l],
                                    page_table, w_phi1[l], b_phi1[l], w_phi2[l])
        o_h, s_new = hgrn2_mixer(hq, hf, hi, hog, lower_bounds[l], g_hgrn_out[l], state_hgrn[l].astype(jnp.float32))
        return o_nsa, o_h, (kv_c, kv_s, new_buf, s_new.astype(state_hgrn.dtype))

    y_p = x_prompt
    y_s = x_sample
    p_states = []
    s_states = []
    for l in range(DEPTH):
        y_p, st_p = run_layer(y_p, c_prompt, l, prompt_mixer)
        p_states.append(st_p)
        y_s, st_s = run_layer(y_s, c_sample, l, sample_mixer)
        s_states.append(st_s)
    p_cmp = jnp.stack([s[0] for s in p_states])
    p_slc = jnp.stack([s[1] for s in p_states])
    p_win = jnp.stack([s[2] for s in p_states])
    p_hgrn = jnp.stack([s[3] for s in p_states])
    s_cmp = jnp.stack([s[0] for s in s_states])
    s_slc = jnp.stack([s[1] for s in s_states])
    s_win = jnp.stack([s[2] for s in s_states])
    s_hgrn = jnp.stack([s[3] for s in s_states])
    return (y_p, y_s, p_cmp, p_slc, p_win, p_hgrn, s_cmp, s_slc, s_win, s_hgrn)
```

```python
import os
import numpy as np
from contextlib import ExitStack
import concourse.bass as bass
import concourse.mybir as mybir
from concourse.bass_utils import run_bass_kernel_spmd

F32 = mybir.dt.float32
BF16 = mybir.dt.bfloat16
I32 = mybir.dt.int32
AF = mybir.ActivationFunctionType
ALU = mybir.AluOpType
AX = mybir.AxisListType

NCORES = 8
D = 1024
SEQ = int(os.environ.get('KSEQ', 4096))
NB = 2
NS = 16
NT = SEQ // 128
NST = SEQ // 512
INC = 3352
PAST = 8192
NPAGE = 64
NPOOL = int(os.environ.get("KNPOOL", 10240))
EPS = 1e-6
NEG = -30000.0
TOKS = NB * SEQ + 128
MB = min(1024, SEQ)
STAGE = int(os.environ.get("KSTAGE", "4"))


class Sem:
    __slots__ = ("h", "val")

    def __init__(self, h):
        self.h = h
        self.val = 0


class Tok:
    __slots__ = ("w", "r", "ds", "x")

    def __init__(self):
        self.w = {}
        self.r = {}
        self.ds = {}
        self.x = False


class K:
    def __init__(self, nc):
        self.nc = nc
        self.eng = {"pe": nc.tensor, "act": nc.scalar, "dve": nc.vector, "pool": nc.gpsimd, "sp": nc.sync}
        self.esem = {n: Sem(nc.alloc_semaphore("e_" + n)) for n in self.eng}
        self.seen = {n: {} for n in self.eng}
        self.dsems = []
        self.nins = 0

    def tok(self):
        return Tok()

    def _wait(self, en, deps):
        seen = self.seen[en]
        e = self.eng[en]
        for s, v in deps.items():
            if seen.get(s, 0) < v:
                e.wait_ge(s.h, v)
                seen[s] = v
                self.nins += 1

    def _deps(self, en, reads, writes):
        deps = {}
        pes = self.esem["pe"]
        own = self.esem.get(en)
        for t in reads:
            for s, v in t.w.items():
                if deps.get(s, 0) < v:
                    deps[s] = v
            if t.x:
                for s, v in t.r.items():
                    if s is not own and deps.get(s, 0) < v:
                        deps[s] = v
        for t in writes:
            for s, v in t.r.items():
                if deps.get(s, 0) < v:
                    deps[s] = v
            for s, v in t.w.items():
                if en == "pe" and s is pes:
                    continue
                if deps.get(s, 0) < v:
                    deps[s] = v
        return deps

    def op(self, en, fn, reads=(), writes=()):
        self._wait(en, self._deps(en, reads, writes))
        ins = fn(self.eng[en])
        s = self.esem[en]
        s.val += 1
        ins.then_inc(s.h, 1)
        self.nins += 1
        for t in reads:
            t.r[s] = s.val
        for t in writes:
            t.w[s] = s.val

    def dsem(self, t, q):
        key = "sw" if q == "pool" else "hw"
        if key not in t.ds:
            t.ds[key] = Sem(self.nc.alloc_semaphore("d%d" % len(self.dsems)))
            self.dsems.append(t.ds[key])
        return t.ds[key]

    def dma(self, q, out, in_, sb, reads=(), writes=(), **kw):
        self._wait(q, self._deps(q, reads, writes))
        s = self.dsem(sb, q)
        ins = self.eng[q].dma_start(out=out, in_=in_, **kw)
        s.val += 16
        ins.then_inc(s.h, 16)
        self.nins += 1
        for t in reads:
            t.r[s] = s.val
        for t in writes:
            t.w[s] = s.val

    def idma(self, out, in_, idx_ap, sb, reads=(), writes=()):
        q = "pool"
        self._wait(q, self._deps(q, reads, writes))
        s = self.dsem(sb, q)
        ins = self.eng[q].indirect_dma_start(out=out, out_offset=None, in_=in_,
                                             in_offset=bass.IndirectOffsetOnAxis(ap=idx_ap, axis=0))
        s.val += 16
        ins.then_inc(s.h, 16)
        self.nins += 1
        for t in reads:
            t.r[s] = s.val
        for t in writes:
            t.w[s] = s.val

    def barrier(self):
        allsems = list(self.esem.values()) + self.dsems
        for en in self.eng:
            self._wait(en, {s: s.val for s in allsems if s.val > 0})
        for en in self.eng:
            s = self.esem[en]
            ins = self.eng[en].nop()
            s.val += 1
            ins.then_inc(s.h, 1)
        for en in self.eng:
            self._wait(en, {s: s.val for s in self.esem.values()})


class Tl:
    def __init__(self, k, t):
        self.t = t
        self.tok = k.tok()

    def __getitem__(self, key):
        return self.t[key]


def _consts(stage=0):
    c = {}
    c["ident"] = np.eye(128, dtype=np.float32)
    key = np.arange(128)[:, None]
    tok = np.arange(128)[None, :]
    masks = np.zeros((128, 19, 128), np.float32)
    masks[:, 0, :] = np.where(key <= tok, 0.0, NEG)
    masks[:, 1, :] = np.where(key > tok, 0.0, NEG)
    for dl in range(17):
        masks[:, 2 + dl, :] = np.where(16 * key + 31 <= 128 * dl + tok, 0.0, NEG)
    c["masks"] = masks
    efull = np.zeros((64, SEQ), np.float32)
    efull[np.arange(SEQ) // 64, np.arange(SEQ)] = 1.0
    c["efull"] = efull
    n = np.arange(256)[:, None]
    j = np.arange(64)[None, :]
    ov = ((n * 16 < (j + 1) * 64) & (n * 16 + 32 > j * 64) & (n < 255)).astype(np.float32)
    ovp = np.zeros((128, 2, 64), np.float32)
    ovp[:, 0, :] = ov[0:128]
    ovp[:, 1, :] = ov[128:256]
    c["ov"] = ovp
    t = np.arange(SEQ)
    kaug = np.stack([128.0 * (t // 128), (t % 128).astype(np.float64), np.ones(SEQ), np.ones(SEQ)]).astype(np.float32)
    c["kaug"] = kaug
    e = np.arange(256) * 16 + 31
    caug = np.stack([128.0 * (e // 128), (e % 128).astype(np.float64), np.ones(256), np.ones(256)]).astype(np.float32)
    c["caug"] = caug
    slopes = np.exp2(-8.0 * np.arange(1, 9) / 8.0)
    qaug = np.zeros((4, 8, SEQ), np.float32)
    for h in range(8):
        qaug[0, h, :] = slopes[h]
        qaug[1, h, :] = slopes[h]
        qaug[2, h, :] = -slopes[h] * 128.0 * (t // 128)
        qaug[3, h, :] = -slopes[h] * 127.0
    c["qaug"] = qaug
    pos = np.arange(SEQ)[:, None]
    jj = np.arange(64)[None, :]
    cur = pos // 64
    forced = (jj == 0) | (jj == cur) | (jj == cur - 1)
    valid = jj * 64 <= pos
    selv = (valid & ~forced).astype(np.float32)
    sela = np.where(forced, 100.0, np.where(valid, 0.0, -1.0)).astype(np.float32)
    c["selv"] = selv
    c["sela"] = sela
    s_ = np.arange(128)[:, None]
    t_ = np.arange(128)[None, :]
    same = (s_ // 64) == (t_ // 64)
    hm = np.zeros((128, 3, 128), np.float32)
    hm[:, 0, :] = same & (s_ <= t_)
    hm[:, 1, :] = same & (s_ > t_)
    hm[:, 2, :] = same & (s_ <= t_)
    c["hmat"] = hm
    ci = np.zeros((128, 2), np.float32)
    ci[0:64, 0] = 1.0
    ci[64:128, 1] = 1.0
    c["cind"] = ci
    se = np.zeros((16, 16, 128), np.float32)
    for E in range(16):
        se[E, E, :] = 1.0
    c["selE"] = se
    if stage >= 4:
        pg = np.arange(NPAGE)[None, :, None]
        sl = slopes[None, None, :]
        c["pgw"] = np.ascontiguousarray(np.broadcast_to(np.exp(-sl * 128.0 * (63 - pg)), (128, NPAGE, 8))).astype(np.float32)
        g4 = np.zeros((128, 32), np.float32)
        for g in range(2):
            for h in range(4):
                for s_i in range(16):
                    g4[g * 64 + h * 16 + s_i, 2 * s_i + g] = 1.0
        c["g4"] = g4
        c["eye16"] = np.ascontiguousarray(np.broadcast_to(np.eye(16, dtype=np.float32), (128, 16, 16)))
        c["eyem"] = np.ascontiguousarray(np.broadcast_to(np.eye(16, dtype=np.float32)[:, None, :], (16, 4, 16)))
        vm = np.ones((128, 8), np.float32)
        vm[127, 3] = 0.0
        vm[0, 4] = 0.0
        c["vm"] = vm
        sadd = np.zeros((32, 128), np.float32)
        sadd[:, 0] = 100.0
        sadd[:, 127] = 100.0
        c["sadd"] = sadd
        c["iop"] = np.arange(128, dtype=np.float32).reshape(128, 1)
        kk = np.arange(PAST)
        efS = np.zeros((128, PAST), np.float32)
        efS[kk // 64, kk] = 1.0
        c["efS"] = efS
        nn_ = (np.arange(4)[None, :, None] * 128 + np.arange(128)[:, None, None])
        j_ = np.arange(128)[None, None, :]
        c["ovS"] = ((nn_ * 16 < (j_ + 1) * 64) & (nn_ * 16 + 32 > j_ * 64) & (nn_ < 511)).astype(np.float32)
        qa = np.zeros((2, 8, 16), np.float32)
        qa[:, :, :] = -slopes[None, :, None]
        c["qaugS"] = qa
        dist = PAST - (np.arange(512) * 16 + 31)
        dist[511] = 0
        c["caugS"] = np.stack([128.0 * (dist // 128), (dist % 128).astype(np.float64)]).astype(np.float32)
        c["paugS"] = np.stack([128.0 - np.arange(128), np.zeros(128)]).astype(np.float32)
        dw = 512 - np.arange(512)
        c["waugS"] = np.stack([128.0 * (dw // 128), (dw % 128).astype(np.float64)]).astype(np.float32)
    return c


def build(stage):
    nc = bass.Bass("TRN2", target_bir_lowering=False)
    k = K(nc)
    cst = _consts(stage)

    def din(name, shape, dt=F32):
        return nc.dram_tensor(name, list(shape), dt, kind="ExternalInput").ap()

    def dout(name, shape, dt=F32):
        return nc.dram_tensor(name, list(shape), dt, kind="ExternalOutput").ap()

    xp = din("xp", [NB, SEQ, D])
    call = din("call", [18, D])
    w_ada = din("w_ada", [D, 6 * D])
    b_ada = din("b_ada", [1, 6 * D])
    gb = din("gb", [128, 4, D])
    gcol_d = din("gcol", [128, 4, 8])
    g512 = din("g512", [128, 4, 512])
    w_in = din("w_in", [D, INC])
    w_out = din("w_out", [D, D])
    wphi1 = din("wphi1", [128, 32, 128])
    wphi2 = din("wphi2", [128, 64])
    bphi1 = din("bphi1", [128, 1])
    wroute = din("wroute", [D, 20])
    broute = din("broute", [128, 20])
    if stage >= 3:
        wg_d = din("wg", [16, D, 256])
        wu_d = din("wu", [16, D, 256])
        wd_d = din("wd", [16, 256, D])
    cd = {n: din("c_" + n, a.shape) for n, a in cst.items()}

    yp = dout("yp", [NB, SEQ, D])
    pcmp = dout("pcmp", [NB, SEQ, 256])
    pslc = dout("pslc", [NB, SEQ, 256])
    pwin = dout("pwin", [NB, 512, 256])
    phg = dout("phg", [NB, 4, 128, 128])
    dbg = dout("dbg", [NB, SEQ, D]) if stage < 0 else None

    x1s = nc.dram_tensor("x1s", [TOKS, D], F32).ap()
    mxs = (nc.dram_tensor("mxs", [8, 128, TOKS], BF16, kind="ExternalOutput").ap() if os.environ.get("KDBG") else nc.dram_tensor("mxs", [8, 128, TOKS], BF16).ap())
    gvs = nc.dram_tensor("gvs", [2, 2, 128, D], F32).ap()
    aas = nc.dram_tensor("aas", [18, 6 * D], F32).ap()
    sms = nc.dram_tensor("sms", [6, NS, D], F32).ap()
    wgs = nc.dram_tensor("wgs", [16, D, 256], BF16).ap()
    wus = nc.dram_tensor("wus", [16, D, 256], BF16).ap()
    wds = nc.dram_tensor("wds", [16, 256, D], BF16).ap()

    def sb(name, shape, dt=F32, es=None):
        if es is not None:
            return Tl(k, es.enter_context(nc.sbuf_tensor(name, list(shape), dt)))
        return Tl(k, nc.alloc_sbuf_tensor(name, list(shape), dt))

    PS = [Tl(k, nc.alloc_psum_tensor("ps%d" % i, [128, 512], F32)) for i in range(8)]
    for p_ in PS:
        p_.tok.x = True
    rr = [0]

    def psum():
        b = PS[rr[0] % 4]
        rr[0] += 1
        return b

    def mm(out, lhsT, rhs, start, stop, reads, writes, skip=False):
        k.op("pe", lambda e: e.matmul(out, lhsT=lhsT, rhs=rhs, start=start, stop=stop, skip_group_check=skip), reads=reads, writes=writes)

    def tr(out, in_, idn, reads, writes):
        k.op("pe", lambda e: e.transpose(out, in_, idn), reads=reads, writes=writes)

    cgrp = k.tok()
    ident = sb("ident", [128, 128])
    identb = sb("identb", [128, 128], BF16)
    k.dma("sp", ident[:], cd["ident"], cgrp, writes=[cgrp])
    k.op("dve", lambda e: e.tensor_copy(identb[:], ident[:]), reads=[cgrp], writes=[identb.tok])
    aT = sb("aT", [128, 48, 18])
    gcol = sb("gcol_s", [128, 4, 8])
    k.dma("sp", gcol[:], gcol_d, cgrp, writes=[cgrp])

    es0 = ExitStack()
    aall = sb("aall", [18, 6 * D], es=es0)
    ones1 = sb("ones1", [1, 128], es=es0)
    ct = sb("ct", [18, D], es=es0)
    ct2 = sb("ct2", [18, D], es=es0)
    cT = sb("cT", [128, 8, 18], es=es0)
    bada = sb("bada", [1, 6 * D], es=es0)
    gbt = sb("gbt", [128, 4, D], es=es0)
    sel = sb("sel", [18, 2, 128], es=es0)
    gvt = sb("gvt", [128, D], es=es0)
    wa = [sb("wa%d" % i, [128, 8, 512], es=es0) for i in range(2)]
    k.dma("sp", ct[:], call, ct.tok, writes=[ct.tok])
    k.dma("sp", bada[:], b_ada, bada.tok, writes=[bada.tok])
    k.dma("sp", gbt[:], gb, gbt.tok, writes=[gbt.tok])
    k.op("pool", lambda e: e.memset(ones1[:], 1.0), writes=[ones1.tok])
    k.op("act", lambda e: e.activation(out=ct2[:], in_=ct[:], func=AF.Silu), reads=[ct.tok], writes=[ct2.tok])
    for kc in range(8):
        p = psum()
        tr(p[:, 0:18], ct2[:, kc * 128:(kc + 1) * 128], ident[0:18, 0:18], [ct2.tok, cgrp], [p.tok])
        k.op("dve", lambda e, kc=kc, p=p: e.tensor_copy(cT[:, kc, :], p[:, 0:18]), reads=[p.tok], writes=[cT.tok])
    for cb in range(12):
        wt = wa[cb % 2]
        k.dma("sp", wt[:], w_ada[:, cb * 512:(cb + 1) * 512].rearrange("(c p) n -> p c n", p=128), wt.tok,
              writes=[wt.tok])
        p = psum()
        for kc in range(8):
            mm(p[0:18, :], cT[:, kc, :], wt[:, kc, :], kc == 0, False, [cT.tok, wt.tok], [p.tok])
        mm(p[0:18, :], ones1[0:1, 0:18], bada[0:1, cb * 512:(cb + 1) * 512], False, True, [ones1.tok, bada.tok], [p.tok])
        k.op("dve", lambda e, p=p, cb=cb: e.tensor_copy(aall[:, cb * 512:(cb + 1) * 512], p[0:18, :]),
             reads=[p.tok], writes=[aall.tok])
    k.dma("sp", aas, aall[:], aall.tok, reads=[aall.tok])
    for ch in range(48):
        p = psum()
        tr(p[:, 0:18], aall[:, ch * 128:(ch + 1) * 128], ident[0:18, 0:18], [aall.tok, cgrp], [p.tok])
        k.op("act", lambda e, ch=ch, p=p: e.copy(aT[:, ch, :], p[:, 0:18]), reads=[p.tok], writes=[aT.tok])
    k.op("pool", lambda e: e.memset(sel[:], 0.0), writes=[sel.tok])
    for b in range(2):
        k.op("pool", lambda e, b=b: e.affine_select(out=sel[:, b, :], in_=sel[:, b, :], pattern=[[0, 128]],
                                                    compare_op=ALU.not_equal, fill=1.0, base=-(16 + b),
                                                    channel_multiplier=1),
             reads=[sel.tok], writes=[sel.tok])
    for b in range(2):
        for gi, (part, gidx) in enumerate(((2, 1), (5, 3))):
            for half in range(2):
                p = psum()
                c0 = part * D + half * 512
                mm(p[:, :], sel[:, b, :], aall[:, c0:c0 + 512], True, True, [sel.tok, aall.tok], [p.tok])
                k.op("dve", lambda e, p=p, half=half, gidx=gidx: e.tensor_mul(
                    gvt[:, half * 512:(half + 1) * 512], p[:, :], gbt[:, gidx, half * 512:(half + 1) * 512]),
                    reads=[p.tok, gbt.tok], writes=[gvt.tok])
            k.dma("sp", gvs[b, gi], gvt[:], gvt.tok, reads=[gvt.tok])
    for i, (gidx, part, kind) in enumerate(((0, 1, "scale"), (None, 0, "shift"), (1, 2, "gate"), (2, 4, "scale"), (None, 3, "shift"), (3, 5, "gate"))):
        src = aall[0:16, part * D:(part + 1) * D]
        if kind == "scale":
            k.op("dve", lambda e, src=src, gidx=gidx: e.scalar_tensor_tensor(out=gvt[0:16, :], in0=src, scalar=1.0, in1=gbt[0:16, gidx, :], op0=ALU.add, op1=ALU.mult),
                 reads=[aall.tok, gbt.tok, gvt.tok], writes=[gvt.tok])
        elif kind == "shift":
            k.op("dve", lambda e, src=src: e.tensor_copy(gvt[0:16, :], src), reads=[aall.tok, gvt.tok], writes=[gvt.tok])
        else:
            k.op("dve", lambda e, src=src, gidx=gidx: e.tensor_mul(gvt[0:16, :], src, gbt[0:16, gidx, :]), reads=[aall.tok, gbt.tok, gvt.tok], writes=[gvt.tok])
        k.dma("sp", sms[i], gvt[0:16, :], gvt.tok, reads=[gvt.tok])
    k.barrier()
    es0.close()

    esA = ExitStack()
    masks = sb("masks", [128, 19, 128], BF16, es=esA)
    efull = sb("efull", [64, SEQ], BF16, es=esA)
    ovt = sb("ov", [128, 2, 64], BF16, es=esA)
    hmat = sb("hmat", [128, 3, 128], es=esA)
    hmaskb = sb("hmaskb", [128, 128], BF16, es=esA)
    hmatb = sb("hmatb", [128, 4, 128], BF16, es=esA)
    cind = sb("cind", [128, 2], es=esA)
    g5 = sb("g5", [128, 4, 512], es=esA)
    omlt = sb("omlt", [128, 512], es=esA)
    wphi1t = sb("wphi1t", [128, 32, 128], BF16, es=esA)
    wphi2t = sb("wphi2t", [128, 64], BF16, es=esA)
    bphi1t = sb("bphi1t", [128, 1], es=esA)
    w_in_t = sb("w_in_t", [128, 8, INC], BF16, es=esA)
    k.dma("sp", hmat[:], cd["hmat"], cgrp, writes=[cgrp])
    k.dma("sp", cind[:], cd["cind"], cgrp, writes=[cgrp])
    k.dma("sp", g5[:], g512, cgrp, writes=[cgrp])
    k.dma("sp", bphi1t[:], bphi1, cgrp, writes=[cgrp])
    k.dma("pool", masks[:], cd["masks"], cgrp, writes=[cgrp])
    k.dma("pool", efull[:], cd["efull"], cgrp, writes=[cgrp])
    k.dma("pool", ovt[:], cd["ov"], cgrp, writes=[cgrp])
    k.dma("pool", wphi1t[:], wphi1, cgrp, writes=[cgrp])
    k.dma("pool", wphi2t[:], wphi2, cgrp, writes=[cgrp])
    for kc in range(8):
        k.dma("pool", w_in_t[:, kc, :], w_in[kc * 128:(kc + 1) * 128, :], cgrp, writes=[cgrp])
    k.op("dve", lambda e: e.tensor_copy(hmaskb[:], hmat[:, 2, :]), reads=[cgrp], writes=[hmaskb.tok])
    k.op("dve", lambda e: e.tensor_copy(hmatb[:, 0:3, :], hmat[:]), reads=[cgrp], writes=[hmatb.tok])
    k.op("dve", lambda e: e.tensor_copy(hmatb[:, 3, 0:2], cind[:]), reads=[cgrp, hmatb.tok], writes=[hmatb.tok])
    k.op("dve", lambda e: e.tensor_sub(omlt[:], g5[:, 2, :], g5[:, 3, :]), reads=[cgrp], writes=[omlt.tok])
    k.op("act", lambda e: e.activation(out=omlt[:], in_=omlt[:], func=AF.Exp), reads=[omlt.tok], writes=[omlt.tok])
    k.op("dve", lambda e: e.tensor_scalar_add(omlt[:], omlt[:], 1.0), reads=[omlt.tok], writes=[omlt.tok])
    k.op("dve", lambda e: e.reciprocal(omlt[:], omlt[:]), reads=[omlt.tok], writes=[omlt.tok])

    KsT = sb("KsT", [68, 2, SEQ], BF16, es=esA)
    Vs = sb("Vs", [128, NT, 2, 65], BF16, es=esA)
    KwT = sb("KwT", [68, 2, 1024], BF16, es=esA)
    Vw = sb("Vw", [128, 8, 2, 65], BF16, es=esA)
    kvcT = sb("kvcT", [128, 2, 528], BF16, es=esA)
    KcT = sb("KcT", [68, 2, 256], BF16, es=esA)
    GT = sb("GT", [128, 2, 256], BF16, es=esA)
    Vc = sb("Vc", [128, 2, 2, 65], BF16, es=esA)
    qT = sb("qT", [68, 8, 512], BF16, es=esA)
    hT = sb("hT", [128, 8, 512], BF16, es=esA)
    k.dma("pool", KsT[64:68, 0, :], cd["kaug"], cgrp, writes=[cgrp])
    k.dma("pool", KsT[64:68, 1, :], cd["kaug"], cgrp, writes=[cgrp])
    k.dma("pool", KcT[64:68, 0, :], cd["caug"], cgrp, writes=[cgrp])
    k.dma("pool", KcT[64:68, 1, :], cd["caug"], cgrp, writes=[cgrp])
    k.op("pool", lambda e: e.memset(Vs[:, :, :, 64:65], 1.0), writes=[Vs.tok])
    k.op("pool", lambda e: e.memset(Vw[:, :, :, 64:65], 1.0), writes=[Vw.tok])
    k.op("pool", lambda e: e.memset(Vc[:, :, :, 64:65], 1.0), writes=[Vc.tok])
    xt = [sb("xt%d" % i, [128, D], es=esA) for i in range(2)]
    xn = sb("xn", [128, D], BF16, es=esA)
    st4 = sb("st4", [128, 16], es=esA)
    mcol = sb("mcol", [128, 2, 8], es=esA)
    kvo = [sb("kvo%d" % i, [128, 768], es=esA) for i in range(2)]
    gat = sb("gat", [128, 4, 24], es=esA)
    mixin = sb("mixin", [128, 4, D], BF16, es=esA)
    mxT = sb("mxT", [128, 8, 128], BF16, es=esA)
    PT = [sb("PT%d" % i, [128, 512], BF16, es=esA) for i in range(4)]
    ptr = [0]
    selv = [sb("selv%d" % i, [128, 2, 64], es=esA) for i in range(2)]
    selT = sb("selT", [64, 4, 128], BF16, es=esA)
    sc = sb("sc", [128, 4, 64], es=esA)
    m8 = sb("m8", [128, 16], es=esA)
    rin = sb("rin", [128, 3, 4], es=esA)
    fac = sb("fac", [128, 3, 4], es=esA)
    sg = sb("sg", [128, 24], es=esA)
    onsa = sb("onsa", [128, 8, 64], es=esA)
    tmpo = sb("tmpo", [128, 4, 64], es=esA)
    GTn = sb("GTn", [128, 32], BF16, es=esA)
    Sf = [sb("Sf%d" % i, [128, 4, 128], es=esA) for i in range(2)]
    Sb_ = [sb("Sb%d" % i, [128, 4, 128], BF16, es=esA) for i in range(2)]
    h1 = sb("h1", [128, 512], es=esA)
    h2 = sb("h2", [128, 512], es=esA)
    qf = sb("qf", [128, 512], es=esA)
    kkf = sb("kkf", [128, 512], es=esA)
    lf = sb("lf", [128, 512], es=esA)
    lfh = sb("lfh", [128, 512], BF16, es=esA)
    lfl = sb("lfl", [128, 512], BF16, es=esA)
    vb = sb("vb", [128, 512], BF16, es=esA)
    sog = sb("sog", [128, 512], es=esA)
    qe = sb("qe", [128, 512], BF16, es=esA)
    ke = sb("ke", [128, 512], BF16, es=esA)
    kd = sb("kd", [128, 512], BF16, es=esA)
    qeT = sb("qeT", [128, 3, 4, 128], BF16, es=esA)
    keT = sb("keT", [128, 4, 128], BF16, es=esA)
    AT = sb("AT", [128, 4, 128], BF16, es=esA)
    dcol = sb("dcol", [128, 8], es=esA)
    ss4 = sb("ss4", [128, 12], es=esA)
    k.op("pool", lambda e: e.memset(qeT[:], 0.0), writes=[qeT.tok])
    k.barrier()
    print("phase A sbuf left:", nc.sbuf_bytes_remaining)

    def evac_scaled(dst, src, scale, reads, writes, eng):
        if eng == "act":
            k.op("act", lambda e: e.activation(out=dst, in_=src, func=AF.Copy, scale=scale), reads=reads, writes=writes)
        else:
            k.op("dve", lambda e: e.tensor_scalar_mul(dst, src, scale), reads=reads, writes=writes)

    def hgrn_tile(b, st, j, S0i):
        KH = int(os.environ.get("KH", "99"))
        tt = st * 4 + j
        S0, S1 = Sf[S0i], Sf[1 - S0i]
        S0b, S1b = Sb_[S0i], Sb_[1 - S0i]
        lhs = lambda kc: hT[:, kc, j * 128:(j + 1) * 128]
        for ci in range(4):
            p = PS[4 + ci]
            c0 = 1304 + ci * 512
            for kc in range(8):
                mm(p[:, :], lhs(kc), w_in_t[:, kc, c0:c0 + 512], kc == 0, kc == 7, [hT.tok], [p.tok])
        pq, pf, pi, po_ = PS[4], PS[5], PS[6], PS[7]
        if KH < 1:
            return S0i
        k.op("act", lambda e: e.activation(out=h1[:], in_=pq[:, :], func=AF.Exp, scale=-1.0), reads=[pq.tok], writes=[h1.tok])
        k.op("dve", lambda e: e.tensor_scalar_add(h1[:], h1[:], 1.0), reads=[h1.tok], writes=[h1.tok])
        k.op("dve", lambda e: e.reciprocal(h1[:], h1[:]), reads=[h1.tok], writes=[h1.tok])
        k.op("dve", lambda e: e.tensor_mul(qf[:], pq[:, :], h1[:]), reads=[pq.tok, h1.tok], writes=[qf.tok])
        if KH < 2:
            return S0i
        k.op("act", lambda e: e.activation(out=h2[:], in_=pf[:, :], func=AF.Exp, scale=-1.0), reads=[pf.tok], writes=[h2.tok])
        k.op("dve", lambda e: e.tensor_scalar_add(h1[:], h2[:], 1.0), reads=[h2.tok, h1.tok], writes=[h1.tok])
        k.op("dve", lambda e: e.reciprocal(h1[:], h1[:]), reads=[h1.tok], writes=[h1.tok])
        k.op("dve", lambda e: e.tensor_mul(h2[:], h2[:], h1[:]), reads=[h2.tok, h1.tok], writes=[h2.tok])
        k.op("dve", lambda e: e.tensor_mul(kkf[:], h2[:], omlt[:]), reads=[h2.tok, omlt.tok], writes=[kkf.tok])
        k.op("dve", lambda e: e.tensor_scalar(h2[:], kkf[:], -1.0, 1.0, ALU.mult, ALU.add), reads=[kkf.tok, h2.tok], writes=[h2.tok])
        k.op("act", lambda e: e.activation(out=lf[:], in_=h2[:], func=AF.Ln), reads=[h2.tok], writes=[lf.tok])
        if KH < 3:
            return S0i
        k.op("act", lambda e: e.copy(vb[:], pi[:, :]), reads=[pi.tok], writes=[vb.tok])
        k.op("act", lambda e: e.activation(out=h1[:], in_=po_[:, :], func=AF.Exp, scale=-1.0), reads=[po_.tok, h1.tok], writes=[h1.tok])
        k.op("dve", lambda e: e.tensor_scalar_add(h1[:], h1[:], 1.0), reads=[h1.tok], writes=[h1.tok])
        k.op("dve", lambda e: e.reciprocal(h1[:], h1[:]), reads=[h1.tok], writes=[h1.tok])
        k.op("dve", lambda e: e.tensor_mul(sog[:], po_[:, :], h1[:]), reads=[po_.tok, h1.tok], writes=[sog.tok])
        if KH < 4:
            return S0i
        pb, pr, pd = PS[4], PS[5], PS[6]
        k.op("dve", lambda e: e.tensor_copy(lfh[:], lf[:]), reads=[lf.tok, lfh.tok], writes=[lfh.tok])
        k.op("dve", lambda e: e.tensor_sub(lfl[:], lf[:], lfh[:]), reads=[lf.tok, lfh.tok, lfl.tok], writes=[lfl.tok])
        mm(pb[:, :], hmatb[:, 0, :], lfh[:], True, False, [lfh.tok, hmatb.tok], [pb.tok])
        mm(pb[:, :], hmatb[:, 0, :], lfl[:], False, True, [lfl.tok, hmatb.tok], [pb.tok])
        mm(pr[:, :], hmatb[:, 1, :], lfh[:], True, False, [lfh.tok, hmatb.tok], [pr.tok])
        mm(pr[:, :], hmatb[:, 1, :], lfl[:], False, True, [lfl.tok, hmatb.tok], [pr.tok])
        for h in range(4):
            mm(pd[:, 2 * h:2 * h + 2], lfh[:, h * 128:(h + 1) * 128], hmatb[:, 3, 0:2], h == 0, False, [lfh.tok, hmatb.tok], [pd.tok])
            mm(pd[:, 2 * h:2 * h + 2], lfl[:, h * 128:(h + 1) * 128], hmatb[:, 3, 0:2], False, h == 3, [lfl.tok, hmatb.tok], [pd.tok])
        KHB = int(os.environ.get("KHB", "99"))
        if KHB > 0:
            k.op("act", lambda e: e.activation(out=h1[:], in_=pb[:, :], func=AF.Exp), reads=[pb.tok, h1.tok] + ([pr.tok, pd.tok] if os.environ.get("KE2") else []), writes=[h1.tok])
        if KHB > 1:
            k.op("dve", lambda e: e.tensor_mul(qe[:], qf[:], h1[:]), reads=[qf.tok, h1.tok], writes=[qe.tok])
        if KHB > 2:
            k.op("act", lambda e: e.activation(out=h2[:], in_=pb[:, :], func=AF.Exp, scale=-1.0), reads=[pb.tok, h2.tok], writes=[h2.tok])
        if KHB > 3:
            k.op("dve", lambda e: e.tensor_mul(ke[:], kkf[:], h2[:]), reads=[kkf.tok, h2.tok], writes=[ke.tok])
        if KHB > 4:
            k.op("act", lambda e: e.activation(out=h1[:], in_=pr[:, :], func=AF.Exp), reads=[pr.tok, h1.tok], writes=[h1.tok])
        if KHB > 5:
            k.op("dve", lambda e: e.tensor_mul(kd[:], kkf[:], h1[:]), reads=[kkf.tok, h1.tok], writes=[kd.tok])
        if KHB > 6:
            k.op("act", lambda e: e.activation(out=dcol[:], in_=pd[:, 0:8], func=AF.Exp), reads=[pd.tok], writes=[dcol.tok])
        if KH < 6:
            return S0i
        pt = PS[7]
        ptb = pt[:].bitcast(BF16)
        for h in range(4):
            tr(ptb[:, h * 128:(h + 1) * 128], qe[:, h * 128:(h + 1) * 128], identb[:], [qe.tok, identb.tok], [pt.tok])
            tr(ptb[:, 512 + h * 128:512 + (h + 1) * 128], ke[:, h * 128:(h + 1) * 128], identb[:], [ke.tok, identb.tok], [pt.tok])
        q4 = ptb[:, 0:512].rearrange("p (h t) -> p h t", h=4)
        k.op("dve", lambda e: e.tensor_copy(qeT[:, 0, :, :], q4), reads=[pt.tok], writes=[qeT.tok])
        k.op("act", lambda e: e.copy(qeT[:, 1, :, 0:64], q4[:, :, 0:64]), reads=[pt.tok], writes=[qeT.tok])
        k.op("act", lambda e: e.copy(qeT[:, 2, :, 64:128], q4[:, :, 64:128]), reads=[pt.tok], writes=[qeT.tok])
        k.op("dve", lambda e: e.tensor_copy(keT[:], ptb[:, 512:1024].rearrange("p (h t) -> p h t", h=4)), reads=[pt.tok], writes=[keT.tok])
        if KH < 7:
            return S0i
        pa = PS[4]
        for h in range(4):
            mm(pa[:, h * 128:(h + 1) * 128], keT[:, h, :], qeT[:, 0, h, :], h == 0, h == 3, [keT.tok, qeT.tok], [pa.tok])
        k.op("dve", lambda e: e.tensor_mul(AT[:], pa[:, :].rearrange("p (h t) -> p h t", h=4),
                                           hmaskb[:].unsqueeze(1).to_broadcast([128, 4, 128])),
             reads=[pa.tok, hmaskb.tok], writes=[AT.tok])
        if KH < 8:
            return S0i
        def state_update(Sin, Sout, Soutb, lo, ci, bank):
            pS = PS[bank]
            for h in range(4):
                mm(pS[:, h * 128:(h + 1) * 128], kd[lo:lo + 64, h * 128:(h + 1) * 128], vb[lo:lo + 64, h * 128:(h + 1) * 128],
                   h == 0, h == 3, [kd.tok, vb.tok], [pS.tok])
            for h in range(4):
                k.op("dve", lambda e, h=h: e.scalar_tensor_tensor(out=Sout[:, h, :], in0=Sin[:, h, :], scalar=dcol[:, 2 * h + ci:2 * h + ci + 1],
                                                                   in1=pS[:, h * 128:(h + 1) * 128], op0=ALU.mult, op1=ALU.add),
                     reads=[Sin.tok, dcol.tok, pS.tok], writes=[Sout.tok])
            k.op("act", lambda e: e.copy(Soutb[:], Sout[:]), reads=[Sout.tok], writes=[Soutb.tok])
        state_update(S0, S1, S1b, 0, 0, 5)
        if KH < 9:
            return S0i
        pO = PS[6]
        for h in range(4):
            o_ = pO[:, h * 128:(h + 1) * 128]
            mm(o_, qeT[:, 1, h, :], S0b[:, h, :], h == 0, False, [qeT.tok, S0b.tok], [pO.tok])
            mm(o_, AT[:, h, :], vb[:, h * 128:(h + 1) * 128], False, False, [AT.tok, vb.tok], [pO.tok])
            mm(o_, qeT[:, 2, h, :], S1b[:, h, :], False, h == 3, [qeT.tok, S1b.tok], [pO.tok])
        if KH < 10:
            return S0i
        state_update(S1, S0, S0b, 64, 1, 7)
        if KH < 11:
            return S0i
        for h in range(4):
            k.op("act", lambda e, h=h: e.activation(out=h2[:, h * 128:(h + 1) * 128], in_=pO[:, h * 128:(h + 1) * 128], func=AF.Square,
                                                    accum_out=ss4[:, h:h + 1]),
                 reads=[pO.tok, h2.tok], writes=[h2.tok, ss4.tok])
        k.op("dve", lambda e: e.tensor_scalar(ss4[:, 4:8], ss4[:, 0:4], 1.0 / 128, EPS, ALU.mult, ALU.add), reads=[ss4.tok], writes=[ss4.tok])
        k.op("act", lambda e: e.activation(out=ss4[:, 4:8], in_=ss4[:, 4:8], func=AF.Ln), reads=[ss4.tok], writes=[ss4.tok])
        k.op("act", lambda e: e.activation(out=ss4[:, 8:12], in_=ss4[:, 4:8], func=AF.Exp, scale=-0.5), reads=[ss4.tok], writes=[ss4.tok])
        k.op("dve", lambda e: e.tensor_mul(h1[:].rearrange("p (h v) -> p h v", h=4), pO[:, :].rearrange("p (h v) -> p h v", h=4),
                                           ss4[:, 8:12].unsqueeze(2).to_broadcast([128, 4, 128])),
             reads=[pO.tok, ss4.tok, h1.tok], writes=[h1.tok])
        k.op("dve", lambda e: e.tensor_mul(h1[:], h1[:], g5[:, 1, :]), reads=[h1.tok, cgrp], writes=[h1.tok])
        k.op("dve", lambda e: e.tensor_mul(mixin[:, j, 512:1024], h1[:], sog[:]), reads=[h1.tok, sog.tok], writes=[mixin.tok])
        return S0i

    def attention_tile(b, st, j):
        qt = st * 4 + j
        tt = qt
        r0 = qt * 128
        tq = slice(j * 128, (j + 1) * 128)
        sv = selv[qt % 2]
        k.dma("sp", sv[:, 0, :], cd["selv"][r0:r0 + 128, :], sv.tok, writes=[sv.tok])
        k.dma("sp", sv[:, 1, :], cd["sela"][r0:r0 + 128, :], sv.tok, writes=[sv.tok])
        k.op("act", lambda e: e.activation(out=sg[:], in_=gat[:, j, :], func=AF.Exp, scale=-1.0), reads=[gat.tok, sg.tok], writes=[sg.tok])
        k.op("dve", lambda e: e.tensor_scalar_add(sg[:], sg[:], 1.0), reads=[sg.tok], writes=[sg.tok])
        k.op("dve", lambda e: e.reciprocal(sg[:], sg[:]), reads=[sg.tok], writes=[sg.tok])
        sg3 = sg[:].rearrange("p (h x) -> p h x", x=3)
        for g in range(2):
            qr = qT[:, 4 * g:4 * g + 4, tq]

            def unit(kT_ap, ktoks, bias, maskidx, v_ap, vtoks, pv, first, last, extra_rhs=None, pu=None):
                s_ = psum()
                s3 = s_[:, :].rearrange("p (h t) -> p h t", h=4)
                mm(s3, kT_ap, qr, True, bias is None and maskidx is None, ktoks + [qT.tok], [s_.tok])
                if bias is not None:
                    mm(s3, bias, selT[:], False, maskidx is None, [cgrp, selT.tok], [s_.tok])
                if maskidx is not None:
                    mm(s3, identb[:], masks[:, maskidx, :].unsqueeze(1).to_broadcast([128, 4, 128]), False, True,
                       [cgrp, identb.tok], [s_.tok])
                P = PT[ptr[0] % 4]
                ptr[0] += 1
                k.op("act", lambda e: e.activation(out=P[:], in_=s_[:, :], func=AF.Exp), reads=[s_.tok, P.tok], writes=[P.tok])
                for h in range(4):
                    mm(pv[:, h * 65:(h + 1) * 65], P[:, h * 128:(h + 1) * 128], v_ap, first and h == 0, last and h == 3,
                       [P.tok] + vtoks, [pv.tok])
                if pu is not None:
                    for h in range(4):
                        mm(pu[:, h * 64:(h + 1) * 64], P[:, h * 128:(h + 1) * 128], extra_rhs, first and h == 0, last and h == 3,
                           [P.tok, cgrp], [pu.tok])

            pvc, pvu, pvs, pvw = PS[4], PS[5], PS[6], PS[7]
            nmax = min(8 * qt + 6, 254)
            nts = nmax // 128 + 1
            for nt in range(nts):
                dl = qt - 16 * nt
                unit(KcT[:, g, nt * 128:(nt + 1) * 128], [KcT.tok], None, (2 + dl) if dl <= 16 else None,
                     Vc[:, nt, g, :], [Vc.tok], pvc, nt == 0, nt == nts - 1, extra_rhs=ovt[:, nt, :], pu=pvu)
            pvc3 = pvc[:, 0:260].rearrange("p (h d) -> p h d", h=4)
            k.op("dve", lambda e: e.tensor_scalar_max(rin[:, 0, :], pvc3[:, :, 64], 1e-30), reads=[pvc.tok, rin.tok], writes=[rin.tok])
            k.op("dve", lambda e: e.reciprocal(rin[:, 0, :], rin[:, 0, :]), reads=[rin.tok], writes=[rin.tok])
            k.op("dve", lambda e: e.tensor_scalar_mul(sc[:, 0, :], pvu[:, 0:64], rin[:, 0, 0:1]), reads=[pvu.tok, rin.tok, sc.tok], writes=[sc.tok])
            for h in range(1, 4):
                k.op("dve", lambda e, h=h: e.scalar_tensor_tensor(out=sc[:, 0, :], in0=pvu[:, h * 64:(h + 1) * 64], scalar=rin[:, 0, h:h + 1],
                                                                   in1=sc[:, 0, :], op0=ALU.mult, op1=ALU.add),
                     reads=[pvu.tok, rin.tok, sc.tok], writes=[sc.tok])
            k.op("dve", lambda e: e.tensor_mul(sc[:, 1, :], sc[:, 0, :], sv[:, 0, :]), reads=[sc.tok, sv.tok], writes=[sc.tok])
            k.op("dve", lambda e: e.tensor_add(sc[:, 1, :], sc[:, 1, :], sv[:, 1, :]), reads=[sc.tok, sv.tok], writes=[sc.tok])
            k.op("dve", lambda e: e.max(m8[:, 0:8], sc[:, 1, :]), reads=[sc.tok, m8.tok], writes=[m8.tok])
            k.op("dve", lambda e: e.match_replace(sc[:, 2, :], m8[:, 0:8], sc[:, 1, :], -1e9), reads=[sc.tok, m8.tok], writes=[sc.tok])
            k.op("dve", lambda e: e.max(m8[:, 8:16], sc[:, 2, :]), reads=[sc.tok, m8.tok], writes=[m8.tok])
            k.op("dve", lambda e: e.tensor_scalar(sc[:, 3, :], sc[:, 1, :], m8[:, 15:16], None, ALU.is_ge), reads=[sc.tok, m8.tok], writes=[sc.tok])
            k.op("dve", lambda e: e.tensor_scalar(sc[:, 3, :], sc[:, 3, :], -NEG, NEG, ALU.mult, ALU.add), reads=[sc.tok], writes=[sc.tok])
            pst = psum()
            tr(pst[0:64, 0:128], sc[:, 3, :], ident[:], [sc.tok, cgrp], [pst.tok])
            k.op("dve", lambda e: e.tensor_copy(selT[:], pst[0:64, 0:128].unsqueeze(1).to_broadcast([64, 4, 128])),
                 reads=[pst.tok, selT.tok], writes=[selT.tok])
            for kt in range(qt + 1):
                unit(KsT[:, g, kt * 128:(kt + 1) * 128], [KsT.tok], efull[:, kt * 128:(kt + 1) * 128], 0 if kt == qt else None,
                     Vs[:, kt, g, :], [Vs.tok], pvs, kt == 0, kt == qt)
            k0 = max(0, qt - 4)
            for kt in range(k0, qt + 1):
                mi = 0 if kt == qt else (1 if kt == qt - 4 else None)
                unit(KwT[:, g, (kt % 8) * 128:(kt % 8 + 1) * 128], [KwT.tok], None, mi,
                     Vw[:, kt % 8, g, :], [Vw.tok], pvw, kt == k0, kt == qt)
            for x, pv in ((1, pvs), (2, pvw)):
                pv3 = pv[:, 0:260].rearrange("p (h d) -> p h d", h=4)
                k.op("dve", lambda e, x=x, pv3=pv3: e.tensor_scalar_max(rin[:, x, :], pv3[:, :, 64], 1e-30), reads=[pv.tok, rin.tok], writes=[rin.tok])
                k.op("dve", lambda e, x=x: e.reciprocal(rin[:, x, :], rin[:, x, :]), reads=[rin.tok], writes=[rin.tok])
            k.op("dve", lambda e: e.tensor_mul(fac[:], rin[:], sg3[:, 4 * g:4 * g + 4, :].rearrange("p h x -> p x h")),
                 reads=[rin.tok, sg.tok, fac.tok], writes=[fac.tok])
            for x, pv in ((0, pvc), (1, pvs), (2, pvw)):
                pv3 = pv[:, 0:260].rearrange("p (h d) -> p h d", h=4)
                fb = fac[:, x, :].unsqueeze(2).to_broadcast([128, 4, 64])
                if x == 0:
                    k.op("dve", lambda e, pv3=pv3, fb=fb: e.tensor_mul(onsa[:, 4 * g:4 * g + 4, :], pv3[:, :, 0:64], fb),
                         reads=[pv.tok, fac.tok, onsa.tok], writes=[onsa.tok])
                else:
                    k.op("dve", lambda e, pv3=pv3, fb=fb: e.tensor_mul(tmpo[:], pv3[:, :, 0:64], fb),
                         reads=[pv.tok, fac.tok, tmpo.tok], writes=[tmpo.tok])
                    k.op("dve", lambda e: e.tensor_add(onsa[:, 4 * g:4 * g + 4, :], onsa[:, 4 * g:4 * g + 4, :], tmpo[:]),
                         reads=[tmpo.tok, onsa.tok], writes=[onsa.tok])
        of = onsa[:].rearrange("p h d -> p (h d)")
        k.op("act", lambda e: e.activation(out=h2[:], in_=of, func=AF.Square, accum_out=ss4[:, 0:1]),
             reads=[onsa.tok, h2.tok, ss4.tok], writes=[h2.tok, ss4.tok])
        k.op("dve", lambda e: e.tensor_scalar(ss4[:, 4:5], ss4[:, 0:1], 1.0 / 512, EPS, ALU.mult, ALU.add), reads=[ss4.tok], writes=[ss4.tok])
        k.op("act", lambda e: e.activation(out=ss4[:, 4:5], in_=ss4[:, 4:5], func=AF.Ln), reads=[ss4.tok], writes=[ss4.tok])
        k.op("act", lambda e: e.activation(out=ss4[:, 8:9], in_=ss4[:, 4:5], func=AF.Exp, scale=-0.5), reads=[ss4.tok], writes=[ss4.tok])
        k.op("dve", lambda e: e.scalar_tensor_tensor(out=mixin[:, j, 0:512], in0=of, scalar=ss4[:, 8:9], in1=g5[:, 0, :],
                                                      op0=ALU.mult, op1=ALU.mult),
             reads=[onsa.tok, ss4.tok, cgrp, mixin.tok], writes=[mixin.tok])
        p = psum()
        pb_ = p[:].bitcast(BF16)
        for kc in range(8):
            tr(pb_[:, kc * 128:(kc + 1) * 128], mixin[:, j, kc * 128:(kc + 1) * 128], identb[:], [mixin.tok, identb.tok], [p.tok])
        k.op("act", lambda e: e.copy(mxT[:], pb_[:, 0:1024].rearrange("p (c t) -> p c t", c=8)), reads=[p.tok, mxT.tok], writes=[mxT.tok])
        g0 = b * SEQ + r0
        k.dma("sp", mxs[:, :, g0:g0 + 128].rearrange("c p t -> p c t"), mxT[:], mxT.tok, reads=[mxT.tok])

    def stage_A(b):
        k.op("dve", lambda e: e.scalar_tensor_tensor(out=mcol[:, 0, :], in0=aT[:, 8:16, 16 + b], scalar=1.0, in1=gcol[:, 0, :],
                                                      op0=ALU.add, op1=ALU.mult),
             reads=[aT.tok, cgrp, mcol.tok], writes=[mcol.tok])
        k.op("dve", lambda e: e.tensor_copy(mcol[:, 1, :], aT[:, 0:8, 16 + b]), reads=[aT.tok, mcol.tok], writes=[mcol.tok])
        k.op("pool", lambda e: e.memset(Sf[0][:], 0.0), reads=[Sf[0].tok], writes=[Sf[0].tok])
        k.op("pool", lambda e: e.memset(Sb_[0][:], 0.0), reads=[Sb_[0].tok], writes=[Sb_[0].tok])
        k.op("pool", lambda e: e.memset(GT[:], 0.0), reads=[GT.tok], writes=[GT.tok])
        k.op("pool", lambda e: e.memset(KcT[0:64, :, :], 0.0), reads=[KcT.tok], writes=[KcT.tok])
        k.op("pool", lambda e: e.memset(kvcT[:], 0.0), reads=[kvcT.tok], writes=[kvcT.tok])
        KSUB = int(os.environ.get('KSUB', '9'))
        for st in range(int(os.environ.get('KNST', NST))):
            t0 = st * 512
            for j in range(4):
                x = xt[j % 2]
                k.dma("sp", x[:], xp[b, t0 + j * 128:t0 + (j + 1) * 128, :], x.tok, writes=[x.tok])
                k.op("act", lambda e, x=x, j=j: e.activation(out=xn[:], in_=x[:], func=AF.Square, accum_out=st4[:, j:j + 1]),
                     reads=[x.tok, xn.tok, st4.tok], writes=[xn.tok, st4.tok])
                k.op("dve", lambda e, j=j: e.tensor_scalar(st4[:, 4 + j:5 + j], st4[:, j:j + 1], 1.0 / D, EPS, ALU.mult, ALU.add),
                     reads=[st4.tok], writes=[st4.tok])
                k.op("act", lambda e, j=j: e.activation(out=st4[:, 4 + j:5 + j], in_=st4[:, 4 + j:5 + j], func=AF.Ln), reads=[st4.tok], writes=[st4.tok])
                k.op("act", lambda e, j=j: e.activation(out=st4[:, 8 + j:9 + j], in_=st4[:, 4 + j:5 + j], func=AF.Exp, scale=-0.5), reads=[st4.tok], writes=[st4.tok])
                k.op("dve", lambda e, x=x, j=j: e.tensor_scalar_mul(xn[:], x[:], st4[:, 8 + j:9 + j]),
                     reads=[x.tok, st4.tok, xn.tok], writes=[xn.tok])
                p = psum()
                pb_ = p[:].bitcast(BF16)
                for kc in range(8):
                    tr(pb_[:, kc * 128:(kc + 1) * 128], xn[:, kc * 128:(kc + 1) * 128], identb[:], [xn.tok, identb.tok], [p.tok])
                for kc in range(8):
                    k.op("act", lambda e, kc=kc, pb_=pb_, j=j: e.activation(
                        out=hT[:, kc, j * 128:(j + 1) * 128], in_=pb_[:, kc * 128:(kc + 1) * 128], func=AF.Identity,
                        scale=mcol[:, 0, kc:kc + 1], bias=mcol[:, 1, kc:kc + 1]),
                        reads=[p.tok, mcol.tok, hT.tok], writes=[hT.tok])
            if KSUB < 2:
                continue
            k.dma("pool", qT[64:68, :, :], cd["qaug"][:, :, t0:t0 + 512], qT.tok, reads=[qT.tok], writes=[qT.tok])
            wsl = (st % 2) * 512
            for g in range(2):
                k.dma("pool", KwT[64:68, g, wsl:wsl + 512], cd["kaug"][:, t0:t0 + 512], KwT.tok, reads=[KwT.tok], writes=[KwT.tok])
            for h in range(8):
                p = psum()
                for kc in range(8):
                    mm(p[0:64, :], w_in_t[:, kc, h * 64:(h + 1) * 64], hT[:, kc, :], kc == 0, kc == 7, [hT.tok, cgrp], [p.tok])
                evac_scaled(qT[0:64, h, :], p[0:64, :], 0.125, [p.tok, qT.tok], [qT.tok], "act" if h % 2 else "dve")
            k.op("pool", lambda e: e.tensor_copy(kvcT[:, :, 0:16], kvcT[:, :, 512:528]), reads=[kvcT.tok], writes=[kvcT.tok])
            for g in range(2):
                p = psum()
                for kc in range(8):
                    mm(p[:, :], w_in_t[:, kc, 512 + g * 128:512 + (g + 1) * 128], hT[:, kc, :], kc == 0, kc == 7, [hT.tok, cgrp], [p.tok])
                k.op("dve", lambda e, p=p, g=g: e.tensor_copy(kvcT[:, g, 16:528], p[:, :]), reads=[p.tok, kvcT.tok], writes=[kvcT.tok])
                p = psum()
                for kc in range(8):
                    mm(p[0:64, :], w_in_t[:, kc, 768 + g * 128:768 + g * 128 + 64], hT[:, kc, :], kc == 0, kc == 7, [hT.tok, cgrp], [p.tok])
                k.op("act", lambda e, p=p, g=g: e.copy(KsT[0:64, g, t0:t0 + 512], p[0:64, :]), reads=[p.tok, KsT.tok], writes=[KsT.tok])
                p = psum()
                for kc in range(8):
                    mm(p[0:64, :], w_in_t[:, kc, 1024 + g * 128:1024 + g * 128 + 64], hT[:, kc, :], kc == 0, kc == 7, [hT.tok, cgrp], [p.tok])
                k.op("dve", lambda e, p=p, g=g: e.tensor_copy(KwT[0:64, g, wsl:wsl + 512], p[0:64, :]), reads=[p.tok, KwT.tok], writes=[KwT.tok])
            if KSUB < 3:
                continue
            S0i = 0
            for j in range(4):
                tt = st * 4 + j
                ko = kvo[j % 2]
                for (c0, n, dst) in ((512, 512, 0), (1024, 256, 512)):
                    p = psum()
                    for kc in range(8):
                        mm(p[:, 0:n], hT[:, kc, j * 128:(j + 1) * 128], w_in_t[:, kc, c0:c0 + n], kc == 0, kc == 7, [hT.tok, cgrp], [p.tok])
                    k.op("dve", lambda e, p=p, n=n, dst=dst, ko=ko: e.tensor_copy(ko[:, dst:dst + n], p[:, 0:n]),
                         reads=[p.tok, ko.tok], writes=[ko.tok])
                r0 = t0 + j * 128
                k.dma("sp", pcmp[b, r0:r0 + 128, :], ko[:, 0:256], ko.tok, reads=[ko.tok])
                k.dma("sp", pslc[b, r0:r0 + 128, :], ko[:, 256:512], ko.tok, reads=[ko.tok])
                if st == NST - 1:
                    k.dma("sp", pwin[b, j * 128:(j + 1) * 128, :], ko[:, 512:768], ko.tok, reads=[ko.tok])
                k.op("act", lambda e, ko=ko, tt=tt: e.copy(Vs[:, tt, :, 0:64], ko[:, 256:512].rearrange("p (g k d) -> p g k d", g=2, k=2)[:, :, 1, :]),
                     reads=[ko.tok, Vs.tok], writes=[Vs.tok])
                k.op("act", lambda e, ko=ko, tt=tt: e.copy(Vw[:, tt % 8, :, 0:64], ko[:, 512:768].rearrange("p (g k d) -> p g k d", g=2, k=2)[:, :, 1, :]),
                     reads=[ko.tok, Vw.tok], writes=[Vw.tok])
                p = psum()
                for kc in range(8):
                    mm(p[:, 0:24], hT[:, kc, j * 128:(j + 1) * 128], w_in_t[:, kc, 1280:1304], kc == 0, kc == 7, [hT.tok, cgrp], [p.tok])
                k.op("dve", lambda e, p=p, j=j: e.tensor_copy(gat[:, j, :], p[:, 0:24]), reads=[p.tok, gat.tok], writes=[gat.tok])
                if KSUB >= 4:
                    hgrn_tile(b, st, j, 0)
            if KSUB < 5:
                continue
            n0 = 32 * st - 1 if st > 0 else 0
            n1 = 32 * st + 31
            nn = n1 - n0
            cbase = 0 if st > 0 else 16
            for g in range(2):
                p = psum()
                for pp in range(32):
                    mm(p[:, 0:nn], wphi1t[:, pp, :], kvcT[:, g, cbase + pp:cbase + pp + 16 * (nn - 1) + 1:16], pp == 0, pp == 31,
                       [kvcT.tok, cgrp], [p.tok])
                k.op("act", lambda e, p=p: e.activation(out=GTn[:, 0:nn], in_=p[:, 0:nn], func=AF.Gelu_apprx_tanh, bias=bphi1t[:, 0:1]),
                     reads=[p.tok, cgrp, GTn.tok], writes=[GTn.tok])
                k.op("dve", lambda e, g=g: e.tensor_copy(GT[:, g, n0:n1], GTn[:, 0:nn]), reads=[GTn.tok, GT.tok], writes=[GT.tok])
                p2 = psum()
                mm(p2[0:64, 0:nn], wphi2t[0:64, :], GTn[0:64, 0:nn], True, True, [GTn.tok, cgrp], [p2.tok])
                k.op("dve", lambda e, p2=p2, g=g: e.tensor_copy(KcT[0:64, g, n0:n1], p2[0:64, 0:nn]), reads=[p2.tok, KcT.tok], writes=[KcT.tok])
                for nt in range(2):
                    p3 = psum()
                    mm(p3[:, 0:64], GT[64:128, g, nt * 128:(nt + 1) * 128], wphi2t[64:128, :], True, True, [GT.tok, cgrp], [p3.tok])
                    k.op("dve", lambda e, p3=p3, g=g, nt=nt: e.tensor_copy(Vc[:, nt, g, 0:64], p3[:, 0:64]), reads=[p3.tok, Vc.tok], writes=[Vc.tok])
            if stage >= 2:
                for j in range(4):
                    attention_tile(b, st, j)
        k.dma("sp", phg[b].rearrange("h k v -> k h v"), Sf[0][:], Sf[0].tok, reads=[Sf[0].tok])

    if stage >= 1:
        for b in range(NB):
            stage_A(b)
    k.barrier()
    esA.close()

    esS = ExitStack()
    cgS = k.tok()
    NPG = int(os.environ.get("KNPG", NPAGE))
    NSMP = int(os.environ.get("KNSMP", NS))
    if stage >= 4:
        xs_d = din("xs", [NS, D])
        poolc = din("poolc", [NPOOL * 128, 256])
        pools = din("pools", [NPOOL * 128, 256])
        winb = din("winb", [NS, 512, 256])
        sth = din("sth", [NS, 4, 128, 128])
        ptab = din("ptab", [1, NS * NPAGE], I32)
        ys = dout("ys", [NS, D])
        scmp = dout("scmp", [NS, 256])
        sslc = dout("sslc", [NS, 256])
        swin = dout("swin", [NS, 512, 256])
        shg = dout("shg", [NS, 4, 128, 128])
        efS = sb("efS", [128, PAST], BF16, es=esS)
        pgw = sb("pgw", [128, NPAGE, 8], es=esS)
        ovS = sb("ovS", [128, 4, 128], BF16, es=esS)
        g4 = sb("g4", [128, 32], es=esS)
        eye16 = sb("eye16", [128, 16, 16], es=esS)
        eyem = sb("eyem", [16, 4, 16], es=esS)
        selS = sb("selS", [16, 16, 128], es=esS)
        vm = sb("vm", [128, 8], es=esS)
        sadd = sb("sadd", [32, 128], es=esS)
        iop = sb("iop", [128, 1], es=esS)
        masksS = sb("masksS", [128, 2, 64], es=esS)
        wphi1s = sb("wphi1s", [128, 32, 128], BF16, es=esS)
        wphi2s = sb("wphi2s", [128, 64], BF16, es=esS)
        bphi1s = sb("bphi1s", [128, 1], es=esS)
        g5s = sb("g5s", [128, 4, 512], es=esS)
        omls = sb("omls", [16, 512], es=esS)
        for t_, n_ in ((pgw, "pgw"), (g4, "g4"), (eye16, "eye16"), (eyem, "eyem"), (selS, "selE"), (vm, "vm"), (sadd, "sadd"), (iop, "iop")):
            k.dma("sp", t_[:], cd[n_], cgS, writes=[cgS])
        k.dma("sp", bphi1s[:], bphi1, cgS, writes=[cgS])
        k.dma("sp", g5s[:], g512, cgS, writes=[cgS])
        k.dma("pool", efS[:], cd["efS"], cgS, writes=[cgS])
        k.dma("pool", ovS[:], cd["ovS"], cgS, writes=[cgS])
        k.dma("pool", wphi1s[:], wphi1, cgS, writes=[cgS])
        k.dma("pool", wphi2s[:], wphi2, cgS, writes=[cgS])
        k.op("dve", lambda e: e.tensor_sub(omls[:], g5s[0:16, 2, :], g5s[0:16, 3, :]), reads=[cgS], writes=[omls.tok])
        k.op("act", lambda e: e.activation(out=omls[:], in_=omls[:], func=AF.Exp), reads=[omls.tok], writes=[omls.tok])
        k.op("dve", lambda e: e.tensor_scalar_add(omls[:], omls[:], 1.0), reads=[omls.tok], writes=[omls.tok])
        k.op("dve", lambda e: e.reciprocal(omls[:], omls[:]), reads=[omls.tok], writes=[omls.tok])
        pti = sb("pti", [128, NS * NPAGE], I32, es=esS)
        ptf = sb("ptf", [128, NS * NPAGE], es=esS)
        idx = sb("idx", [128, NS * NPAGE], I32, es=esS)
        k.dma("sp", pti[:], ptab.partition_broadcast(128), pti.tok, writes=[pti.tok])
        k.op("dve", lambda e: e.tensor_copy(ptf[:], pti[:]), reads=[pti.tok], writes=[ptf.tok])
        k.op("dve", lambda e: e.tensor_scalar(ptf[:], ptf[:], 128.0, iop[:, 0:1], ALU.mult, ALU.add), reads=[ptf.tok, cgS], writes=[ptf.tok])
        k.op("dve", lambda e: e.tensor_copy(idx[:], ptf[:]), reads=[ptf.tok], writes=[idx.tok])
        xst = sb("xst", [16, D], es=esS)
        smod = [sb("smod%d" % i, [16, D], es=esS) for i in range(2)]
        hsf = sb("hsf", [16, D], es=esS)
        hsT = sb("hsT", [128, 8, 16], es=esS)
        projs = sb("projs", [16, INC], es=esS)
        sst = sb("sst", [128, 16], es=esS)
        wst = [sb("wst%d" % i, [128, 8, 512], es=esS) for i in range(2)]
        k.dma("sp", xst[:], xs_d, xst.tok, writes=[xst.tok])
        k.dma("sp", smod[0][:], sms[0], smod[0].tok, writes=[smod[0].tok])
        k.dma("sp", smod[1][:], sms[1], smod[1].tok, writes=[smod[1].tok])
        k.op("act", lambda e: e.activation(out=hsf[:], in_=xst[:], func=AF.Square, accum_out=sst[0:16, 0:1]), reads=[xst.tok], writes=[hsf.tok, sst.tok])
        k.op("dve", lambda e: e.tensor_scalar(sst[0:16, 1:2], sst[0:16, 0:1], 1.0 / D, EPS, ALU.mult, ALU.add), reads=[sst.tok], writes=[sst.tok])
        k.op("act", lambda e: e.activation(out=sst[0:16, 1:2], in_=sst[0:16, 1:2], func=AF.Ln), reads=[sst.tok], writes=[sst.tok])
        k.op("act", lambda e: e.activation(out=sst[0:16, 2:3], in_=sst[0:16, 1:2], func=AF.Exp, scale=-0.5), reads=[sst.tok], writes=[sst.tok])
        k.op("dve", lambda e: e.scalar_tensor_tensor(out=hsf[:], in0=xst[:], scalar=sst[0:16, 2:3], in1=smod[0][:], op0=ALU.mult, op1=ALU.mult),
             reads=[xst.tok, sst.tok, smod[0].tok, hsf.tok], writes=[hsf.tok])
        k.op("dve", lambda e: e.tensor_add(hsf[:], hsf[:], smod[1][:]), reads=[hsf.tok, smod[1].tok], writes=[hsf.tok])
        for kc in range(8):
            p = psum()
            tr(p[:, 0:16], hsf[:, kc * 128:(kc + 1) * 128], ident[0:16, 0:16], [hsf.tok, cgrp], [p.tok])
            k.op("dve", lambda e, kc=kc, p=p: e.tensor_copy(hsT[:, kc, :], p[:, 0:16]), reads=[p.tok, hsT.tok], writes=[hsT.tok])
        c0 = 0
        ci = 0
        while c0 < INC:
            n = min(512, INC - c0)
            wt = wst[ci % 2]
            k.dma("sp", wt[:, :, 0:n], w_in[:, c0:c0 + n].rearrange("(c p) n -> p c n", p=128), wt.tok, reads=[wt.tok], writes=[wt.tok])
            p = psum()
            for kc in range(8):
                mm(p[0:16, 0:n], hsT[:, kc, :], wt[:, kc, 0:n], kc == 0, kc == 7, [hsT.tok, wt.tok], [p.tok])
            k.op("dve", lambda e, p=p, c0=c0, n=n: e.tensor_copy(projs[:, c0:c0 + n], p[0:16, 0:n]), reads=[p.tok, projs.tok], writes=[projs.tok])
            c0 += n
            ci += 1
        k.dma("sp", scmp, projs[:, 512:768], projs.tok, reads=[projs.tok])
        k.dma("sp", sslc, projs[:, 768:1024], projs.tok, reads=[projs.tok])
        k.dma("sp", swin[:, 511, :], projs[:, 1024:1280], projs.tok, reads=[projs.tok])
        for s in range(NS):
            k.dma("sp", swin[s, 0:511, :], winb[s, 1:512, :], projs.tok)
        qsT = sb("qsT", [66, 8, 16], BF16, es=esS)
        KnT = sb("KnT", [64, 2, 2, 16], BF16, es=esS)
        Vn = sb("Vn", [16, 2, 2, 65], BF16, es=esS)
        k.dma("pool", qsT[64:66, :, :], cd["qaugS"], qsT.tok, writes=[qsT.tok])
        for h in range(8):
            p = psum()
            tr(p[0:64, 0:16], projs[:, h * 64:(h + 1) * 64], ident[0:16, 0:16], [projs.tok, cgrp], [p.tok])
            k.op("act", lambda e, h=h, p=p: e.activation(out=qsT[0:64, h, :], in_=p[0:64, 0:16], func=AF.Copy, scale=0.125),
                 reads=[p.tok, qsT.tok], writes=[qsT.tok])
        for br in range(2):
            for g in range(2):
                cb_ = 768 + br * 256 + g * 128
                p = psum()
                tr(p[0:64, 0:16], projs[:, cb_:cb_ + 64], ident[0:16, 0:16], [projs.tok, cgrp], [p.tok])
                k.op("dve", lambda e, br=br, g=g, p=p: e.tensor_copy(KnT[:, br, g, :], p[0:64, 0:16]), reads=[p.tok, KnT.tok], writes=[KnT.tok])
                k.op("act", lambda e, br=br, g=g, cb_=cb_: e.copy(Vn[:, br, g, 0:64], projs[:, cb_ + 64:cb_ + 128]), reads=[projs.tok, Vn.tok], writes=[Vn.tok])
        k.op("pool", lambda e: e.memset(Vn[:, :, :, 64:65], 1.0), reads=[Vn.tok], writes=[Vn.tok])
        pvcT, puT, pvsT, pvwT = PS[4], PS[5], PS[6], PS[7]
        bank_first = {4: True, 5: True, 6: True, 7: True}
        for bi_ in range(4, 8):
            k.op("dve", lambda e, bi_=bi_: e.memset(PS[bi_][:, :], 0.0), reads=[PS[bi_].tok], writes=[PS[bi_].tok])

        def acc(bi, out_ap, lhsT, rhs, reads):
            mm(out_ap, lhsT, rhs, bank_first[bi], False, reads, [PS[bi].tok], skip=True)
            bank_first[bi] = False

        def cols(g, s):
            return slice(g * 64 + s, g * 64 + s + 49, 16)

        pgc = [sb("pgc%d" % i, [128, 256], es=esS) for i in range(3)]
        kvcS = sb("kvcS", [128, 2, 16 + 1024], BF16, es=esS)
        GTs = sb("GTs", [128, 2, 512], BF16, es=esS)
        KcTs = sb("KcTs", [66, 2, 512], BF16, es=esS)
        Vcs = sb("Vcs", [128, 4, 2, 65], BF16, es=esS)
        GTn2 = sb("GTn2", [128, 64], BF16, es=esS)
        Pc = [sb("Pc%d" % i, [128, 4], BF16, es=esS) for i in range(4)]
        pci = [0]
        k.dma("pool", KcTs[64:66, 0, :], cd["caugS"], KcTs.tok, writes=[KcTs.tok])
        k.dma("pool", KcTs[64:66, 1, :], cd["caugS"], KcTs.tok, writes=[KcTs.tok])
        k.op("pool", lambda e: e.memset(Vcs[:, :, :, 64:65], 1.0), reads=[Vcs.tok], writes=[Vcs.tok])
        for s in range(NSMP):
            k.op("pool", lambda e: e.memset(GTs[:], 0.0), reads=[GTs.tok], writes=[GTs.tok])
            k.op("pool", lambda e: e.memset(KcTs[0:64, :, :], 0.0), reads=[KcTs.tok], writes=[KcTs.tok])
            k.op("pool", lambda e: e.memset(kvcS[:], 0.0), reads=[kvcS.tok], writes=[kvcS.tok])
            for pg in range(NPG):
                pt_ = pgc[pg % 3]
                c_ = s * NPAGE + pg
                k.idma(pt_[:], poolc, idx[:, c_:c_ + 1], pt_.tok, reads=[idx.tok, pt_.tok], writes=[pt_.tok])
                pl = pg % 8
                for g in range(2):
                    p = psum()
                    tr(p[:, 0:128], pt_[:, g * 128:(g + 1) * 128], ident[:], [pt_.tok, cgrp], [p.tok])
                    if g == 0:
                        k.op("act", lambda e, p=p, g=g, pl=pl: e.copy(kvcS[:, g, 16 + pl * 128:16 + (pl + 1) * 128], p[:, 0:128]), reads=[p.tok, kvcS.tok], writes=[kvcS.tok])
                    else:
                        k.op("dve", lambda e, p=p, g=g, pl=pl: e.tensor_copy(kvcS[:, g, 16 + pl * 128:16 + (pl + 1) * 128], p[:, 0:128]), reads=[p.tok, kvcS.tok], writes=[kvcS.tok])
                if pl == 7 or pg == NPG - 1:
                    pgp = pg // 8
                    n0 = 64 * pgp - 1 if pgp > 0 else 0
                    n1 = 8 * (pg + 1) - 1
                    nn = n1 - n0
                    cbase = 0 if pgp > 0 else 16
                    for g in range(2):
                        p = psum()
                        for pp in range(32):
                            mm(p[:, 0:nn], wphi1s[:, pp, :], kvcS[:, g, cbase + pp:cbase + pp + 16 * (nn - 1) + 1:16], pp == 0, pp == 31, [kvcS.tok, cgS], [p.tok])
                        k.op("act", lambda e, p=p, nn=nn: e.activation(out=GTn2[:, 0:nn], in_=p[:, 0:nn], func=AF.Gelu_apprx_tanh, bias=bphi1s[:, 0:1]),
                             reads=[p.tok, cgS, GTn2.tok], writes=[GTn2.tok])
                        k.op("dve", lambda e, g=g, n0=n0, n1=n1, nn=nn: e.tensor_copy(GTs[:, g, n0:n1], GTn2[:, 0:nn]), reads=[GTn2.tok, GTs.tok], writes=[GTs.tok])
                        p2 = psum()
                        mm(p2[0:64, 0:nn], wphi2s[0:64, :], GTn2[0:64, 0:nn], True, True, [GTn2.tok, cgS], [p2.tok])
                        k.op("dve", lambda e, p2=p2, g=g, n0=n0, n1=n1, nn=nn: e.tensor_copy(KcTs[0:64, g, n0:n1], p2[0:64, 0:nn]), reads=[p2.tok, KcTs.tok], writes=[KcTs.tok])
                    k.op("pool", lambda e: e.tensor_copy(kvcS[:, :, 0:16], kvcS[:, :, 1024:1040]), reads=[kvcS.tok], writes=[kvcS.tok])
            ntl = (8 * NPG - 1 + 127) // 128
            for g in range(2):
                for nt in range(ntl):
                    p3 = psum()
                    mm(p3[:, 0:64], GTs[64:128, g, nt * 128:(nt + 1) * 128], wphi2s[64:128, :], True, True, [GTs.tok, cgS], [p3.tok])
                    k.op("dve", lambda e, p3=p3, g=g, nt=nt: e.tensor_copy(Vcs[:, nt, g, 0:64], p3[:, 0:64]), reads=[p3.tok, Vcs.tok], writes=[Vcs.tok])
                for nt in range(ntl):
                    s_ = psum()
                    mm(s_[:, 0:4], KcTs[:, g, nt * 128:(nt + 1) * 128], qsT[:, 4 * g:4 * g + 4, s], True, True, [KcTs.tok, qsT.tok], [s_.tok])
                    P = Pc[pci[0] % 4]
                    pci[0] += 1
                    k.op("act", lambda e, s_=s_, P=P: e.activation(out=P[:], in_=s_[:, 0:4], func=AF.Exp), reads=[s_.tok, P.tok], writes=[P.tok])
                    k.op("dve", lambda e, P=P, nt=nt: e.tensor_scalar_mul(P[:], P[:], vm[:, nt:nt + 1]), reads=[P.tok, cgS], writes=[P.tok])
                    acc(4, pvcT[0:65, cols(g, s)], Vcs[:, nt, g, :], P[:], [Vcs.tok, P.tok])
                    acc(5, puT[:, cols(g, s)], ovS[:, nt, :], P[:], [cgS, P.tok])
        cT = sb("cT_s", [65, 128], es=esS)
        uT = sb("uT_s", [128, 128], es=esS)
        cTt = sb("cTt", [128, 65], es=esS)
        uTt = sb("uTt", [128, 128], es=esS)
        scS = sb("scS", [32, 4, 128], es=esS)
        m8s = sb("m8s", [32, 16], es=esS)
        selTj = sb("selTj", [128, 32], BF16, es=esS)
        SEL2 = sb("SEL2", [128, NPAGE, 32], es=esS)
        rns = sb("rns", [128, 2], es=esS)
        k.op("act", lambda e: e.copy(cT[:], pvcT[0:65, 0:128]), reads=[pvcT.tok], writes=[cT.tok])
        k.op("dve", lambda e: e.tensor_copy(uT[:], puT[:, 0:128]), reads=[puT.tok], writes=[uT.tok])
        p = psum()
        tr(p[:, 0:65], cT[:], ident[0:65, 0:65], [cT.tok, cgrp], [p.tok])
        k.op("dve", lambda e: e.tensor_copy(cTt[:], p[:, 0:65]), reads=[p.tok], writes=[cTt.tok])
        p = psum()
        tr(p[:, 0:128], uT[:], ident[:], [uT.tok, cgrp], [p.tok])
        k.op("dve", lambda e: e.tensor_scalar_max(rns[:, 0:1], cTt[:, 64:65], 1e-30), reads=[cTt.tok], writes=[rns.tok])
        k.op("dve", lambda e: e.reciprocal(rns[:, 1:2], rns[:, 0:1]), reads=[rns.tok], writes=[rns.tok])
        k.op("dve", lambda e: e.tensor_scalar_mul(uTt[:], p[:, 0:128], rns[:, 1:2]), reads=[p.tok, rns.tok], writes=[uTt.tok])
        p = psum()
        mm(p[0:32, 0:128], g4[:], uTt[:], True, True, [uTt.tok, cgS], [p.tok])
        k.op("dve", lambda e: e.tensor_add(scS[:, 0, :], p[0:32, 0:128], sadd[:]), reads=[p.tok, cgS], writes=[scS.tok])
        k.op("dve", lambda e: e.max(m8s[:, 0:8], scS[:, 0, :]), reads=[scS.tok], writes=[m8s.tok])
        k.op("dve", lambda e: e.match_replace(scS[:, 1, :], m8s[:, 0:8], scS[:, 0, :], -1e9), reads=[scS.tok, m8s.tok], writes=[scS.tok])
        k.op("dve", lambda e: e.max(m8s[:, 8:16], scS[:, 1, :]), reads=[scS.tok, m8s.tok], writes=[m8s.tok])
        k.op("dve", lambda e: e.tensor_scalar(scS[:, 2, :], scS[:, 0, :], m8s[:, 14:15], None, ALU.is_ge), reads=[scS.tok, m8s.tok], writes=[scS.tok])
        p = psum()
        tr(p[:, 0:32], scS[:, 2, :], ident[0:32, 0:32], [scS.tok, cgrp], [p.tok])
        k.op("dve", lambda e: e.tensor_copy(selTj[:], p[:, 0:32]), reads=[p.tok], writes=[selTj.tok])
        for pg in range(NPG):
            p = psum()
            mm(p[:, 0:32], efS[:, pg * 128:(pg + 1) * 128], selTj[:], True, True, [selTj.tok, cgS], [p.tok])
            k.op("act" if pg % 2 else "dve", (lambda e, p=p, pg=pg: e.copy(SEL2[:, pg, :], p[:, 0:32])) if pg % 2 else
                 (lambda e, p=p, pg=pg: e.tensor_copy(SEL2[:, pg, :], p[:, 0:32])), reads=[p.tok, SEL2.tok], writes=[SEL2.tok])
        KpT = [sb("KpT%d" % i, [66, 2, 128], BF16, es=esS) for i in range(2)]
        Vp = [sb("Vp%d" % i, [128, 2, 65], BF16, es=esS) for i in range(2)]
        for i in range(2):
            k.dma("pool", KpT[i][64:66, 0, :], cd["paugS"], KpT[i].tok, writes=[KpT[i].tok])
            k.dma("pool", KpT[i][64:66, 1, :], cd["paugS"], KpT[i].tok, writes=[KpT[i].tok])
            k.op("pool", lambda e, i=i: e.memset(Vp[i][:, :, 64:65], 1.0), reads=[Vp[i].tok], writes=[Vp[i].tok])
        it = 0
        for s in range(NSMP):
            for pg in range(NPG):
                pt_ = pgc[it % 3]
                kp = KpT[it % 2]
                vp = Vp[it % 2]
                it += 1
                c_ = s * NPAGE + pg
                k.idma(pt_[:], pools, idx[:, c_:c_ + 1], pt_.tok, reads=[idx.tok, pt_.tok], writes=[pt_.tok])
                for g in range(2):
                    p = psum()
                    tr(p[0:64, 0:128], pt_[:, g * 128:g * 128 + 64], ident[:], [pt_.tok, cgrp], [p.tok])
                    k.op("act", lambda e, p=p, g=g, kp=kp: e.copy(kp[0:64, g, :], p[0:64, 0:128]), reads=[p.tok, kp.tok], writes=[kp.tok])
                k.op("dve", lambda e, pt_=pt_, vp=vp: e.tensor_copy(vp[:, :, 0:64], pt_[:].rearrange("p (g k d) -> p g k d", g=2, k=2)[:, :, 1, :]),
                     reads=[pt_.tok, vp.tok], writes=[vp.tok])
                for g in range(2):
                    s_ = psum()
                    mm(s_[:, 0:4], kp[:, g, :], qsT[:, 4 * g:4 * g + 4, s], True, True, [kp.tok, qsT.tok], [s_.tok])
                    P = Pc[pci[0] % 4]
                    pci[0] += 1
                    k.op("act", lambda e, s_=s_, P=P: e.activation(out=P[:], in_=s_[:, 0:4], func=AF.Exp), reads=[s_.tok, P.tok], writes=[P.tok])
                    k.op("dve", lambda e, P=P, pg=pg, g=g, s=s: e.scalar_tensor_tensor(out=P[:], in0=P[:], scalar=SEL2[:, pg, 2 * s + g:2 * s + g + 1],
                                                                                       in1=pgw[:, pg, 4 * g:4 * g + 4], op0=ALU.mult, op1=ALU.mult),
                         reads=[P.tok, SEL2.tok, cgS], writes=[P.tok])
                    acc(6, pvsT[0:65, cols(g, s)], vp[:, g, :], P[:], [vp.tok, P.tok])
        wbt = sb("wbt", [128, 4, 256], es=esS)
        KwTs = sb("KwTs", [66, 2, 512], BF16, es=esS)
        Vws = sb("Vws", [128, 4, 2, 65], BF16, es=esS)
        k.dma("pool", KwTs[64:66, 0, :], cd["waugS"], KwTs.tok, writes=[KwTs.tok])
        k.dma("pool", KwTs[64:66, 1, :], cd["waugS"], KwTs.tok, writes=[KwTs.tok])
        k.op("pool", lambda e: e.memset(Vws[:, :, :, 64:65], 1.0), reads=[Vws.tok], writes=[Vws.tok])
        for s in range(NSMP):
            k.dma("sp", wbt[:], winb[s].rearrange("(t p) c -> p t c", p=128), wbt.tok, reads=[wbt.tok], writes=[wbt.tok])
            for t in range(4):
                for g in range(2):
                    p = psum()
                    tr(p[0:64, 0:128], wbt[:, t, g * 128:g * 128 + 64], ident[:], [wbt.tok, cgrp], [p.tok])
                    k.op("act", lambda e, p=p, g=g, t=t: e.copy(KwTs[0:64, g, t * 128:(t + 1) * 128], p[0:64, 0:128]), reads=[p.tok, KwTs.tok], writes=[KwTs.tok])
                k.op("dve", lambda e, t=t: e.tensor_copy(Vws[:, t, :, 0:64], wbt[:, t, :].rearrange("p (g k d) -> p g k d", g=2, k=2)[:, :, 1, :]),
                     reads=[wbt.tok, Vws.tok], writes=[Vws.tok])
            for g in range(2):
                for t in range(4):
                    s_ = psum()
                    mm(s_[:, 0:4], KwTs[:, g, t * 128:(t + 1) * 128], qsT[:, 4 * g:4 * g + 4, s], True, True, [KwTs.tok, qsT.tok], [s_.tok])
                    P = Pc[pci[0] % 4]
                    pci[0] += 1
                    k.op("act", lambda e, s_=s_, P=P: e.activation(out=P[:], in_=s_[:, 0:4], func=AF.Exp), reads=[s_.tok, P.tok], writes=[P.tok])
                    if t == 0:
                        k.op("dve", lambda e, P=P: e.tensor_scalar_mul(P[:], P[:], vm[:, 4:5]), reads=[P.tok, cgS], writes=[P.tok])
                    acc(7, pvwT[0:65, cols(g, s)], Vws[:, t, g, :], P[:], [Vws.tok, P.tok])
        Pn = sb("Pn", [16, 4, 16], BF16, es=esS)
        for br, bi, bank in ((0, 6, pvsT), (1, 7, pvwT)):
            for g in range(2):
                s_ = psum()
                mm(s_[0:16, 0:64], KnT[:, br, g, :], qsT[0:64, 4 * g:4 * g + 4, :], True, True, [KnT.tok, qsT.tok], [s_.tok])
                k.op("act", lambda e, s_=s_: e.activation(out=Pn[:], in_=s_[0:16, 0:64].rearrange("p (h s) -> p h s", h=4), func=AF.Exp),
                     reads=[s_.tok, Pn.tok], writes=[Pn.tok])
                k.op("dve", lambda e: e.tensor_mul(Pn[:], Pn[:], eyem[:]), reads=[Pn.tok, cgS], writes=[Pn.tok])
                acc(bi, bank[0:65, g * 64:(g + 1) * 64], Vn[:, br, g, :], Pn[:].rearrange("p h s -> p (h s)"), [Vn.tok, Pn.tok])
        pvt = sb("pvt", [16, 3, 8, 65], es=esS)
        xT = sb("xT_s", [65, 128], es=esS)
        for x, bank in ((0, pvcT), (1, pvsT), (2, pvwT)):
            if x == 0:
                src = cT
            else:
                k.op("act", lambda e, bank=bank: e.copy(xT[:], bank[0:65, 0:128]), reads=[bank.tok, xT.tok], writes=[xT.tok])
                src = xT
            for hh in range(2):
                p = psum()
                for h4 in range(4):
                    h8 = hh * 4 + h4
                    tr(p[0:16, h4 * 65:(h4 + 1) * 65], src[:, h8 * 16:(h8 + 1) * 16], ident[0:65, 0:65], [src.tok, cgrp], [p.tok])
                k.op("dve", lambda e, x=x, p=p, hh=hh: e.tensor_copy(pvt[:, x, hh * 4:hh * 4 + 4, :], p[0:16, 0:260].rearrange("p (h d) -> p h d", h=4)),
                     reads=[p.tok, pvt.tok], writes=[pvt.tok])
        sgs = sb("sgs", [16, 24], es=esS)
        rinS = sb("rinS", [16, 3, 8], es=esS)
        facS = sb("facS", [16, 3, 8], es=esS)
        onS = sb("onS", [16, 8, 64], es=esS)
        tmS = sb("tmS", [16, 8, 64], es=esS)
        mixS = sb("mixS", [16, D], es=esS)
        k.op("act", lambda e: e.activation(out=sgs[:], in_=projs[:, 1280:1304], func=AF.Exp, scale=-1.0), reads=[projs.tok], writes=[sgs.tok])
        k.op("dve", lambda e: e.tensor_scalar_add(sgs[:], sgs[:], 1.0), reads=[sgs.tok], writes=[sgs.tok])
        k.op("dve", lambda e: e.reciprocal(sgs[:], sgs[:]), reads=[sgs.tok], writes=[sgs.tok])
        k.op("dve", lambda e: e.tensor_scalar_max(rinS[:], pvt[:, :, :, 64], 1e-30), reads=[pvt.tok], writes=[rinS.tok])
        k.op("dve", lambda e: e.reciprocal(rinS[:], rinS[:]), reads=[rinS.tok], writes=[rinS.tok])
        k.op("dve", lambda e: e.tensor_mul(facS[:], rinS[:], sgs[:].rearrange("p (h x) -> p x h", x=3)), reads=[rinS.tok, sgs.tok], writes=[facS.tok])
        for x in range(3):
            fb = facS[:, x, :].unsqueeze(2).to_broadcast([16, 8, 64])
            if x == 0:
                k.op("dve", lambda e, fb=fb: e.tensor_mul(onS[:], pvt[:, 0, :, 0:64], fb), reads=[pvt.tok, facS.tok], writes=[onS.tok])
            else:
                k.op("dve", lambda e, fb=fb, x=x: e.tensor_mul(tmS[:], pvt[:, x, :, 0:64], fb), reads=[pvt.tok, facS.tok, tmS.tok], writes=[tmS.tok])
                k.op("dve", lambda e: e.tensor_add(onS[:], onS[:], tmS[:]), reads=[onS.tok, tmS.tok], writes=[onS.tok])
        onf = onS[:].rearrange("p h d -> p (h d)")
        k.op("act", lambda e: e.activation(out=tmS[:].rearrange("p h d -> p (h d)"), in_=onf, func=AF.Square, accum_out=sst[0:16, 4:5]),
             reads=[onS.tok, tmS.tok, sst.tok], writes=[tmS.tok, sst.tok])
        k.op("dve", lambda e: e.tensor_scalar(sst[0:16, 5:6], sst[0:16, 4:5], 1.0 / 512, EPS, ALU.mult, ALU.add), reads=[sst.tok], writes=[sst.tok])
        k.op("act", lambda e: e.activation(out=sst[0:16, 5:6], in_=sst[0:16, 5:6], func=AF.Ln), reads=[sst.tok], writes=[sst.tok])
        k.op("act", lambda e: e.activation(out=sst[0:16, 6:7], in_=sst[0:16, 5:6], func=AF.Exp, scale=-0.5), reads=[sst.tok], writes=[sst.tok])
        k.op("dve", lambda e: e.scalar_tensor_tensor(out=mixS[:, 0:512], in0=onf, scalar=sst[0:16, 6:7], in1=g5s[0:16, 0, :], op0=ALU.mult, op1=ALU.mult),
             reads=[onS.tok, sst.tok, cgS, mixS.tok], writes=[mixS.tok])
        e1 = sb("e1s", [16, 512], es=esS)
        qs_ = sb("qs_", [16, 512], es=esS)
        ks_ = sb("ks_", [16, 512], es=esS)
        fs_ = sb("fs_", [16, 512], es=esS)
        sos = sb("sos", [16, 512], es=esS)
        colT = sb("colT", [128, 3, 4, 16], es=esS)
        qTm = sb("qTm", [128, 4, 16, 16], es=esS)
        Sst = [sb("Sst%d" % i, [128, 4, 128], es=esS) for i in range(2)]
        Snw = [sb("Snw%d" % i, [128, 4, 128], es=esS) for i in range(2)]
        tmv = sb("tmv", [128, 128], es=esS)
        HQ, HF, HI, HO = 1304, 1816, 2328, 2840

        def sigm(dst, c0):
            k.op("act", lambda e: e.activation(out=e1[:], in_=projs[:, c0:c0 + 512], func=AF.Exp, scale=-1.0), reads=[projs.tok, e1.tok], writes=[e1.tok])
            k.op("dve", lambda e: e.tensor_scalar_add(dst[:], e1[:], 1.0), reads=[e1.tok, dst.tok], writes=[dst.tok])
            k.op("dve", lambda e: e.reciprocal(dst[:], dst[:]), reads=[dst.tok], writes=[dst.tok])
        sigm(qs_, HQ)
        k.op("dve", lambda e: e.tensor_mul(qs_[:], qs_[:], projs[:, HQ:HQ + 512]), reads=[qs_.tok, projs.tok], writes=[qs_.tok])
        sigm(ks_, HF)
        k.op("dve", lambda e: e.tensor_mul(ks_[:], ks_[:], e1[:]), reads=[ks_.tok, e1.tok], writes=[ks_.tok])
        k.op("dve", lambda e: e.tensor_mul(ks_[:], ks_[:], omls[:]), reads=[ks_.tok, omls.tok], writes=[ks_.tok])
        k.op("dve", lambda e: e.tensor_scalar(fs_[:], ks_[:], -1.0, 1.0, ALU.mult, ALU.add), reads=[ks_.tok, fs_.tok], writes=[fs_.tok])
        sigm(sos, HO)
        k.op("dve", lambda e: e.tensor_mul(sos[:], sos[:], projs[:, HO:HO + 512]), reads=[sos.tok, projs.tok], writes=[sos.tok])
        for qi, src in enumerate((qs_, ks_, fs_)):
            for h in range(4):
                p = psum()
                tr(p[:, 0:16], src[:, h * 128:(h + 1) * 128], ident[0:16, 0:16], [src.tok, cgrp], [p.tok])
                k.op("dve", lambda e, qi=qi, h=h, p=p: e.tensor_copy(colT[:, qi, h, :], p[:, 0:16]), reads=[p.tok, colT.tok], writes=[colT.tok])
        for h in range(4):
            k.op("dve", lambda e, h=h: e.tensor_mul(qTm[:, h, :, :], eye16[:], colT[:, 0, h, :].unsqueeze(2).to_broadcast([128, 16, 16])),
                 reads=[colT.tok, cgS, qTm.tok], writes=[qTm.tok])
        pO = PS[4]
        first = True
        for s in range(NS):
            Sin, Sout = Sst[s % 2], Snw[s % 2]
            k.dma("sp", Sin[:], sth[s].rearrange("h k v -> k h v"), Sin.tok, reads=[Sin.tok], writes=[Sin.tok])
            pv_ = psum()
            mm(pv_[:, :], selS[:, s, :], projs[:, HI:HI + 512], True, True, [cgS, projs.tok], [pv_.tok])
            for h in range(4):
                k.op("dve", lambda e, h=h, s=s, pv_=pv_: e.tensor_scalar_mul(tmv[:], pv_[:, h * 128:(h + 1) * 128], colT[:, 1, h, s:s + 1]),
                     reads=[pv_.tok, colT.tok, tmv.tok], writes=[tmv.tok])
                k.op("dve", lambda e, h=h, s=s, Sin=Sin, Sout=Sout: e.scalar_tensor_tensor(out=Sout[:, h, :], in0=Sin[:, h, :], scalar=colT[:, 2, h, s:s + 1],
                                                                                            in1=tmv[:], op0=ALU.mult, op1=ALU.add),
                     reads=[Sin.tok, colT.tok, tmv.tok, Sout.tok], writes=[Sout.tok])
            k.dma("sp", shg[s].rearrange("h k v -> k h v"), Sout[:], Sout.tok, reads=[Sout.tok])
            for h in range(4):
                mm(pO[0:16, h * 128:(h + 1) * 128], qTm[:, h, s, :], Sout[:, h, :], first, False, [qTm.tok, Sout.tok], [pO.tok], skip=True)
                first = False
        for h in range(4):
            k.op("act", lambda e, h=h: e.activation(out=e1[:, h * 128:(h + 1) * 128], in_=pO[0:16, h * 128:(h + 1) * 128], func=AF.Square,
                                                    accum_out=sst[0:16, 8 + h:9 + h]),
                 reads=[pO.tok, e1.tok, sst.tok], writes=[e1.tok, sst.tok])
        k.op("dve", lambda e: e.tensor_scalar(sst[0:16, 8:12], sst[0:16, 8:12], 1.0 / 128, EPS, ALU.mult, ALU.add), reads=[sst.tok], writes=[sst.tok])
        k.op("act", lambda e: e.activation(out=sst[0:16, 8:12], in_=sst[0:16, 8:12], func=AF.Ln), reads=[sst.tok], writes=[sst.tok])
        k.op("act", lambda e: e.activation(out=sst[0:16, 12:16], in_=sst[0:16, 8:12], func=AF.Exp, scale=-0.5), reads=[sst.tok], writes=[sst.tok])
        k.op("dve", lambda e: e.tensor_mul(e1[:].rearrange("p (h v) -> p h v", h=4), pO[0:16, :].rearrange("p (h v) -> p h v", h=4),
                                           sst[0:16, 12:16].unsqueeze(2).to_broadcast([16, 4, 128])),
             reads=[pO.tok, sst.tok, e1.tok], writes=[e1.tok])
        k.op("dve", lambda e: e.tensor_mul(e1[:], e1[:], g5s[0:16, 1, :]), reads=[e1.tok, cgS], writes=[e1.tok])
        k.op("dve", lambda e: e.tensor_mul(mixS[:, 512:1024], e1[:], sos[:]), reads=[e1.tok, sos.tok, mixS.tok], writes=[mixS.tok])
        mxS = sb("mxS", [128, 8, 128], BF16, es=esS)
        k.op("pool", lambda e: e.memset(mxS[:], 0.0), reads=[mxS.tok], writes=[mxS.tok])
        for kc in range(8):
            p = psum()
            tr(p[:, 0:16], mixS[:, kc * 128:(kc + 1) * 128], ident[0:16, 0:16], [mixS.tok, cgrp], [p.tok])
            k.op("dve", lambda e, kc=kc, p=p: e.tensor_copy(mxS[:, kc, 0:16], p[:, 0:16]), reads=[p.tok, mxS.tok], writes=[mxS.tok])
        k.dma("sp", mxs[:, :, NB * SEQ:NB * SEQ + 128].rearrange("c p t -> p c t"), mxS[:], mxS.tok, reads=[mxS.tok])
    k.barrier()
    esS.close()

    esB = ExitStack()
    cgB = k.tok()
    w_out_t = sb("w_out_t", [128, 8, D], BF16, es=esB)
    wrt = sb("wrt", [128, 8, 20], es=esB)
    brt = sb("brt", [128, 20], es=esB)
    selE = sb("selE", [16, 16, 128], es=esB)
    for kc in range(8):
        k.dma("pool", w_out_t[:, kc, :], w_out[kc * 128:(kc + 1) * 128, :], cgB, writes=[cgB])
    k.dma("sp", wrt[:], wroute.rearrange("(c p) n -> p c n", p=128), cgB, writes=[cgB])
    k.dma("sp", brt[:], broute, cgB, writes=[cgB])
    k.dma("sp", selE[:], cd["selE"], cgB, writes=[cgB])
    gv1t = sb("gv1t", [128, D], es=esB)
    gv2t = sb("gv2t", [128, D], es=esB)
    mcol2 = sb("mcol2", [128, 2, 8], es=esB)
    mT = sb("mT", [128, 8, 128], BF16, es=esB)
    xbt = sb("xbt", [128, D], es=esB)
    x1t = sb("x1t", [128, D], es=esB)
    xn2 = sb("xn2", [128, D], es=esB)
    h2Tf = sb("h2Tf", [128, 8, 128], es=esB)
    h2T = sb("h2T", [128, 8, MB], BF16, es=esB)
    combT = sb("combT", [16, MB], es=esB)
    yacc = sb("yacc", [128, MB // 128, D], es=esB)
    WG = sb("WG", [128, 8, 4, 256], BF16, es=esB)
    WU = sb("WU", [128, 8, 4, 256], BF16, es=esB)
    WD = sb("WD", [128, 4, 2, D], BF16, es=esB)
    actT = sb("actT", [128, 4, 2, 512], BF16, es=esB)
    cbt = sb("cbt", [128, 512], es=esB)
    sil = sb("sil", [128, 512], es=esB)
    rt = sb("rt", [128, 16], es=esB)
    lg = sb("lg", [128, 20], es=esB)
    ohg = sb("ohg", [128, 4], es=esB)
    t16 = sb("t16", [128, 4, 4], es=esB)
    leg = sb("leg", [128, 3, 4], es=esB)
    oh12 = sb("oh12", [128, 2, 4], es=esB)
    c16 = sb("c16", [128, 4, 4], es=esB)
    ssb = sb("ssb", [128, 8], es=esB)
    print("phase B sbuf left:", nc.sbuf_bytes_remaining)

    def rstd_of(dst, src, n):
        k.op("dve", lambda e: e.tensor_scalar(dst, src, 1.0 / n, EPS, ALU.mult, ALU.add), reads=[ssb.tok], writes=[ssb.tok])
        k.op("act", lambda e: e.activation(out=dst, in_=dst, func=AF.Ln), reads=[ssb.tok], writes=[ssb.tok])
        k.op("act", lambda e: e.activation(out=dst, in_=dst, func=AF.Exp, scale=-0.5), reads=[ssb.tok], writes=[ssb.tok])

    def routing(t):
        pl = psum()
        for kc in range(8):
            mm(pl[:, 0:20], h2Tf[:, kc, :], wrt[:, kc, :], kc == 0, kc == 7, [h2Tf.tok, cgB], [pl.tok])
        k.op("dve", lambda e: e.tensor_add(lg[:], pl[:, 0:20], brt[:]), reads=[pl.tok, cgB, lg.tok], writes=[lg.tok])
        R = lambda i: rt[:, i:i + 1]
        k.op("dve", lambda e: e.reduce_max(R(0), lg[:, 0:4], axis=AX.X), reads=[lg.tok, rt.tok], writes=[rt.tok])
        k.op("dve", lambda e: e.tensor_scalar_mul(R(1), R(0), -1.0), reads=[rt.tok], writes=[rt.tok])
        k.op("act", lambda e: e.activation(out=ohg[:], in_=lg[:, 0:4], func=AF.Exp, bias=R(1), accum_out=R(2)),
             reads=[lg.tok, rt.tok, ohg.tok], writes=[ohg.tok, rt.tok])
        k.op("dve", lambda e: e.reciprocal(R(3), R(2)), reads=[rt.tok], writes=[rt.tok])
        k.op("dve", lambda e: e.tensor_scalar(ohg[:], lg[:, 0:4], R(0), None, ALU.is_equal), reads=[lg.tok, rt.tok, ohg.tok], writes=[ohg.tok])
        for g in range(4):
            k.op("dve", lambda e, g=g: e.tensor_scalar_mul(t16[:, g, :], lg[:, 4 + 4 * g:8 + 4 * g], ohg[:, g:g + 1]),
                 reads=[lg.tok, ohg.tok, t16.tok], writes=[t16.tok])
        k.op("dve", lambda e: e.tensor_add(leg[:, 0, :], t16[:, 0, :], t16[:, 1, :]), reads=[t16.tok, leg.tok], writes=[leg.tok])
        k.op("dve", lambda e: e.tensor_add(leg[:, 0, :], leg[:, 0, :], t16[:, 2, :]), reads=[t16.tok, leg.tok], writes=[leg.tok])
        k.op("dve", lambda e: e.tensor_add(leg[:, 0, :], leg[:, 0, :], t16[:, 3, :]), reads=[t16.tok, leg.tok], writes=[leg.tok])
        k.op("dve", lambda e: e.reduce_max(R(4), leg[:, 0, :], axis=AX.X), reads=[leg.tok, rt.tok], writes=[rt.tok])
        k.op("dve", lambda e: e.tensor_scalar(oh12[:, 0, :], leg[:, 0, :], R(4), None, ALU.is_equal), reads=[leg.tok, rt.tok, oh12.tok], writes=[oh12.tok])
        k.op("dve", lambda e: e.scalar_tensor_tensor(out=leg[:, 1, :], in0=oh12[:, 0, :], scalar=-1e9, in1=leg[:, 0, :], op0=ALU.mult, op1=ALU.add),
             reads=[oh12.tok, leg.tok], writes=[leg.tok])
        k.op("dve", lambda e: e.reduce_max(R(5), leg[:, 1, :], axis=AX.X), reads=[leg.tok, rt.tok], writes=[rt.tok])
        k.op("dve", lambda e: e.tensor_scalar(oh12[:, 1, :], leg[:, 1, :], R(5), None, ALU.is_equal), reads=[leg.tok, rt.tok, oh12.tok], writes=[oh12.tok])
        k.op("dve", lambda e: e.tensor_sub(R(6), R(5), R(4)), reads=[rt.tok], writes=[rt.tok])
        k.op("act", lambda e: e.activation(out=R(7), in_=R(6), func=AF.Exp), reads=[rt.tok], writes=[rt.tok])
        k.op("dve", lambda e: e.tensor_scalar_add(R(8), R(7), 1.0), reads=[rt.tok], writes=[rt.tok])
        k.op("dve", lambda e: e.reciprocal(R(9), R(8)), reads=[rt.tok], writes=[rt.tok])
        k.op("dve", lambda e: e.tensor_mul(R(10), R(9), R(3)), reads=[rt.tok], writes=[rt.tok])
        k.op("dve", lambda e: e.tensor_mul(R(11), R(10), R(7)), reads=[rt.tok], writes=[rt.tok])
        k.op("dve", lambda e: e.tensor_scalar_mul(leg[:, 2, :], oh12[:, 0, :], R(10)), reads=[oh12.tok, rt.tok, leg.tok], writes=[leg.tok])
        k.op("dve", lambda e: e.scalar_tensor_tensor(out=leg[:, 2, :], in0=oh12[:, 1, :], scalar=R(11), in1=leg[:, 2, :], op0=ALU.mult, op1=ALU.add),
             reads=[oh12.tok, rt.tok, leg.tok], writes=[leg.tok])
        for g in range(4):
            k.op("dve", lambda e, g=g: e.tensor_scalar_mul(c16[:, g, :], leg[:, 2, :], ohg[:, g:g + 1]),
                 reads=[leg.tok, ohg.tok, c16.tok], writes=[c16.tok])
        pc = psum()
        tr(pc[0:16, 0:128], c16[:].rearrange("p g e -> p (g e)"), ident[:], [c16.tok, cgrp], [pc.tok])
        k.op("act", lambda e: e.copy(combT[:, t * 128:(t + 1) * 128], pc[0:16, 0:128]), reads=[pc.tok, combT.tok], writes=[combT.tok])

    def batch_B(b, g0, xsrc, ydst, ntile, sample=False):
        for t in range(ntile):
            r0 = g0 + t * 128
            k.dma("sp", mT[:], mxs[:, :, r0:r0 + 128].rearrange("c p t -> p c t"), mT.tok, reads=[mT.tok], writes=[mT.tok])
            if sample:
                k.op("pool", lambda e: e.memset(xbt[:], 0.0), reads=[xbt.tok], writes=[xbt.tok])
                k.dma("sp", xbt[0:16, :], xsrc(t), xbt.tok, reads=[xbt.tok], writes=[xbt.tok])
            else:
                k.dma("sp", xbt[:], xsrc(t), xbt.tok, reads=[xbt.tok], writes=[xbt.tok])
            pm = (PS[6], PS[7])
            for half in range(2):
                for kc in range(8):
                    mm(pm[half][:, :], mT[:, kc, :], w_out_t[:, kc, half * 512:(half + 1) * 512], kc == 0, kc == 7, [mT.tok, cgB], [pm[half].tok])
                k.op("act", lambda e, half=half: e.activation(out=xn2[:, half * 512:(half + 1) * 512], in_=pm[half][:, :], func=AF.Square,
                                                              accum_out=ssb[:, half:half + 1]),
                     reads=[pm[half].tok, xn2.tok, ssb.tok], writes=[xn2.tok, ssb.tok])
            k.op("dve", lambda e: e.tensor_add(ssb[:, 2:3], ssb[:, 0:1], ssb[:, 1:2]), reads=[ssb.tok], writes=[ssb.tok])
            rstd_of(ssb[:, 3:4], ssb[:, 2:3], D)
            for half in range(2):
                hs = slice(half * 512, (half + 1) * 512)
                k.op("dve", lambda e, half=half, hs=hs: e.scalar_tensor_tensor(out=x1t[:, hs], in0=pm[half][:, :], scalar=ssb[:, 3:4], in1=gv1t[:, hs],
                                                                                op0=ALU.mult, op1=ALU.mult),
                     reads=[pm[half].tok, ssb.tok, gv1t.tok, x1t.tok], writes=[x1t.tok])
            k.op("dve", lambda e: e.tensor_add(x1t[:], x1t[:], xbt[:]), reads=[x1t.tok, xbt.tok], writes=[x1t.tok])
            k.dma("sp", x1s[r0:r0 + 128, :], x1t[:], x1t.tok, reads=[x1t.tok])
            k.op("act", lambda e: e.activation(out=xn2[:], in_=x1t[:], func=AF.Square, accum_out=ssb[:, 4:5]),
                 reads=[x1t.tok, xn2.tok, ssb.tok], writes=[xn2.tok, ssb.tok])
            rstd_of(ssb[:, 5:6], ssb[:, 4:5], D)
            k.op("dve", lambda e: e.tensor_scalar_mul(xn2[:], x1t[:], ssb[:, 5:6]), reads=[x1t.tok, ssb.tok, xn2.tok], writes=[xn2.tok])
            if sample:
                k.op("dve", lambda e: e.tensor_mul(xn2[0:16, :], xn2[0:16, :], yacc[0:16, 1, :]), reads=[xn2.tok, yacc.tok], writes=[xn2.tok])
                k.op("dve", lambda e: e.tensor_add(xn2[0:16, :], xn2[0:16, :], yacc[0:16, 2, :]), reads=[xn2.tok, yacc.tok], writes=[xn2.tok])
            for hh in range(2):
                p = psum()
                for c4 in range(4):
                    kc = hh * 4 + c4
                    tr(p[:, c4 * 128:(c4 + 1) * 128], xn2[:, kc * 128:(kc + 1) * 128], ident[:], [xn2.tok, cgrp], [p.tok])
                for c4 in range(4):
                    kc = hh * 4 + c4
                    k.op("act", lambda e, kc=kc, c4=c4, p=p: e.activation(out=h2Tf[:, kc, :], in_=p[:, c4 * 128:(c4 + 1) * 128], func=AF.Identity,
                                                                          scale=mcol2[:, 0, kc:kc + 1], bias=mcol2[:, 1, kc:kc + 1]),
                         reads=[p.tok, mcol2.tok, h2Tf.tok], writes=[h2Tf.tok])
            k.op("dve", lambda e, t=t: e.tensor_copy(h2T[:, :, t * 128:(t + 1) * 128], h2Tf[:]), reads=[h2Tf.tok, h2T.tok], writes=[h2T.tok])
            routing(t)
        nsub = (ntile + 3) // 4
        for g in range(4):
            for e_ in range(4):
                E = 4 * g + e_
                k.dma("pool", WG[:, :, e_, :], wg_d[E].rearrange("(c p) f -> p c f", p=128), WG.tok, reads=[WG.tok], writes=[WG.tok])
                k.dma("pool", WU[:, :, e_, :], wu_d[E].rearrange("(c p) f -> p c f", p=128), WU.tok, reads=[WU.tok], writes=[WU.tok])
                k.dma("pool", WD[:, e_, :, :], wd_d[E].rearrange("(c p) n -> p c n", p=128), WD.tok, reads=[WD.tok], writes=[WD.tok])
            for sub in range(nsub):
                nt_sub = min(4, ntile - sub * 4)
                ncol = nt_sub * 128
                cs = slice(sub * 512, sub * 512 + ncol)
                for e_ in range(4):
                    E = 4 * g + e_
                    pc = psum()
                    mm(pc[:, 0:ncol], selE[:, E, :], combT[:, cs], True, True, [cgB, combT.tok], [pc.tok])
                    k.op("act", lambda e, pc=pc: e.copy(cbt[:, 0:ncol], pc[:, 0:ncol]), reads=[pc.tok, cbt.tok], writes=[cbt.tok])
                    for fc in range(2):
                        pg = psum()
                        for kc in range(8):
                            mm(pg[:, 0:ncol], WG[:, kc, e_, fc * 128:(fc + 1) * 128], h2T[:, kc, cs], kc == 0, kc == 7, [WG.tok, h2T.tok], [pg.tok])
                        pu = psum()
                        for kc in range(8):
                            mm(pu[:, 0:ncol], WU[:, kc, e_, fc * 128:(fc + 1) * 128], h2T[:, kc, cs], kc == 0, kc == 7, [WU.tok, h2T.tok], [pu.tok])
                        k.op("act", lambda e, pg=pg: e.activation(out=sil[:, 0:ncol], in_=pg[:, 0:ncol], func=AF.Silu), reads=[pg.tok, sil.tok], writes=[sil.tok])
                        k.op("dve", lambda e, pu=pu: e.tensor_mul(sil[:, 0:ncol], sil[:, 0:ncol], pu[:, 0:ncol]), reads=[pu.tok, sil.tok], writes=[sil.tok])
                        k.op("dve", lambda e, e_=e_, fc=fc: e.tensor_mul(actT[:, e_, fc, 0:ncol], sil[:, 0:ncol], cbt[:, 0:ncol]),
                             reads=[sil.tok, cbt.tok, actT.tok], writes=[actT.tok])
                for tl in range(nt_sub):
                    t = sub * 4 + tl
                    for half in range(2):
                        py = PS[4 + half]
                        i = 0
                        for e_ in range(4):
                            for fc in range(2):
                                mm(py[:, :], actT[:, e_, fc, tl * 128:(tl + 1) * 128], WD[:, e_, fc, half * 512:(half + 1) * 512], i == 0, i == 7,
                                   [actT.tok, WD.tok], [py.tok])
                                i += 1
                        hs = slice(half * 512, (half + 1) * 512)
                        if g == 0:
                            k.op("act", lambda e, py=py, t=t, hs=hs: e.copy(yacc[:, t, hs], py[:, :]), reads=[py.tok, yacc.tok], writes=[yacc.tok])
                        else:
                            k.op("dve", lambda e, py=py, t=t, hs=hs: e.tensor_add(yacc[:, t, hs], yacc[:, t, hs], py[:, :]), reads=[py.tok, yacc.tok], writes=[yacc.tok])
        for t in range(ntile):
            r0 = g0 + t * 128
            k.op("act", lambda e, t=t: e.activation(out=xn2[:], in_=yacc[:, t, :], func=AF.Square, accum_out=ssb[:, 6:7]),
                 reads=[yacc.tok, xn2.tok, ssb.tok], writes=[xn2.tok, ssb.tok])
            rstd_of(ssb[:, 7:8], ssb[:, 6:7], D)
            k.dma("sp", x1t[:], x1s[r0:r0 + 128, :], x1t.tok, reads=[x1t.tok], writes=[x1t.tok])
            k.op("dve", lambda e, t=t: e.scalar_tensor_tensor(out=xn2[:], in0=yacc[:, t, :], scalar=ssb[:, 7:8], in1=gv2t[:], op0=ALU.mult, op1=ALU.mult),
                 reads=[yacc.tok, ssb.tok, gv2t.tok, xn2.tok], writes=[xn2.tok])
            k.op("dve", lambda e: e.tensor_add(xn2[:], xn2[:], x1t[:]), reads=[xn2.tok, x1t.tok], writes=[xn2.tok])
            if sample:
                k.dma("sp", ydst(t), xn2[0:16, :], xn2.tok, reads=[xn2.tok])
            else:
                k.dma("sp", ydst(t), xn2[:], xn2.tok, reads=[xn2.tok])

    if stage >= 3:
        for b in range(NB):
            k.dma("sp", gv1t[:], gvs[b, 0], gv1t.tok, reads=[gv1t.tok], writes=[gv1t.tok])
            k.dma("sp", gv2t[:], gvs[b, 1], gv2t.tok, reads=[gv2t.tok], writes=[gv2t.tok])
            k.op("dve", lambda e, b=b: e.scalar_tensor_tensor(out=mcol2[:, 0, :], in0=aT[:, 32:40, 16 + b], scalar=1.0, in1=gcol[:, 2, :],
                                                               op0=ALU.add, op1=ALU.mult),
                 reads=[aT.tok, cgrp, mcol2.tok], writes=[mcol2.tok])
            k.op("dve", lambda e, b=b: e.tensor_copy(mcol2[:, 1, :], aT[:, 24:32, 16 + b]), reads=[aT.tok, mcol2.tok], writes=[mcol2.tok])
            nbat = int(os.environ.get("KNBAT", SEQ // MB))
            for bt in range(nbat):
                t00 = bt * MB
                batch_B(b, b * SEQ + t00,
                        lambda t, b=b, t00=t00: xp[b, t00 + t * 128:t00 + (t + 1) * 128, :],
                        lambda t, b=b, t00=t00: yp[b, t00 + t * 128:t00 + (t + 1) * 128, :], MB // 128)
    if stage >= 4:
        k.op("pool", lambda e: e.memset(gv1t[:], 0.0), reads=[gv1t.tok], writes=[gv1t.tok])
        k.op("pool", lambda e: e.memset(gv2t[:], 0.0), reads=[gv2t.tok], writes=[gv2t.tok])
        k.dma("sp", gv1t[0:16, :], sms[2], gv1t.tok, reads=[gv1t.tok], writes=[gv1t.tok])
        k.dma("sp", gv2t[0:16, :], sms[5], gv2t.tok, reads=[gv2t.tok], writes=[gv2t.tok])
        k.dma("sp", yacc[0:16, 1, :], sms[3], yacc.tok, reads=[yacc.tok], writes=[yacc.tok])
        k.dma("sp", yacc[0:16, 2, :], sms[4], yacc.tok, reads=[yacc.tok], writes=[yacc.tok])
        k.op("pool", lambda e: e.memset(mcol2[:, 0, :], 1.0), reads=[mcol2.tok], writes=[mcol2.tok])
        k.op("pool", lambda e: e.memset(mcol2[:, 1, :], 0.0), reads=[mcol2.tok], writes=[mcol2.tok])
        batch_B(None, NB * SEQ, lambda t: xs_d, lambda t: ys, 1, sample=True)
    k.barrier()
    esB.close()

    print("instructions:", k.nins, "dma sems:", len(k.dsems), "sbuf left:", nc.sbuf_bytes_remaining)
    return nc, cst


_CACHE = {}


def kernel(x_prompt, x_sample, c_prompt, c_sample, cache_cmp_kv, cache_slc_kv, cache_win_kv, state_hgrn, page_table,
           w_ada, b_ada, g_pre_mix, g_post_mix, g_pre_ffn, g_post_ffn, w_in, w_phi1, b_phi1, w_phi2, g_nsa_out,
           hgrn_lb_logits, g_hgrn_out, w_out, w_route_group, b_route_group, w_route_expert, b_route_expert,
           w_exp_gate, w_exp_up, w_exp_down):
    stage = STAGE
    if "nc" not in _CACHE:
        _CACHE["nc"] = build(stage)
    nc, cst = _CACHE["nc"]
    f = lambda a: np.ascontiguousarray(np.asarray(a, dtype=np.float32))
    bc = lambda v: np.ascontiguousarray(np.broadcast_to(np.asarray(v, np.float32).reshape(1, -1), (128, v.size)))
    common = {
        "w_ada": f(w_ada[0]), "b_ada": f(b_ada[0]).reshape(1, -1),
        "gcol": np.ascontiguousarray(np.stack([np.asarray(v[0], np.float32).reshape(8, 128).T for v in (g_pre_mix, g_post_mix, g_pre_ffn, g_post_ffn)], axis=1)),
        "gb": np.ascontiguousarray(np.stack([bc(g_pre_mix[0]), bc(g_post_mix[0]), bc(g_pre_ffn[0]), bc(g_post_ffn[0])], axis=1)),
        "g512": np.ascontiguousarray(np.stack([bc(g_nsa_out[0]), bc(np.asarray(g_hgrn_out[0]).reshape(-1)),
                                               bc(hgrn_lb_logits[0]), bc(hgrn_lb_logits[1])], axis=1)),
        "w_in": f(w_in[0]), "w_out": f(w_out[0]),
        "wroute": np.ascontiguousarray(np.concatenate([f(w_route_group[0]), f(w_route_expert[0])], axis=1)),
        "broute": bc(np.concatenate([np.asarray(b_route_group[0]), np.asarray(b_route_expert[0])])),
        **({"wg": f(w_exp_gate[0]), "wu": f(w_exp_up[0]), "wd": f(w_exp_down[0])} if stage >= 3 else {}),
        "bphi1": f(b_phi1[0]).reshape(128, 1),
        "wphi2": f(w_phi2[0]).reshape(128, 64),
    }
    w1 = f(w_phi1[0])
    bd = np.zeros((128, 32, 128), np.float32)
    bd[0:64, :, 0:64] = w1[0].transpose(1, 0, 2)
    bd[64:128, :, 64:128] = w1[1].transpose(1, 0, 2)
    common["wphi1"] = bd
    for n, a in cst.items():
        common["c_" + n] = a
    if stage >= 4:
        poolc_all = f(cache_cmp_kv[0]).reshape(NPOOL * 128, 256)
        pools_all = f(cache_slc_kv[0]).reshape(NPOOL * 128, 256)
    in_maps = []
    for c in range(NCORES):
        m = dict(common)
        m["xp"] = f(x_prompt[NB * c:NB * (c + 1)])
        if stage >= 4:
            m["xs"] = f(x_sample[NS * c:NS * (c + 1), 0])
            m["poolc"] = poolc_all
            m["pools"] = pools_all
            m["winb"] = f(cache_win_kv[0, NS * c:NS * (c + 1)]).reshape(NS, 512, 256)
            m["sth"] = f(state_hgrn[0, NS * c:NS * (c + 1)])
            m["ptab"] = np.ascontiguousarray(np.asarray(page_table[NS * c:NS * (c + 1)], np.int32).reshape(1, NS * NPAGE))
        m["call"] = np.ascontiguousarray(np.concatenate([f(c_sample[NS * c:NS * (c + 1)]), f(c_prompt[NB * c:NB * (c + 1)])], axis=0))
        in_maps.append(m)
    res = run_bass_kernel_spmd(nc, in_maps, core_ids=list(range(NCORES)))
    R = res.results
    cat = lambda n: np.concatenate([r[n] for r in R], axis=0)
    y_p = cat("yp")
    p_cmp = cat("pcmp").reshape(1, 16, SEQ, 2, 2, 64)
    p_slc = cat("pslc").reshape(1, 16, SEQ, 2, 2, 64)
    p_win = cat("pwin").reshape(1, 16, 512, 2, 2, 64)
    p_hg = cat("phg").reshape(1, 16, 4, 128, 128)
    z = lambda *s: np.zeros(s, np.float32)
    if stage < 4:
        return (y_p, z(128, 1, D), p_cmp, p_slc, p_win, p_hg, z(1, 128, 1, 2, 2, 64), z(1, 128, 1, 2, 2, 64),
                z(1, 128, 512, 2, 2, 64), z(1, 128, 4, 128, 128))
    y_s = cat("ys").reshape(128, 1, D)
    s_cmp = cat("scmp").reshape(1, 128, 1, 2, 2, 64)
    s_slc = cat("sslc").reshape(1, 128, 1, 2, 2, 64)
    s_win = cat("swin").reshape(1, 128, 512, 2, 2, 64)
    s_hg = cat("shg").reshape(1, 128, 4, 128, 128)
    return (y_p, y_s, p_cmp, p_slc, p_win, p_hg, s_cmp, s_slc, s_win, s_hg)
```

```python
import os
import numpy as np
from contextlib import ExitStack
import concourse.bass as bass
import concourse.mybir as mybir
from concourse.bass_utils import run_bass_kernel_spmd

F32 = mybir.dt.float32
BF16 = mybir.dt.bfloat16
I32 = mybir.dt.int32
AF = mybir.ActivationFunctionType
ALU = mybir.AluOpType
AX = mybir.AxisListType

NCORES = 8
D = 1024
SEQ = int(os.environ.get('KSEQ', 4096))
NB = 2
NS = 16
NT = SEQ // 128
NST = SEQ // 512
INC = 3352
PAST = 8192
NPAGE = 64
NPOOL = int(os.environ.get("KNPOOL", 10240))
EPS = 1e-6
NEG = -30000.0
TOKS = NB * SEQ + 128
MB = min(1024, SEQ)
STAGE = int(os.environ.get("KSTAGE", "4"))


class Sem:
    __slots__ = ("h", "val")

    def __init__(self, h):
        self.h = h
        self.val = 0


class Tok:
    __slots__ = ("w", "r", "ds", "x")

    def __init__(self):
        self.w = {}
        self.r = {}
        self.ds = {}
        self.x = False


class K:
    def __init__(self, nc):
        self.nc = nc
        self.eng = {"pe": nc.tensor, "act": nc.scalar, "dve": nc.vector, "pool": nc.gpsimd, "sp": nc.sync}
        self.esem = {n: Sem(nc.alloc_semaphore("e_" + n)) for n in self.eng}
        self.seen = {n: {} for n in self.eng}
        self.dsems = []
        self.nins = 0

    def tok(self):
        return Tok()

    def _wait(self, en, deps):
        seen = self.seen[en]
        e = self.eng[en]
        for s, v in deps.items():
            if seen.get(s, 0) < v:
                e.wait_ge(s.h, v)
                seen[s] = v
                self.nins += 1

    def _deps(self, en, reads, writes):
        deps = {}
        pes = self.esem["pe"]
        own = self.esem.get(en)
        for t in reads:
            for s, v in t.w.items():
                if deps.get(s, 0) < v:
                    deps[s] = v
            if t.x:
                for s, v in t.r.items():
                    if s is not own and deps.get(s, 0) < v:
                        deps[s] = v
        for t in writes:
            for s, v in t.r.items():
                if deps.get(s, 0) < v:
                    deps[s] = v
            for s, v in t.w.items():
                if en == "pe" and s is pes:
                    continue
                if deps.get(s, 0) < v:
                    deps[s] = v
        return deps

    def op(self, en, fn, reads=(), writes=()):
        self._wait(en, self._deps(en, reads, writes))
        ins = fn(self.eng[en])
        s = self.esem[en]
        s.val += 1
        ins.then_inc(s.h, 1)
        self.nins += 1
        for t in reads:
            t.r[s] = s.val
        for t in writes:
            t.w[s] = s.val

    def dsem(self, t, q):
        key = "sw" if q == "pool" else "hw"
        if key not in t.ds:
            t.ds[key] = Sem(self.nc.alloc_semaphore("d%d" % len(self.dsems)))
            self.dsems.append(t.ds[key])
        return t.ds[key]

    def dma(self, q, out, in_, sb, reads=(), writes=(), **kw):
        self._wait(q, self._deps(q, reads, writes))
        s = self.dsem(sb, q)
        ins = self.eng[q].dma_start(out=out, in_=in_, **kw)
        s.val += 16
        ins.then_inc(s.h, 16)
        self.nins += 1
        for t in reads:
            t.r[s] = s.val
        for t in writes:
            t.w[s] = s.val

    def idma(self, out, in_, idx_ap, sb, reads=(), writes=()):
        q = "pool"
        self._wait(q, self._deps(q, reads, writes))
        s = self.dsem(sb, q)
        ins = self.eng[q].indirect_dma_start(out=out, out_offset=None, in_=in_,
                                             in_offset=bass.IndirectOffsetOnAxis(ap=idx_ap, axis=0))
        s.val += 16
        ins.then_inc(s.h, 16)
        self.nins += 1
        for t in reads:
            t.r[s] = s.val
        for t in writes:
            t.w[s] = s.val

    def barrier(self):
        allsems = list(self.esem.values()) + self.dsems
        for en in self.eng:
            self._wait(en, {s: s.val for s in allsems if s.val > 0})
        for en in self.eng:
            s = self.esem[en]
            ins = self.eng[en].nop()
            s.val += 1
            ins.then_inc(s.h, 1)
        for en in self.eng:
            self._wait(en, {s: s.val for s in self.esem.values()})


class Tl:
    def __init__(self, k, t):
        self.t = t
        self.tok = k.tok()

    def __getitem__(self, key):
        return self.t[key]


def _consts(stage=0):
    c = {}
    c["ident"] = np.eye(128, dtype=np.float32)
    key = np.arange(128)[:, None]
    tok = np.arange(128)[None, :]
    masks = np.zeros((128, 19, 128), np.float32)
    masks[:, 0, :] = np.where(key <= tok, 0.0, NEG)
    masks[:, 1, :] = np.where(key > tok, 0.0, NEG)
    for dl in range(17):
        masks[:, 2 + dl, :] = np.where(16 * key + 31 <= 128 * dl + tok, 0.0, NEG)
    c["masks"] = masks
    efull = np.zeros((64, SEQ), np.float32)
    efull[np.arange(SEQ) // 64, np.arange(SEQ)] = 1.0
    c["efull"] = efull
    n = np.arange(256)[:, None]
    j = np.arange(64)[None, :]
    ov = ((n * 16 < (j + 1) * 64) & (n * 16 + 32 > j * 64) & (n < 255)).astype(np.float32)
    ovp = np.zeros((128, 2, 64), np.float32)
    ovp[:, 0, :] = ov[0:128]
    ovp[:, 1, :] = ov[128:256]
    c["ov"] = ovp
    t = np.arange(SEQ)
    kaug = np.stack([128.0 * (t // 128), (t % 128).astype(np.float64), np.ones(SEQ), np.ones(SEQ)]).astype(np.float32)
    c["kaug"] = kaug
    e = np.arange(256) * 16 + 31
    caug = np.stack([128.0 * (e // 128), (e % 128).astype(np.float64), np.ones(256), np.ones(256)]).astype(np.float32)
    c["caug"] = caug
    slopes = np.exp2(-8.0 * np.arange(1, 9) / 8.0)
    qaug = np.zeros((4, 8, SEQ), np.float32)
    for h in range(8):
        qaug[0, h, :] = slopes[h]
        qaug[1, h, :] = slopes[h]
        qaug[2, h, :] = -slopes[h] * 128.0 * (t // 128)
        qaug[3, h, :] = -slopes[h] * 127.0
    c["qaug"] = qaug
    pos = np.arange(SEQ)[:, None]
    jj = np.arange(64)[None, :]
    cur = pos // 64
    forced = (jj == 0) | (jj == cur) | (jj == cur - 1)
    valid = jj * 64 <= pos
    selv = (valid & ~forced).astype(np.float32)
    sela = np.where(forced, 100.0, np.where(valid, 0.0, -1.0)).astype(np.float32)
    c["selv"] = selv
    c["sela"] = sela
    s_ = np.arange(128)[:, None]
    t_ = np.arange(128)[None, :]
    same = (s_ // 64) == (t_ // 64)
    hm = np.zeros((128, 3, 128), np.float32)
    hm[:, 0, :] = same & (s_ <= t_)
    hm[:, 1, :] = same & (s_ > t_)
    hm[:, 2, :] = same & (s_ <= t_)
    c["hmat"] = hm
    ci = np.zeros((128, 2), np.float32)
    ci[0:64, 0] = 1.0
    ci[64:128, 1] = 1.0
    c["cind"] = ci
    se = np.zeros((16, 16, 128), np.float32)
    for E in range(16):
        se[E, E, :] = 1.0
    c["selE"] = se
    if stage >= 4:
        pg = np.arange(NPAGE)[None, :, None]
        sl = slopes[None, None, :]
        c["pgw"] = np.ascontiguousarray(np.broadcast_to(np.exp(-sl * 128.0 * (63 - pg)), (128, NPAGE, 8))).astype(np.float32)
        g4 = np.zeros((128, 32), np.float32)
        for g in range(2):
            for h in range(4):
                for s_i in range(16):
                    g4[g * 64 + h * 16 + s_i, 2 * s_i + g] = 1.0
        c["g4"] = g4
        c["eye16"] = np.ascontiguousarray(np.broadcast_to(np.eye(16, dtype=np.float32), (128, 16, 16)))
        c["eyem"] = np.ascontiguousarray(np.broadcast_to(np.eye(16, dtype=np.float32)[:, None, :], (16, 4, 16)))
        vm = np.ones((128, 8), np.float32)
        vm[127, 3] = 0.0
        vm[0, 4] = 0.0
        c["vm"] = vm
        sadd = np.zeros((32, 128), np.float32)
        sadd[:, 0] = 100.0
        sadd[:, 127] = 100.0
        c["sadd"] = sadd
        c["iop"] = np.arange(128, dtype=np.float32).reshape(128, 1)
        kk = np.arange(PAST)
        efS = np.zeros((128, PAST), np.float32)
        efS[kk // 64, kk] = 1.0
        c["efS"] = efS
        nn_ = (np.arange(4)[None, :, None] * 128 + np.arange(128)[:, None, None])
        j_ = np.arange(128)[None, None, :]
        c["ovS"] = ((nn_ * 16 < (j_ + 1) * 64) & (nn_ * 16 + 32 > j_ * 64) & (nn_ < 511)).astype(np.float32)
        qa = np.zeros((2, 8, 16), np.float32)
        qa[:, :, :] = -slopes[None, :, None]
        c["qaugS"] = qa
        dist = PAST - (np.arange(512) * 16 + 31)
        dist[511] = 0
        c["caugS"] = np.stack([128.0 * (dist // 128), (dist % 128).astype(np.float64)]).astype(np.float32)
        c["paugS"] = np.stack([128.0 - np.arange(128), np.zeros(128)]).astype(np.float32)
        dw = 512 - np.arange(512)
        c["waugS"] = np.stack([128.0 * (dw // 128), (dw % 128).astype(np.float64)]).astype(np.float32)
    return c


def build(stage):
    nc = bass.Bass("TRN2", target_bir_lowering=False)
    k = K(nc)
    cst = _consts(stage)

    def din(name, shape, dt=F32):
        return nc.dram_tensor(name, list(shape), dt, kind="ExternalInput").ap()

    def dout(name, shape, dt=F32):
        return nc.dram_tensor(name, list(shape), dt, kind="ExternalOutput").ap()

    xp = din("xp", [NB, SEQ, D])
    call = din("call", [18, D])
    w_ada = din("w_ada", [D, 6 * D])
    b_ada = din("b_ada", [1, 6 * D])
    gb = din("gb", [128, 4, D])
    gcol_d = din("gcol", [128, 4, 8])
    g512 = din("g512", [128, 4, 512])
    w_in = din("w_in", [D, INC])
    w_out = din("w_out", [D, D])
    wphi1 = din("wphi1", [128, 32, 128])
    wphi2 = din("wphi2", [128, 64])
    bphi1 = din("bphi1", [128, 1])
    wroute = din("wroute", [D, 20])
    broute = din("broute", [128, 20])
    if stage >= 3:
        wg_d = din("wg", [16, D, 256])
        wu_d = din("wu", [16, D, 256])
        wd_d = din("wd", [16, 256, D])
    cd = {n: din("c_" + n, a.shape) for n, a in cst.items()}

    yp = dout("yp", [NB, SEQ, D])
    pcmp = dout("pcmp", [NB, SEQ, 256])
    pslc = dout("pslc", [NB, SEQ, 256])
    pwin = dout("pwin", [NB, 512, 256])
    phg = dout("phg", [NB, 4, 128, 128])
    dbg = dout("dbg", [NB, SEQ, D]) if stage < 0 else None

    x1s = nc.dram_tensor("x1s", [TOKS, D], F32).ap()
    mxs = (nc.dram_tensor("mxs", [8, 128, TOKS], BF16, kind="ExternalOutput").ap() if os.environ.get("KDBG") else nc.dram_tensor("mxs", [8, 128, TOKS], BF16).ap())
    gvs = nc.dram_tensor("gvs", [2, 2, 128, D], F32).ap()
    aas = nc.dram_tensor("aas", [18, 6 * D], F32).ap()
    sms = nc.dram_tensor("sms", [6, NS, D], F32).ap()
    wgs = nc.dram_tensor("wgs", [16, D, 256], BF16).ap()
    wus = nc.dram_tensor("wus", [16, D, 256], BF16).ap()
    wds = nc.dram_tensor("wds", [16, 256, D], BF16).ap()

    def sb(name, shape, dt=F32, es=None):
        if es is not None:
            return Tl(k, es.enter_context(nc.sbuf_tensor(name, list(shape), dt)))
        return Tl(k, nc.alloc_sbuf_tensor(name, list(shape), dt))

    PS = [Tl(k, nc.alloc_psum_tensor("ps%d" % i, [128, 512], F32)) for i in range(8)]
    for p_ in PS:
        p_.tok.x = True
    rr = [0]

    def psum():
        b = PS[rr[0] % 4]
        rr[0] += 1
        return b

    def mm(out, lhsT, rhs, start, stop, reads, writes, skip=False):
        k.op("pe", lambda e: e.matmul(out, lhsT=lhsT, rhs=rhs, start=start, stop=stop, skip_group_check=skip), reads=reads, writes=writes)

    def tr(out, in_, idn, reads, writes):
        k.op("pe", lambda e: e.transpose(out, in_, idn), reads=reads, writes=writes)

    cgrp = k.tok()
    ident = sb("ident", [128, 128])
    identb = sb("identb", [128, 128], BF16)
    k.dma("sp", ident[:], cd["ident"], cgrp, writes=[cgrp])
    k.op("dve", lambda e: e.tensor_copy(identb[:], ident[:]), reads=[cgrp], writes=[identb.tok])
    aT = sb("aT", [128, 48, 18])
    gcol = sb("gcol_s", [128, 4, 8])
    k.dma("sp", gcol[:], gcol_d, cgrp, writes=[cgrp])

    es0 = ExitStack()
    aall = sb("aall", [18, 6 * D], es=es0)
    ones1 = sb("ones1", [1, 128], es=es0)
    ct = sb("ct", [18, D], es=es0)
    ct2 = sb("ct2", [18, D], es=es0)
    cT = sb("cT", [128, 8, 18], es=es0)
    bada = sb("bada", [1, 6 * D], es=es0)
    gbt = sb("gbt", [128, 4, D], es=es0)
    sel = sb("sel", [18, 2, 128], es=es0)
    gvt = sb("gvt", [128, D], es=es0)
    wa = [sb("wa%d" % i, [128, 8, 512], es=es0) for i in range(2)]
    k.dma("sp", ct[:], call, ct.tok, writes=[ct.tok])
    k.dma("sp", bada[:], b_ada, bada.tok, writes=[bada.tok])
    k.dma("sp", gbt[:], gb, gbt.tok, writes=[gbt.tok])
    k.op("pool", lambda e: e.memset(ones1[:], 1.0), writes=[ones1.tok])
    k.op("act", lambda e: e.activation(out=ct2[:], in_=ct[:], func=AF.Silu), reads=[ct.tok], writes=[ct2.tok])
    for kc in range(8):
        p = psum()
        tr(p[:, 0:18], ct2[:, kc * 128:(kc + 1) * 128], ident[0:18, 0:18], [ct2.tok, cgrp], [p.tok])
        k.op("dve", lambda e, kc=kc, p=p: e.tensor_copy(cT[:, kc, :], p[:, 0:18]), reads=[p.tok], writes=[cT.tok])
    for cb in range(12):
        wt = wa[cb % 2]
        k.dma("sp", wt[:], w_ada[:, cb * 512:(cb + 1) * 512].rearrange("(c p) n -> p c n", p=128), wt.tok,
              writes=[wt.tok])
        p = psum()
        for kc in range(8):
            mm(p[0:18, :], cT[:, kc, :], wt[:, kc, :], kc == 0, False, [cT.tok, wt.tok], [p.tok])
        mm(p[0:18, :], ones1[0:1, 0:18], bada[0:1, cb * 512:(cb + 1) * 512], False, True, [ones1.tok, bada.tok], [p.tok])
        k.op("dve", lambda e, p=p, cb=cb: e.tensor_copy(aall[:, cb * 512:(cb + 1) * 512], p[0:18, :]),
             reads=[p.tok], writes=[aall.tok])
    k.dma("sp", aas, aall[:], aall.tok, reads=[aall.tok])
    for ch in range(48):
        p = psum()
        tr(p[:, 0:18], aall[:, ch * 128:(ch + 1) * 128], ident[0:18, 0:18], [aall.tok, cgrp], [p.tok])
        k.op("act", lambda e, ch=ch, p=p: e.copy(aT[:, ch, :], p[:, 0:18]), reads=[p.tok], writes=[aT.tok])
    k.op("pool", lambda e: e.memset(sel[:], 0.0), writes=[sel.tok])
    for b in range(2):
        k.op("pool", lambda e, b=b: e.affine_select(out=sel[:, b, :], in_=sel[:, b, :], pattern=[[0, 128]],
                                                    compare_op=ALU.not_equal, fill=1.0, base=-(16 + b),
                                                    channel_multiplier=1),
             reads=[sel.tok], writes=[sel.tok])
    for b in range(2):
        for gi, (part, gidx) in enumerate(((2, 1), (5, 3))):
            for half in range(2):
                p = psum()
                c0 = part * D + half * 512
                mm(p[:, :], sel[:, b, :], aall[:, c0:c0 + 512], True, True, [sel.tok, aall.tok], [p.tok])
                k.op("dve", lambda e, p=p, half=half, gidx=gidx: e.tensor_mul(
                    gvt[:, half * 512:(half + 1) * 512], p[:, :], gbt[:, gidx, half * 512:(half + 1) * 512]),
                    reads=[p.tok, gbt.tok], writes=[gvt.tok])
            k.dma("sp", gvs[b, gi], gvt[:], gvt.tok, reads=[gvt.tok])
    for i, (gidx, part, kind) in enumerate(((0, 1, "scale"), (None, 0, "shift"), (1, 2, "gate"), (2, 4, "scale"), (None, 3, "shift"), (3, 5, "gate"))):
        src = aall[0:16, part * D:(part + 1) * D]
        if kind == "scale":
            k.op("dve", lambda e, src=src, gidx=gidx: e.scalar_tensor_tensor(out=gvt[0:16, :], in0=src, scalar=1.0, in1=gbt[0:16, gidx, :], op0=ALU.add, op1=ALU.mult),
                 reads=[aall.tok, gbt.tok, gvt.tok], writes=[gvt.tok])
        elif kind == "shift":
            k.op("dve", lambda e, src=src: e.tensor_copy(gvt[0:16, :], src), reads=[aall.tok, gvt.tok], writes=[gvt.tok])
        else:
            k.op("dve", lambda e, src=src, gidx=gidx: e.tensor_mul(gvt[0:16, :], src, gbt[0:16, gidx, :]), reads=[aall.tok, gbt.tok, gvt.tok], writes=[gvt.tok])
        k.dma("sp", sms[i], gvt[0:16, :], gvt.tok, reads=[gvt.tok])
    k.barrier()
    es0.close()

    esA = ExitStack()
    masks = sb("masks", [128, 19, 128], BF16, es=esA)
    efull = sb("efull", [64, SEQ], BF16, es=esA)
    ovt = sb("ov", [128, 2, 64], BF16, es=esA)
    hmat = sb("hmat", [128, 3, 128], es=esA)
    hmaskb = sb("hmaskb", [128, 128], BF16, es=esA)
    hmatb = sb("hmatb", [128, 4, 128], BF16, es=esA)
    cind = sb("cind", [128, 2], es=esA)
    g5 = sb("g5", [128, 4, 512], es=esA)
    omlt = sb("omlt", [128, 512], es=esA)
    wphi1t = sb("wphi1t", [128, 32, 128], BF16, es=esA)
    wphi2t = sb("wphi2t", [128, 64], BF16, es=esA)
    bphi1t = sb("bphi1t", [128, 1], es=esA)
    w_in_t = sb("w_in_t", [128, 8, INC], BF16, es=esA)
    k.dma("sp", hmat[:], cd["hmat"], cgrp, writes=[cgrp])
    k.dma("sp", cind[:], cd["cind"], cgrp, writes=[cgrp])
    k.dma("sp", g5[:], g512, cgrp, writes=[cgrp])
    k.dma("sp", bphi1t[:], bphi1, cgrp, writes=[cgrp])
    k.dma("pool", masks[:], cd["masks"], cgrp, writes=[cgrp])
    k.dma("pool", efull[:], cd["efull"], cgrp, writes=[cgrp])
    k.dma("pool", ovt[:], cd["ov"], cgrp, writes=[cgrp])
    k.dma("pool", wphi1t[:], wphi1, cgrp, writes=[cgrp])
    k.dma("pool", wphi2t[:], wphi2, cgrp, writes=[cgrp])
    for kc in range(8):
        k.dma("pool", w_in_t[:, kc, :], w_in[kc * 128:(kc + 1) * 128, :], cgrp, writes=[cgrp])
    k.op("dve", lambda e: e.tensor_copy(hmaskb[:], hmat[:, 2, :]), reads=[cgrp], writes=[hmaskb.tok])
    k.op("dve", lambda e: e.tensor_copy(hmatb[:, 0:3, :], hmat[:]), reads=[cgrp], writes=[hmatb.tok])
    k.op("dve", lambda e: e.tensor_copy(hmatb[:, 3, 0:2], cind[:]), reads=[cgrp, hmatb.tok], writes=[hmatb.tok])
    k.op("dve", lambda e: e.tensor_sub(omlt[:], g5[:, 2, :], g5[:, 3, :]), reads=[cgrp], writes=[omlt.tok])
    k.op("act", lambda e: e.activation(out=omlt[:], in_=omlt[:], func=AF.Exp), reads=[omlt.tok], writes=[omlt.tok])
    k.op("dve", lambda e: e.tensor_scalar_add(omlt[:], omlt[:], 1.0), reads=[omlt.tok], writes=[omlt.tok])
    k.op("dve", lambda e: e.reciprocal(omlt[:], omlt[:]), reads=[omlt.tok], writes=[omlt.tok])

    KsT = sb("KsT", [68, 2, SEQ], BF16, es=esA)
    Vs = sb("Vs", [128, NT, 2, 65], BF16, es=esA)
    KwT = sb("KwT", [68, 2, 1024], BF16, es=esA)
    Vw = sb("Vw", [128, 8, 2, 65], BF16, es=esA)
    kvcT = sb("kvcT", [128, 2, 528], BF16, es=esA)
    KcT = sb("KcT", [68, 2, 256], BF16, es=esA)
    GT = sb("GT", [128, 2, 256], BF16, es=esA)
    Vc = sb("Vc", [128, 2, 2, 65], BF16, es=esA)
    qT = sb("qT", [68, 8, 512], BF16, es=esA)
    hT = sb("hT", [128, 8, 512], BF16, es=esA)
    k.dma("pool", KsT[64:68, 0, :], cd["kaug"], cgrp, writes=[cgrp])
    k.dma("pool", KsT[64:68, 1, :], cd["kaug"], cgrp, writes=[cgrp])
    k.dma("pool", KcT[64:68, 0, :], cd["caug"], cgrp, writes=[cgrp])
    k.dma("pool", KcT[64:68, 1, :], cd["caug"], cgrp, writes=[cgrp])
    k.op("pool", lambda e: e.memset(Vs[:, :, :, 64:65], 1.0), writes=[Vs.tok])
    k.op("pool", lambda e: e.memset(Vw[:, :, :, 64:65], 1.0), writes=[Vw.tok])
    k.op("pool", lambda e: e.memset(Vc[:, :, :, 64:65], 1.0), writes=[Vc.tok])
    xt = [sb("xt%d" % i, [128, D], es=esA) for i in range(2)]
    xn = sb("xn", [128, D], BF16, es=esA)
    st4 = sb("st4", [128, 16], es=esA)
    mcol = sb("mcol", [128, 2, 8], es=esA)
    kvo = [sb("kvo%d" % i, [128, 768], es=esA) for i in range(2)]
    gat = sb("gat", [128, 4, 24], es=esA)
    mixin = sb("mixin", [128, 4, D], BF16, es=esA)
    mxT = sb("mxT", [128, 8, 128], BF16, es=esA)
    PT = [sb("PT%d" % i, [128, 512], BF16, es=esA) for i in range(4)]
    ptr = [0]
    selv = [sb("selv%d" % i, [128, 2, 64], es=esA) for i in range(2)]
    selT = sb("selT", [64, 4, 128], BF16, es=esA)
    sc = sb("sc", [128, 4, 64], es=esA)
    m8 = sb("m8", [128, 16], es=esA)
    rin = sb("rin", [128, 3, 4], es=esA)
    fac = sb("fac", [128, 3, 4], es=esA)
    sg = sb("sg", [128, 24], es=esA)
    onsa = sb("onsa", [128, 8, 64], es=esA)
    tmpo = sb("tmpo", [128, 4, 64], es=esA)
    GTn = sb("GTn", [128, 32], BF16, es=esA)
    Sf = [sb("Sf%d" % i, [128, 4, 128], es=esA) for i in range(2)]
    Sb_ = [sb("Sb%d" % i, [128, 4, 128], BF16, es=esA) for i in range(2)]
    h1 = sb("h1", [128, 512], es=esA)
    h2 = sb("h2", [128, 512], es=esA)
    qf = sb("qf", [128, 512], es=esA)
    kkf = sb("kkf", [128, 512], es=esA)
    lf = sb("lf", [128, 512], es=esA)
    lfh = sb("lfh", [128, 512], BF16, es=esA)
    lfl = sb("lfl", [128, 512], BF16, es=esA)
    vb = sb("vb", [128, 512], BF16, es=esA)
    sog = sb("sog", [128, 512], es=esA)
    qe = sb("qe", [128, 512], BF16, es=esA)
    ke = sb("ke", [128, 512], BF16, es=esA)
    kd = sb("kd", [128, 512], BF16, es=esA)
    qeT = sb("qeT", [128, 3, 4, 128], BF16, es=esA)
    keT = sb("keT", [128, 4, 128], BF16, es=esA)
    AT = sb("AT", [128, 4, 128], BF16, es=esA)
    dcol = sb("dcol", [128, 8], es=esA)
    ss4 = sb("ss4", [128, 12], es=esA)
    k.op("pool", lambda e: e.memset(qeT[:], 0.0), writes=[qeT.tok])
    k.barrier()
    print("phase A sbuf left:", nc.sbuf_bytes_remaining)

    def evac_scaled(dst, src, scale, reads, writes, eng):
        if eng == "act":
            k.op("act", lambda e: e.activation(out=dst, in_=src, func=AF.Copy, scale=scale), reads=reads, writes=writes)
        else:
            k.op("dve", lambda e: e.tensor_scalar_mul(dst, src, scale), reads=reads, writes=writes)

    def hgrn_tile(b, st, j, S0i):
        KH = int(os.environ.get("KH", "99"))
        tt = st * 4 + j
        S0, S1 = Sf[S0i], Sf[1 - S0i]
        S0b, S1b = Sb_[S0i], Sb_[1 - S0i]
        lhs = lambda kc: hT[:, kc, j * 128:(j + 1) * 128]
        for ci in range(4):
            p = PS[4 + ci]
            c0 = 1304 + ci * 512
            for kc in range(8):
                mm(p[:, :], lhs(kc), w_in_t[:, kc, c0:c0 + 512], kc == 0, kc == 7, [hT.tok], [p.tok])
        pq, pf, pi, po_ = PS[4], PS[5], PS[6], PS[7]
        if KH < 1:
            return S0i
        k.op("act", lambda e: e.activation(out=h1[:], in_=pq[:, :], func=AF.Exp, scale=-1.0), reads=[pq.tok], writes=[h1.tok])
        k.op("dve", lambda e: e.tensor_scalar_add(h1[:], h1[:], 1.0), reads=[h1.tok], writes=[h1.tok])
        k.op("dve", lambda e: e.reciprocal(h1[:], h1[:]), reads=[h1.tok], writes=[h1.tok])
        k.op("dve", lambda e: e.tensor_mul(qf[:], pq[:, :], h1[:]), reads=[pq.tok, h1.tok], writes=[qf.tok])
        if KH < 2:
            return S0i
        k.op("act", lambda e: e.activation(out=h2[:], in_=pf[:, :], func=AF.Exp, scale=-1.0), reads=[pf.tok], writes=[h2.tok])
        k.op("dve", lambda e: e.tensor_scalar_add(h1[:], h2[:], 1.0), reads=[h2.tok, h1.tok], writes=[h1.tok])
        k.op("dve", lambda e: e.reciprocal(h1[:], h1[:]), reads=[h1.tok], writes=[h1.tok])
        k.op("dve", lambda e: e.tensor_mul(h2[:], h2[:], h1[:]), reads=[h2.tok, h1.tok], writes=[h2.tok])
        k.op("dve", lambda e: e.tensor_mul(kkf[:], h2[:], omlt[:]), reads=[h2.tok, omlt.tok], writes=[kkf.tok])
        k.op("dve", lambda e: e.tensor_scalar(h2[:], kkf[:], -1.0, 1.0, ALU.mult, ALU.add), reads=[kkf.tok, h2.tok], writes=[h2.tok])
        k.op("act", lambda e: e.activation(out=lf[:], in_=h2[:], func=AF.Ln), reads=[h2.tok], writes=[lf.tok])
        if KH < 3:
            return S0i
        k.op("act", lambda e: e.copy(vb[:], pi[:, :]), reads=[pi.tok], writes=[vb.tok])
        k.op("act", lambda e: e.activation(out=h1[:], in_=po_[:, :], func=AF.Exp, scale=-1.0), reads=[po_.tok, h1.tok], writes=[h1.tok])
        k.op("dve", lambda e: e.tensor_scalar_add(h1[:], h1[:], 1.0), reads=[h1.tok], writes=[h1.tok])
        k.op("dve", lambda e: e.reciprocal(h1[:], h1[:]), reads=[h1.tok], writes=[h1.tok])
        k.op("dve", lambda e: e.tensor_mul(sog[:], po_[:, :], h1[:]), reads=[po_.tok, h1.tok], writes=[sog.tok])
        if KH < 4:
            return S0i
        pb, pr, pd = PS[4], PS[5], PS[6]
        k.op("dve", lambda e: e.tensor_copy(lfh[:], lf[:]), reads=[lf.tok, lfh.tok], writes=[lfh.tok])
        k.op("dve", lambda e: e.tensor_sub(lfl[:], lf[:], lfh[:]), reads=[lf.tok, lfh.tok, lfl.tok], writes=[lfl.tok])
        mm(pb[:, :], hmatb[:, 0, :], lfh[:], True, False, [lfh.tok, hmatb.tok], [pb.tok])
        mm(pb[:, :], hmatb[:, 0, :], lfl[:], False, True, [lfl.tok, hmatb.tok], [pb.tok])
        mm(pr[:, :], hmatb[:, 1, :], lfh[:], True, False, [lfh.tok, hmatb.tok], [pr.tok])
        mm(pr[:, :], hmatb[:, 1, :], lfl[:], False, True, [lfl.tok, hmatb.tok], [pr.tok])
        for h in range(4):
            mm(pd[:, 2 * h:2 * h + 2], lfh[:, h * 128:(h + 1) * 128], hmatb[:, 3, 0:2], h == 0, False, [lfh.tok, hmatb.tok], [pd.tok])
            mm(pd[:, 2 * h:2 * h + 2], lfl[:, h * 128:(h + 1) * 128], hmatb[:, 3, 0:2], False, h == 3, [lfl.tok, hmatb.tok], [pd.tok])
        KHB = int(os.environ.get("KHB", "99"))
        if KHB > 0:
            k.op("act", lambda e: e.activation(out=h1[:], in_=pb[:, :], func=AF.Exp), reads=[pb.tok, h1.tok] + ([pr.tok, pd.tok] if os.environ.get("KE2") else []), writes=[h1.tok])
        if KHB > 1:
            k.op("dve", lambda e: e.tensor_mul(qe[:], qf[:], h1[:]), reads=[qf.tok, h1.tok], writes=[qe.tok])
        if KHB > 2:
            k.op("act", lambda e: e.activation(out=h2[:], in_=pb[:, :], func=AF.Exp, scale=-1.0), reads=[pb.tok, h2.tok], writes=[h2.tok])
        if KHB > 3:
            k.op("dve", lambda e: e.tensor_mul(ke[:], kkf[:], h2[:]), reads=[kkf.tok, h2.tok], writes=[ke.tok])
        if KHB > 4:
            k.op("act", lambda e: e.activation(out=h1[:], in_=pr[:, :], func=AF.Exp), reads=[pr.tok, h1.tok], writes=[h1.tok])
        if KHB > 5:
            k.op("dve", lambda e: e.tensor_mul(kd[:], kkf[:], h1[:]), reads=[kkf.tok, h1.tok], writes=[kd.tok])
        if KHB > 6:
            k.op("act", lambda e: e.activation(out=dcol[:], in_=pd[:, 0:8], func=AF.Exp), reads=[pd.tok], writes=[dcol.tok])
        if KH < 6:
            return S0i
        pt = PS[7]
        ptb = pt[:].bitcast(BF16)
        for h in range(4):
            tr(ptb[:, h * 128:(h + 1) * 128], qe[:, h * 128:(h + 1) * 128], identb[:], [qe.tok, identb.tok], [pt.tok])
            tr(ptb[:, 512 + h * 128:512 + (h + 1) * 128], ke[:, h * 128:(h + 1) * 128], identb[:], [ke.tok, identb.tok], [pt.tok])
        q4 = ptb[:, 0:512].rearrange("p (h t) -> p h t", h=4)
        k.op("dve", lambda e: e.tensor_copy(qeT[:, 0, :, :], q4), reads=[pt.tok], writes=[qeT.tok])
        k.op("act", lambda e: e.copy(qeT[:, 1, :, 0:64], q4[:, :, 0:64]), reads=[pt.tok], writes=[qeT.tok])
        k.op("act", lambda e: e.copy(qeT[:, 2, :, 64:128], q4[:, :, 64:128]), reads=[pt.tok], writes=[qeT.tok])
        k.op("dve", lambda e: e.tensor_copy(keT[:], ptb[:, 512:1024].rearrange("p (h t) -> p h t", h=4)), reads=[pt.tok], writes=[keT.tok])
        if KH < 7:
            return S0i
        pa = PS[4]
        for h in range(4):
            mm(pa[:, h * 128:(h + 1) * 128], keT[:, h, :], qeT[:, 0, h, :], h == 0, h == 3, [keT.tok, qeT.tok], [pa.tok])
        k.op("dve", lambda e: e.tensor_mul(AT[:], pa[:, :].rearrange("p (h t) -> p h t", h=4),
                                           hmaskb[:].unsqueeze(1).to_broadcast([128, 4, 128])),
             reads=[pa.tok, hmaskb.tok], writes=[AT.tok])
        if KH < 8:
            return S0i
        def state_update(Sin, Sout, Soutb, lo, ci, bank):
            pS = PS[bank]
            for h in range(4):
                mm(pS[:, h * 128:(h + 1) * 128], kd[lo:lo + 64, h * 128:(h + 1) * 128], vb[lo:lo + 64, h * 128:(h + 1) * 128],
                   h == 0, h == 3, [kd.tok, vb.tok], [pS.tok])
            for h in range(4):
                k.op("dve", lambda e, h=h: e.scalar_tensor_tensor(out=Sout[:, h, :], in0=Sin[:, h, :], scalar=dcol[:, 2 * h + ci:2 * h + ci + 1],
                                                                   in1=pS[:, h * 128:(h + 1) * 128], op0=ALU.mult, op1=ALU.add),
                     reads=[Sin.tok, dcol.tok, pS.tok], writes=[Sout.tok])
            k.op("act", lambda e: e.copy(Soutb[:], Sout[:]), reads=[Sout.tok], writes=[Soutb.tok])
        state_update(S0, S1, S1b, 0, 0, 5)
        if KH < 9:
            return S0i
        pO = PS[6]
        for h in range(4):
            o_ = pO[:, h * 128:(h + 1) * 128]
            mm(o_, qeT[:, 1, h, :], S0b[:, h, :], h == 0, False, [qeT.tok, S0b.tok], [pO.tok])
            mm(o_, AT[:, h, :], vb[:, h * 128:(h + 1) * 128], False, False, [AT.tok, vb.tok], [pO.tok])
            mm(o_, qeT[:, 2, h, :], S1b[:, h, :], False, h == 3, [qeT.tok, S1b.tok], [pO.tok])
        if KH < 10:
            return S0i
        state_update(S1, S0, S0b, 64, 1, 7)
        if KH < 11:
            return S0i
        for h in range(4):
            k.op("act", lambda e, h=h: e.activation(out=h2[:, h * 128:(h + 1) * 128], in_=pO[:, h * 128:(h + 1) * 128], func=AF.Square,
                                                    accum_out=ss4[:, h:h + 1]),
                 reads=[pO.tok, h2.tok], writes=[h2.tok, ss4.tok])
        k.op("dve", lambda e: e.tensor_scalar(ss4[:, 4:8], ss4[:, 0:4], 1.0 / 128, EPS, ALU.mult, ALU.add), reads=[ss4.tok], writes=[ss4.tok])
        k.op("act", lambda e: e.activation(out=ss4[:, 4:8], in_=ss4[:, 4:8], func=AF.Ln), reads=[ss4.tok], writes=[ss4.tok])
        k.op("act", lambda e: e.activation(out=ss4[:, 8:12], in_=ss4[:, 4:8], func=AF.Exp, scale=-0.5), reads=[ss4.tok], writes=[ss4.tok])
        k.op("dve", lambda e: e.tensor_mul(h1[:].rearrange("p (h v) -> p h v", h=4), pO[:, :].rearrange("p (h v) -> p h v", h=4),
                                           ss4[:, 8:12].unsqueeze(2).to_broadcast([128, 4, 128])),
             reads=[pO.tok, ss4.tok, h1.tok], writes=[h1.tok])
        k.op("dve", lambda e: e.tensor_mul(h1[:], h1[:], g5[:, 1, :]), reads=[h1.tok, cgrp], writes=[h1.tok])
        k.op("dve", lambda e: e.tensor_mul(mixin[:, j, 512:1024], h1[:], sog[:]), reads=[h1.tok, sog.tok], writes=[mixin.tok])
        return S0i

    def attention_tile(b, st, j):
        qt = st * 4 + j
        tt = qt
        r0 = qt * 128
        tq = slice(j * 128, (j + 1) * 128)
        sv = selv[qt % 2]
        k.dma("sp", sv[:, 0, :], cd["selv"][r0:r0 + 128, :], sv.tok, writes=[sv.tok])
        k.dma("sp", sv[:, 1, :], cd["sela"][r0:r0 + 128, :], sv.tok, writes=[sv.tok])
        k.op("act", lambda e: e.activation(out=sg[:], in_=gat[:, j, :], func=AF.Exp, scale=-1.0), reads=[gat.tok, sg.tok], writes=[sg.tok])
        k.op("dve", lambda e: e.tensor_scalar_add(sg[:], sg[:], 1.0), reads=[sg.tok], writes=[sg.tok])
        k.op("dve", lambda e: e.reciprocal(sg[:], sg[:]), reads=[sg.tok], writes=[sg.tok])
        sg3 = sg[:].rearrange("p (h x) -> p h x", x=3)
        for g in range(2):
            qr = qT[:, 4 * g:4 * g + 4, tq]

            def unit(kT_ap, ktoks, bias, maskidx, v_ap, vtoks, pv, first, last, extra_rhs=None, pu=None):
                s_ = psum()
                s3 = s_[:, :].rearrange("p (h t) -> p h t", h=4)
                mm(s3, kT_ap, qr, True, bias is None and maskidx is None, ktoks + [qT.tok], [s_.tok])
                if bias is not None:
                    mm(s3, bias, selT[:], False, maskidx is None, [cgrp, selT.tok], [s_.tok])
                if maskidx is not None:
                    mm(s3, identb[:], masks[:, maskidx, :].unsqueeze(1).to_broadcast([128, 4, 128]), False, True,
                       [cgrp, identb.tok], [s_.tok])
                P = PT[ptr[0] % 4]
                ptr[0] += 1
                k.op("act", lambda e: e.activation(out=P[:], in_=s_[:, :], func=AF.Exp), reads=[s_.tok, P.tok], writes=[P.tok])
                for h in range(4):
                    mm(pv[:, h * 65:(h + 1) * 65], P[:, h * 128:(h + 1) * 128], v_ap, first and h == 0, last and h == 3,
                       [P.tok] + vtoks, [pv.tok])
                if pu is not None:
                    for h in range(4):
                        mm(pu[:, h * 64:(h + 1) * 64], P[:, h * 128:(h + 1) * 128], extra_rhs, first and h == 0, last and h == 3,
                           [P.tok, cgrp], [pu.tok])

            pvc, pvu, pvs, pvw = PS[4], PS[5], PS[6], PS[7]
            nmax = min(8 * qt + 6, 254)
            nts = nmax // 128 + 1
            for nt in range(nts):
                dl = qt - 16 * nt
                unit(KcT[:, g, nt * 128:(nt + 1) * 128], [KcT.tok], None, (2 + dl) if dl <= 16 else None,
                     Vc[:, nt, g, :], [Vc.tok], pvc, nt == 0, nt == nts - 1, extra_rhs=ovt[:, nt, :], pu=pvu)
            pvc3 = pvc[:, 0:260].rearrange("p (h d) -> p h d", h=4)
            k.op("dve", lambda e: e.tensor_scalar_max(rin[:, 0, :], pvc3[:, :, 64], 1e-30), reads=[pvc.tok, rin.tok], writes=[rin.tok])
            k.op("dve", lambda e: e.reciprocal(rin[:, 0, :], rin[:, 0, :]), reads=[rin.tok], writes=[rin.tok])
            k.op("dve", lambda e: e.tensor_scalar_mul(sc[:, 0, :], pvu[:, 0:64], rin[:, 0, 0:1]), reads=[pvu.tok, rin.tok, sc.tok], writes=[sc.tok])
            for h in range(1, 4):
                k.op("dve", lambda e, h=h: e.scalar_tensor_tensor(out=sc[:, 0, :], in0=pvu[:, h * 64:(h + 1) * 64], scalar=rin[:, 0, h:h + 1],
                                                                   in1=sc[:, 0, :], op0=ALU.mult, op1=ALU.add),
                     reads=[pvu.tok, rin.tok, sc.tok], writes=[sc.tok])
            k.op("dve", lambda e: e.tensor_mul(sc[:, 1, :], sc[:, 0, :], sv[:, 0, :]), reads=[sc.tok, sv.tok], writes=[sc.tok])
            k.op("dve", lambda e: e.tensor_add(sc[:, 1, :], sc[:, 1, :], sv[:, 1, :]), reads=[sc.tok, sv.tok], writes=[sc.tok])
            k.op("dve", lambda e: e.max(m8[:, 0:8], sc[:, 1, :]), reads=[sc.tok, m8.tok], writes=[m8.tok])
            k.op("dve", lambda e: e.match_replace(sc[:, 2, :], m8[:, 0:8], sc[:, 1, :], -1e9), reads=[sc.tok, m8.tok], writes=[sc.tok])
            k.op("dve", lambda e: e.max(m8[:, 8:16], sc[:, 2, :]), reads=[sc.tok, m8.tok], writes=[m8.tok])
            k.op("dve", lambda e: e.tensor_scalar(sc[:, 3, :], sc[:, 1, :], m8[:, 15:16], None, ALU.is_ge), reads=[sc.tok, m8.tok], writes=[sc.tok])
            k.op("dve", lambda e: e.tensor_scalar(sc[:, 3, :], sc[:, 3, :], -NEG, NEG, ALU.mult, ALU.add), reads=[sc.tok], writes=[sc.tok])
            pst = psum()
            tr(pst[0:64, 0:128], sc[:, 3, :], ident[:], [sc.tok, cgrp], [pst.tok])
            k.op("dve", lambda e: e.tensor_copy(selT[:], pst[0:64, 0:128].unsqueeze(1).to_broadcast([64, 4, 128])),
                 reads=[pst.tok, selT.tok], writes=[selT.tok])
            for kt in range(qt + 1):
                unit(KsT[:, g, kt * 128:(kt + 1) * 128], [KsT.tok], efull[:, kt * 128:(kt + 1) * 128], 0 if kt == qt else None,
                     Vs[:, kt, g, :], [Vs.tok], pvs, kt == 0, kt == qt)
            k0 = max(0, qt - 4)
            for kt in range(k0, qt + 1):
                mi = 0 if kt == qt else (1 if kt == qt - 4 else None)
                unit(KwT[:, g, (kt % 8) * 128:(kt % 8 + 1) * 128], [KwT.tok], None, mi,
                     Vw[:, kt % 8, g, :], [Vw.tok], pvw, kt == k0, kt == qt)
            for x, pv in ((1, pvs), (2, pvw)):
                pv3 = pv[:, 0:260].rearrange("p (h d) -> p h d", h=4)
                k.op("dve", lambda e, x=x, pv3=pv3: e.tensor_scalar_max(rin[:, x, :], pv3[:, :, 64], 1e-30), reads=[pv.tok, rin.tok], writes=[rin.tok])
                k.op("dve", lambda e, x=x: e.reciprocal(rin[:, x, :], rin[:, x, :]), reads=[rin.tok], writes=[rin.tok])
            k.op("dve", lambda e: e.tensor_mul(fac[:], rin[:], sg3[:, 4 * g:4 * g + 4, :].rearrange("p h x -> p x h")),
                 reads=[rin.tok, sg.tok, fac.tok], writes=[fac.tok])
            for x, pv in ((0, pvc), (1, pvs), (2, pvw)):
                pv3 = pv[:, 0:260].rearrange("p (h d) -> p h d", h=4)
                fb = fac[:, x, :].unsqueeze(2).to_broadcast([128, 4, 64])
                if x == 0:
                    k.op("dve", lambda e, pv3=pv3, fb=fb: e.tensor_mul(onsa[:, 4 * g:4 * g + 4, :], pv3[:, :, 0:64], fb),
                         reads=[pv.tok, fac.tok, onsa.tok], writes=[onsa.tok])
                else:
                    k.op("dve", lambda e, pv3=pv3, fb=fb: e.tensor_mul(tmpo[:], pv3[:, :, 0:64], fb),
                         reads=[pv.tok, fac.tok, tmpo.tok], writes=[tmpo.tok])
                    k.op("dve", lambda e: e.tensor_add(onsa[:, 4 * g:4 * g + 4, :], onsa[:, 4 * g:4 * g + 4, :], tmpo[:]),
                         reads=[tmpo.tok, onsa.tok], writes=[onsa.tok])
        of = onsa[:].rearrange("p h d -> p (h d)")
        k.op("act", lambda e: e.activation(out=h2[:], in_=of, func=AF.Square, accum_out=ss4[:, 0:1]),
             reads=[onsa.tok, h2.tok, ss4.tok], writes=[h2.tok, ss4.tok])
        k.op("dve", lambda e: e.tensor_scalar(ss4[:, 4:5], ss4[:, 0:1], 1.0 / 512, EPS, ALU.mult, ALU.add), reads=[ss4.tok], writes=[ss4.tok])
        k.op("act", lambda e: e.activation(out=ss4[:, 4:5], in_=ss4[:, 4:5], func=AF.Ln), reads=[ss4.tok], writes=[ss4.tok])
        k.op("act", lambda e: e.activation(out=ss4[:, 8:9], in_=ss4[:, 4:5], func=AF.Exp, scale=-0.5), reads=[ss4.tok], writes=[ss4.tok])
        k.op("dve", lambda e: e.scalar_tensor_tensor(out=mixin[:, j, 0:512], in0=of, scalar=ss4[:, 8:9], in1=g5[:, 0, :],
                                                      op0=ALU.mult, op1=ALU.mult),
             reads=[onsa.tok, ss4.tok, cgrp, mixin.tok], writes=[mixin.tok])
        p = psum()
        pb_ = p[:].bitcast(BF16)
        for kc in range(8):
            tr(pb_[:, kc * 128:(kc + 1) * 128], mixin[:, j, kc * 128:(kc + 1) * 128], identb[:], [mixin.tok, identb.tok], [p.tok])
        k.op("act", lambda e: e.copy(mxT[:], pb_[:, 0:1024].rearrange("p (c t) -> p c t", c=8)), reads=[p.tok, mxT.tok], writes=[mxT.tok])
        g0 = b * SEQ + r0
        k.dma("sp", mxs[:, :, g0:g0 + 128].rearrange("c p t -> p c t"), mxT[:], mxT.tok, reads=[mxT.tok])

    def stage_A(b):
        k.op("dve", lambda e: e.scalar_tensor_tensor(out=mcol[:, 0, :], in0=aT[:, 8:16, 16 + b], scalar=1.0, in1=gcol[:, 0, :],
                                                      op0=ALU.add, op1=ALU.mult),
             reads=[aT.tok, cgrp, mcol.tok], writes=[mcol.tok])
        k.op("dve", lambda e: e.tensor_copy(mcol[:, 1, :], aT[:, 0:8, 16 + b]), reads=[aT.tok, mcol.tok], writes=[mcol.tok])
        k.op("pool", lambda e: e.memset(Sf[0][:], 0.0), reads=[Sf[0].tok], writes=[Sf[0].tok])
        k.op("pool", lambda e: e.memset(Sb_[0][:], 0.0), reads=[Sb_[0].tok], writes=[Sb_[0].tok])
        k.op("pool", lambda e: e.memset(GT[:], 0.0), reads=[GT.tok], writes=[GT.tok])
        k.op("pool", lambda e: e.memset(KcT[0:64, :, :], 0.0), reads=[KcT.tok], writes=[KcT.tok])
        k.op("pool", lambda e: e.memset(kvcT[:], 0.0), reads=[kvcT.tok], writes=[kvcT.tok])
        KSUB = int(os.environ.get('KSUB', '9'))
        for st in range(int(os.environ.get('KNST', NST))):
            t0 = st * 512
            for j in range(4):
                x = xt[j % 2]
                k.dma("sp", x[:], xp[b, t0 + j * 128:t0 + (j + 1) * 128, :], x.tok, writes=[x.tok])
                k.op("act", lambda e, x=x, j=j: e.activation(out=xn[:], in_=x[:], func=AF.Square, accum_out=st4[:, j:j + 1]),
                     reads=[x.tok, xn.tok, st4.tok], writes=[xn.tok, st4.tok])
                k.op("dve", lambda e, j=j: e.tensor_scalar(st4[:, 4 + j:5 + j], st4[:, j:j + 1], 1.0 / D, EPS, ALU.mult, ALU.add),
                     reads=[st4.tok], writes=[st4.tok])
                k.op("act", lambda e, j=j: e.activation(out=st4[:, 4 + j:5 + j], in_=st4[:, 4 + j:5 + j], func=AF.Ln), reads=[st4.tok], writes=[st4.tok])
                k.op("act", lambda e, j=j: e.activation(out=st4[:, 8 + j:9 + j], in_=st4[:, 4 + j:5 + j], func=AF.Exp, scale=-0.5), reads=[st4.tok], writes=[st4.tok])
                k.op("dve", lambda e, x=x, j=j: e.tensor_scalar_mul(xn[:], x[:], st4[:, 8 + j:9 + j]),
                     reads=[x.tok, st4.tok, xn.tok], writes=[xn.tok])
                p = psum()
                pb_ = p[:].bitcast(BF16)
                for kc in range(8):
                    tr(pb_[:, kc * 128:(kc + 1) * 128], xn[:, kc * 128:(kc + 1) * 128], identb[:], [xn.tok, identb.tok], [p.tok])
                for kc in range(8):
                    k.op("act", lambda e, kc=kc, pb_=pb_, j=j: e.activation(
                        out=hT[:, kc, j * 128:(j + 1) * 128], in_=pb_[:, kc * 128:(kc + 1) * 128], func=AF.Identity,
                        scale=mcol[:, 0, kc:kc + 1], bias=mcol[:, 1, kc:kc + 1]),
                        reads=[p.tok, mcol.tok, hT.tok], writes=[hT.tok])
            if KSUB < 2:
                continue
            k.dma("pool", qT[64:68, :, :], cd["qaug"][:, :, t0:t0 + 512], qT.tok, reads=[qT.tok], writes=[qT.tok])
            wsl = (st % 2) * 512
            for g in range(2):
                k.dma("pool", KwT[64:68, g, wsl:wsl + 512], cd["kaug"][:, t0:t0 + 512], KwT.tok, reads=[KwT.tok], writes=[KwT.tok])
            for h in range(8):
                p = psum()
                for kc in range(8):
                    mm(p[0:64, :], w_in_t[:, kc, h * 64:(h + 1) * 64], hT[:, kc, :], kc == 0, kc == 7, [hT.tok, cgrp], [p.tok])
                evac_scaled(qT[0:64, h, :], p[0:64, :], 0.125, [p.tok, qT.tok], [qT.tok], "act" if h % 2 else "dve")
            k.op("pool", lambda e: e.tensor_copy(kvcT[:, :, 0:16], kvcT[:, :, 512:528]), reads=[kvcT.tok], writes=[kvcT.tok])
            for g in range(2):
                p = psum()
                for kc in range(8):
                    mm(p[:, :], w_in_t[:, kc, 512 + g * 128:512 + (g + 1) * 128], hT[:, kc, :], kc == 0, kc == 7, [hT.tok, cgrp], [p.tok])
                k.op("dve", lambda e, p=p, g=g: e.tensor_copy(kvcT[:, g, 16:528], p[:, :]), reads=[p.tok, kvcT.tok], writes=[kvcT.tok])
                p = psum()
                for kc in range(8):
                    mm(p[0:64, :], w_in_t[:, kc, 768 + g * 128:768 + g * 128 + 64], hT[:, kc, :], kc == 0, kc == 7, [hT.tok, cgrp], [p.tok])
                k.op("act", lambda e, p=p, g=g: e.copy(KsT[0:64, g, t0:t0 + 512], p[0:64, :]), reads=[p.tok, KsT.tok], writes=[KsT.tok])
                p = psum()
                for kc in range(8):
                    mm(p[0:64, :], w_in_t[:, kc, 1024 + g * 128:1024 + g * 128 + 64], hT[:, kc, :], kc == 0, kc == 7, [hT.tok, cgrp], [p.tok])
                k.op("dve", lambda e, p=p, g=g: e.tensor_copy(KwT[0:64, g, wsl:wsl + 512], p[0:64, :]), reads=[p.tok, KwT.tok], writes=[KwT.tok])
            if KSUB < 3:
                continue
            S0i = 0
            for j in range(4):
                tt = st * 4 + j
                ko = kvo[j % 2]
                for (c0, n, dst) in ((512, 512, 0), (1024, 256, 512)):
                    p = psum()
                    for kc in range(8):
                        mm(p[:, 0:n], hT[:, kc, j * 128:(j + 1) * 128], w_in_t[:, kc, c0:c0 + n], kc == 0, kc == 7, [hT.tok, cgrp], [p.tok])
                    k.op("dve", lambda e, p=p, n=n, dst=dst, ko=ko: e.tensor_copy(ko[:, dst:dst + n], p[:, 0:n]),
                         reads=[p.tok, ko.tok], writes=[ko.tok])
                r0 = t0 + j * 128
                k.dma("sp", pcmp[b, r0:r0 + 128, :], ko[:, 0:256], ko.tok, reads=[ko.tok])
                k.dma("sp", pslc[b, r0:r0 + 128, :], ko[:, 256:512], ko.tok, reads=[ko.tok])
                if st == NST - 1:
                    k.dma("sp", pwin[b, j * 128:(j + 1) * 128, :], ko[:, 512:768], ko.tok, reads=[ko.tok])
                k.op("act", lambda e, ko=ko, tt=tt: e.copy(Vs[:, tt, :, 0:64], ko[:, 256:512].rearrange("p (g k d) -> p g k d", g=2, k=2)[:, :, 1, :]),
                     reads=[ko.tok, Vs.tok], writes=[Vs.tok])
                k.op("act", lambda e, ko=ko, tt=tt: e.copy(Vw[:, tt % 8, :, 0:64], ko[:, 512:768].rearrange("p (g k d) -> p g k d", g=2, k=2)[:, :, 1, :]),
                     reads=[ko.tok, Vw.tok], writes=[Vw.tok])
                p = psum()
                for kc in range(8):
                    mm(p[:, 0:24], hT[:, kc, j * 128:(j + 1) * 128], w_in_t[:, kc, 1280:1304], kc == 0, kc == 7, [hT.tok, cgrp], [p.tok])
                k.op("dve", lambda e, p=p, j=j: e.tensor_copy(gat[:, j, :], p[:, 0:24]), reads=[p.tok, gat.tok], writes=[gat.tok])
                if KSUB >= 4:
                    hgrn_tile(b, st, j, 0)
            if KSUB < 5:
                continue
            n0 = 32 * st - 1 if st > 0 else 0
            n1 = 32 * st + 31
            nn = n1 - n0
            cbase = 0 if st > 0 else 16
            for g in range(2):
                p = psum()
                for pp in range(32):
                    mm(p[:, 0:nn], wphi1t[:, pp, :], kvcT[:, g, cbase + pp:cbase + pp + 16 * (nn - 1) + 1:16], pp == 0, pp == 31,
                       [kvcT.tok, cgrp], [p.tok])
                k.op("act", lambda e, p=p: e.activation(out=GTn[:, 0:nn], in_=p[:, 0:nn], func=AF.Gelu_apprx_tanh, bias=bphi1t[:, 0:1]),
                     reads=[p.tok, cgrp, GTn.tok], writes=[GTn.tok])
                k.op("dve", lambda e, g=g: e.tensor_copy(GT[:, g, n0:n1], GTn[:, 0:nn]), reads=[GTn.tok, GT.tok], writes=[GT.tok])
                p2 = psum()
                mm(p2[0:64, 0:nn], wphi2t[0:64, :], GTn[0:64, 0:nn], True, True, [GTn.tok, cgrp], [p2.tok])
                k.op("dve", lambda e, p2=p2, g=g: e.tensor_copy(KcT[0:64, g, n0:n1], p2[0:64, 0:nn]), reads=[p2.tok, KcT.tok], writes=[KcT.tok])
                for nt in range(2):
                    p3 = psum()
                    mm(p3[:, 0:64], GT[64:128, g, nt * 128:(nt + 1) * 128], wphi2t[64:128, :], True, True, [GT.tok, cgrp], [p3.tok])
                    k.op("dve", lambda e, p3=p3, g=g, nt=nt: e.tensor_copy(Vc[:, nt, g, 0:64], p3[:, 0:64]), reads=[p3.tok, Vc.tok], writes=[Vc.tok])
            if stage >= 2:
                for j in range(4):
                    attention_tile(b, st, j)
        k.dma("sp", phg[b].rearrange("h k v -> k h v"), Sf[0][:], Sf[0].tok, reads=[Sf[0].tok])

    if stage >= 1:
        for b in range(NB):
            stage_A(b)
    k.barrier()
    esA.close()

    esS = ExitStack()
    cgS = k.tok()
    NPG = int(os.environ.get("KNPG", NPAGE))
    NSMP = int(os.environ.get("KNSMP", NS))
    if stage >= 4:
        xs_d = din("xs", [NS, D])
        poolc = din("poolc", [NPOOL * 128, 256])
        pools = din("pools", [NPOOL * 128, 256])
        winb = din("winb", [NS, 512, 256])
        sth = din("sth", [NS, 4, 128, 128])
        ptab = din("ptab", [1, NS * NPAGE], I32)
        ys = dout("ys", [NS, D])
        scmp = dout("scmp", [NS, 256])
        sslc = dout("sslc", [NS, 256])
        swin = dout("swin", [NS, 512, 256])
        shg = dout("shg", [NS, 4, 128, 128])
        efS = sb("efS", [128, PAST], BF16, es=esS)
        pgw = sb("pgw", [128, NPAGE, 8], es=esS)
        ovS = sb("ovS", [128, 4, 128], BF16, es=esS)
        g4 = sb("g4", [128, 32], es=esS)
        eye16 = sb("eye16", [128, 16, 16], es=esS)
        eyem = sb("eyem", [16, 4, 16], es=esS)
        selS = sb("selS", [16, 16, 128], es=esS)
        vm = sb("vm", [128, 8], es=esS)
        sadd = sb("sadd", [32, 128], es=esS)
        iop = sb("iop", [128, 1], es=esS)
        masksS = sb("masksS", [128, 2, 64], es=esS)
        wphi1s = sb("wphi1s", [128, 32, 128], BF16, es=esS)
        wphi2s = sb("wphi2s", [128, 64], BF16, es=esS)
        bphi1s = sb("bphi1s", [128, 1], es=esS)
        g5s = sb("g5s", [128, 4, 512], es=esS)
        omls = sb("omls", [16, 512], es=esS)
        for t_, n_ in ((pgw, "pgw"), (g4, "g4"), (eye16, "eye16"), (eyem, "eyem"), (selS, "selE"), (vm, "vm"), (sadd, "sadd"), (iop, "iop")):
            k.dma("sp", t_[:], cd[n_], cgS, writes=[cgS])
        k.dma("sp", bphi1s[:], bphi1, cgS, writes=[cgS])
        k.dma("sp", g5s[:], g512, cgS, writes=[cgS])
        k.dma("pool", efS[:], cd["efS"], cgS, writes=[cgS])
        k.dma("pool", ovS[:], cd["ovS"], cgS, writes=[cgS])
        k.dma("pool", wphi1s[:], wphi1, cgS, writes=[cgS])
        k.dma("pool", wphi2s[:], wphi2, cgS, writes=[cgS])
        k.op("dve", lambda e: e.tensor_sub(omls[:], g5s[0:16, 2, :], g5s[0:16, 3, :]), reads=[cgS], writes=[omls.tok])
        k.op("act", lambda e: e.activation(out=omls[:], in_=omls[:], func=AF.Exp), reads=[omls.tok], writes=[omls.tok])
        k.op("dve", lambda e: e.tensor_scalar_add(omls[:], omls[:], 1.0), reads=[omls.tok], writes=[omls.tok])
        k.op("dve", lambda e: e.reciprocal(omls[:], omls[:]), reads=[omls.tok], writes=[omls.tok])
        pti = sb("pti", [128, NS * NPAGE], I32, es=esS)
        ptf = sb("ptf", [128, NS * NPAGE], es=esS)
        idx = sb("idx", [128, NS * NPAGE], I32, es=esS)
        k.dma("sp", pti[:], ptab.partition_broadcast(128), pti.tok, writes=[pti.tok])
        k.op("dve", lambda e: e.tensor_copy(ptf[:], pti[:]), reads=[pti.tok], writes=[ptf.tok])
        k.op("dve", lambda e: e.tensor_scalar(ptf[:], ptf[:], 128.0, iop[:, 0:1], ALU.mult, ALU.add), reads=[ptf.tok, cgS], writes=[ptf.tok])
        k.op("dve", lambda e: e.tensor_copy(idx[:], ptf[:]), reads=[ptf.tok], writes=[idx.tok])
        xst = sb("xst", [16, D], es=esS)
        smod = [sb("smod%d" % i, [16, D], es=esS) for i in range(2)]
        hsf = sb("hsf", [16, D], es=esS)
        hsT = sb("hsT", [128, 8, 16], es=esS)
        projs = sb("projs", [16, INC], es=esS)
        sst = sb("sst", [128, 16], es=esS)
        wst = [sb("wst%d" % i, [128, 8, 512], es=esS) for i in range(2)]
        k.dma("sp", xst[:], xs_d, xst.tok, writes=[xst.tok])
        k.dma("sp", smod[0][:], sms[0], smod[0].tok, writes=[smod[0].tok])
        k.dma("sp", smod[1][:], sms[1], smod[1].tok, writes=[smod[1].tok])
        k.op("act", lambda e: e.activation(out=hsf[:], in_=xst[:], func=AF.Square, accum_out=sst[0:16, 0:1]), reads=[xst.tok], writes=[hsf.tok, sst.tok])
        k.op("dve", lambda e: e.tensor_scalar(sst[0:16, 1:2], sst[0:16, 0:1], 1.0 / D, EPS, ALU.mult, ALU.add), reads=[sst.tok], writes=[sst.tok])
        k.op("act", lambda e: e.activation(out=sst[0:16, 1:2], in_=sst[0:16, 1:2], func=AF.Ln), reads=[sst.tok], writes=[sst.tok])
        k.op("act", lambda e: e.activation(out=sst[0:16, 2:3], in_=sst[0:16, 1:2], func=AF.Exp, scale=-0.5), reads=[sst.tok], writes=[sst.tok])
        k.op("dve", lambda e: e.scalar_tensor_tensor(out=hsf[:], in0=xst[:], scalar=sst[0:16, 2:3], in1=smod[0][:], op0=ALU.mult, op1=ALU.mult),
             reads=[xst.tok, sst.tok, smod[0].tok, hsf.tok], writes=[hsf.tok])
        k.op("dve", lambda e: e.tensor_add(hsf[:], hsf[:], smod[1][:]), reads=[hsf.tok, smod[1].tok], writes=[hsf.tok])
        for kc in range(8):
            p = psum()
            tr(p[:, 0:16], hsf[:, kc * 128:(kc + 1) * 128], ident[0:16, 0:16], [hsf.tok, cgrp], [p.tok])
            k.op("dve", lambda e, kc=kc, p=p: e.tensor_copy(hsT[:, kc, :], p[:, 0:16]), reads=[p.tok, hsT.tok], writes=[hsT.tok])
        c0 = 0
        ci = 0
        while c0 < INC:
            n = min(512, INC - c0)
            wt = wst[ci % 2]
            k.dma("sp", wt[:, :, 0:n], w_in[:, c0:c0 + n].rearrange("(c p) n -> p c n", p=128), wt.tok, reads=[wt.tok], writes=[wt.tok])
            p = psum()
            for kc in range(8):
                mm(p[0:16, 0:n], hsT[:, kc, :], wt[:, kc, 0:n], kc == 0, kc == 7, [hsT.tok, wt.tok], [p.tok])
            k.op("dve", lambda e, p=p, c0=c0, n=n: e.tensor_copy(projs[:, c0:c0 + n], p[0:16, 0:n]), reads=[p.tok, projs.tok], writes=[projs.tok])
            c0 += n
            ci += 1
        k.dma("sp", scmp, projs[:, 512:768], projs.tok, reads=[projs.tok])
        k.dma("sp", sslc, projs[:, 768:1024], projs.tok, reads=[projs.tok])
        k.dma("sp", swin[:, 511, :], projs[:, 1024:1280], projs.tok, reads=[projs.tok])
        for s in range(NS):
            k.dma("sp", swin[s, 0:511, :], winb[s, 1:512, :], projs.tok)
        qsT = sb("qsT", [66, 8, 16], BF16, es=esS)
        KnT = sb("KnT", [64, 2, 2, 16], BF16, es=esS)
        Vn = sb("Vn", [16, 2, 2, 65], BF16, es=esS)
        k.dma("pool", qsT[64:66, :, :], cd["qaugS"], qsT.tok, writes=[qsT.tok])
        for h in range(8):
            p = psum()
            tr(p[0:64, 0:16], projs[:, h * 64:(h + 1) * 64], ident[0:16, 0:16], [projs.tok, cgrp], [p.tok])
            k.op("act", lambda e, h=h, p=p: e.activation(out=qsT[0:64, h, :], in_=p[0:64, 0:16], func=AF.Copy, scale=0.125),
                 reads=[p.tok, qsT.tok], writes=[qsT.tok])
        for br in range(2):
            for g in range(2):
                cb_ = 768 + br * 256 + g * 128
                p = psum()
                tr(p[0:64, 0:16], projs[:, cb_:cb_ + 64], ident[0:16, 0:16], [projs.tok, cgrp], [p.tok])
                k.op("dve", lambda e, br=br, g=g, p=p: e.tensor_copy(KnT[:, br, g, :], p[0:64, 0:16]), reads=[p.tok, KnT.tok], writes=[KnT.tok])
                k.op("act", lambda e, br=br, g=g, cb_=cb_: e.copy(Vn[:, br, g, 0:64], projs[:, cb_ + 64:cb_ + 128]), reads=[projs.tok, Vn.tok], writes=[Vn.tok])
        k.op("pool", lambda e: e.memset(Vn[:, :, :, 64:65], 1.0), reads=[Vn.tok], writes=[Vn.tok])
        pvcT, puT, pvsT, pvwT = PS[4], PS[5], PS[6], PS[7]
        bank_first = {4: True, 5: True, 6: True, 7: True}
        for bi_ in range(4, 8):
            k.op("dve", lambda e, bi_=bi_: e.memset(PS[bi_][:, :], 0.0), reads=[PS[bi_].tok], writes=[PS[bi_].tok])

        def acc(bi, out_ap, lhsT, rhs, reads):
            mm(out_ap, lhsT, rhs, bank_first[bi], False, reads, [PS[bi].tok], skip=True)
            bank_first[bi] = False

        def cols(g, s):
            return slice(g * 64 + s, g * 64 + s + 49, 16)

        pgc = [sb("pgc%d" % i, [128, 256], es=esS) for i in range(8)]
        kvcS = sb("kvcS", [128, 2, 16 + 1024], BF16, es=esS)
        GTs = sb("GTs", [128, 2, 512], BF16, es=esS)
        KcTs = sb("KcTs", [66, 2, 512], BF16, es=esS)
        Vcs = sb("Vcs", [128, 4, 2, 65], BF16, es=esS)
        GTn2 = sb("GTn2", [128, 64], BF16, es=esS)
        Pc = [sb("Pc%d" % i, [128, 4], BF16, es=esS) for i in range(8)]
        pci = [0]
        k.dma("pool", KcTs[64:66, 0, :], cd["caugS"], KcTs.tok, writes=[KcTs.tok])
        k.dma("pool", KcTs[64:66, 1, :], cd["caugS"], KcTs.tok, writes=[KcTs.tok])
        k.op("pool", lambda e: e.memset(Vcs[:, :, :, 64:65], 1.0), reads=[Vcs.tok], writes=[Vcs.tok])
        for s in range(NSMP):
            k.op("pool", lambda e: e.memset(GTs[:], 0.0), reads=[GTs.tok], writes=[GTs.tok])
            k.op("pool", lambda e: e.memset(KcTs[0:64, :, :], 0.0), reads=[KcTs.tok], writes=[KcTs.tok])
            k.op("pool", lambda e: e.memset(kvcS[:], 0.0), reads=[kvcS.tok], writes=[kvcS.tok])
            for pg in range(NPG):
                pt_ = pgc[pg % 8]
                c_ = s * NPAGE + pg
                k.idma(pt_[:], poolc, idx[:, c_:c_ + 1], pt_.tok, reads=[idx.tok, pt_.tok], writes=[pt_.tok])
                pl = pg % 8
                for g in range(2):
                    p = psum()
                    tr(p[:, 0:128], pt_[:, g * 128:(g + 1) * 128], ident[:], [pt_.tok, cgrp], [p.tok])
                    if g == 0:
                        k.op("act", lambda e, p=p, g=g, pl=pl: e.copy(kvcS[:, g, 16 + pl * 128:16 + (pl + 1) * 128], p[:, 0:128]), reads=[p.tok, kvcS.tok], writes=[kvcS.tok])
                    else:
                        k.op("dve", lambda e, p=p, g=g, pl=pl: e.tensor_copy(kvcS[:, g, 16 + pl * 128:16 + (pl + 1) * 128], p[:, 0:128]), reads=[p.tok, kvcS.tok], writes=[kvcS.tok])
                if pl == 7 or pg == NPG - 1:
                    pgp = pg // 8
                    n0 = 64 * pgp - 1 if pgp > 0 else 0
                    n1 = 8 * (pg + 1) - 1
                    nn = n1 - n0
                    cbase = 0 if pgp > 0 else 16
                    for g in range(2):
                        p = psum()
                        for pp in range(32):
                            mm(p[:, 0:nn], wphi1s[:, pp, :], kvcS[:, g, cbase + pp:cbase + pp + 16 * (nn - 1) + 1:16], pp == 0, pp == 31, [kvcS.tok, cgS], [p.tok])
                        k.op("act", lambda e, p=p, nn=nn: e.activation(out=GTn2[:, 0:nn], in_=p[:, 0:nn], func=AF.Gelu_apprx_tanh, bias=bphi1s[:, 0:1]),
                             reads=[p.tok, cgS, GTn2.tok], writes=[GTn2.tok])
                        k.op("dve", lambda e, g=g, n0=n0, n1=n1, nn=nn: e.tensor_copy(GTs[:, g, n0:n1], GTn2[:, 0:nn]), reads=[GTn2.tok, GTs.tok], writes=[GTs.tok])
                        p2 = psum()
                        mm(p2[0:64, 0:nn], wphi2s[0:64, :], GTn2[0:64, 0:nn], True, True, [GTn2.tok, cgS], [p2.tok])
                        k.op("dve", lambda e, p2=p2, g=g, n0=n0, n1=n1, nn=nn: e.tensor_copy(KcTs[0:64, g, n0:n1], p2[0:64, 0:nn]), reads=[p2.tok, KcTs.tok], writes=[KcTs.tok])
                    k.op("pool", lambda e: e.tensor_copy(kvcS[:, :, 0:16], kvcS[:, :, 1024:1040]), reads=[kvcS.tok], writes=[kvcS.tok])
            ntl = (8 * NPG - 1 + 127) // 128
            for g in range(2):
                for nt in range(ntl):
                    p3 = psum()
                    mm(p3[:, 0:64], GTs[64:128, g, nt * 128:(nt + 1) * 128], wphi2s[64:128, :], True, True, [GTs.tok, cgS], [p3.tok])
                    k.op("dve", lambda e, p3=p3, g=g, nt=nt: e.tensor_copy(Vcs[:, nt, g, 0:64], p3[:, 0:64]), reads=[p3.tok, Vcs.tok], writes=[Vcs.tok])
                for nt in range(ntl):
                    s_ = psum()
                    mm(s_[:, 0:4], KcTs[:, g, nt * 128:(nt + 1) * 128], qsT[:, 4 * g:4 * g + 4, s], True, True, [KcTs.tok, qsT.tok], [s_.tok])
                    P = Pc[pci[0] % 8]
                    pci[0] += 1
                    k.op("act", lambda e, s_=s_, P=P: e.activation(out=P[:], in_=s_[:, 0:4], func=AF.Exp), reads=[s_.tok, P.tok], writes=[P.tok])
                    k.op("dve", lambda e, P=P, nt=nt: e.tensor_scalar_mul(P[:], P[:], vm[:, nt:nt + 1]), reads=[P.tok, cgS], writes=[P.tok])
                    acc(4, pvcT[0:65, cols(g, s)], Vcs[:, nt, g, :], P[:], [Vcs.tok, P.tok])
                    acc(5, puT[:, cols(g, s)], ovS[:, nt, :], P[:], [cgS, P.tok])
        cT = sb("cT_s", [65, 128], es=esS)
        uT = sb("uT_s", [128, 128], es=esS)
        cTt = sb("cTt", [128, 65], es=esS)
        uTt = sb("uTt", [128, 128], es=esS)
        scS = sb("scS", [32, 4, 128], es=esS)
        m8s = sb("m8s", [32, 16], es=esS)
        selTj = sb("selTj", [128, 32], BF16, es=esS)
        SEL2 = sb("SEL2", [128, NPAGE, 32], es=esS)
        rns = sb("rns", [128, 2], es=esS)
        k.op("act", lambda e: e.copy(cT[:], pvcT[0:65, 0:128]), reads=[pvcT.tok], writes=[cT.tok])
        k.op("dve", lambda e: e.tensor_copy(uT[:], puT[:, 0:128]), reads=[puT.tok], writes=[uT.tok])
        p = psum()
        tr(p[:, 0:65], cT[:], ident[0:65, 0:65], [cT.tok, cgrp], [p.tok])
        k.op("dve", lambda e: e.tensor_copy(cTt[:], p[:, 0:65]), reads=[p.tok], writes=[cTt.tok])
        p = psum()
        tr(p[:, 0:128], uT[:], ident[:], [uT.tok, cgrp], [p.tok])
        k.op("dve", lambda e: e.tensor_scalar_max(rns[:, 0:1], cTt[:, 64:65], 1e-30), reads=[cTt.tok], writes=[rns.tok])
        k.op("dve", lambda e: e.reciprocal(rns[:, 1:2], rns[:, 0:1]), reads=[rns.tok], writes=[rns.tok])
        k.op("dve", lambda e: e.tensor_scalar_mul(uTt[:], p[:, 0:128], rns[:, 1:2]), reads=[p.tok, rns.tok], writes=[uTt.tok])
        p = psum()
        mm(p[0:32, 0:128], g4[:], uTt[:], True, True, [uTt.tok, cgS], [p.tok])
        k.op("dve", lambda e: e.tensor_add(scS[:, 0, :], p[0:32, 0:128], sadd[:]), reads=[p.tok, cgS], writes=[scS.tok])
        k.op("dve", lambda e: e.max(m8s[:, 0:8], scS[:, 0, :]), reads=[scS.tok], writes=[m8s.tok])
        k.op("dve", lambda e: e.match_replace(scS[:, 1, :], m8s[:, 0:8], scS[:, 0, :], -1e9), reads=[scS.tok, m8s.tok], writes=[scS.tok])
        k.op("dve", lambda e: e.max(m8s[:, 8:16], scS[:, 1, :]), reads=[scS.tok, m8s.tok], writes=[m8s.tok])
        k.op("dve", lambda e: e.tensor_scalar(scS[:, 2, :], scS[:, 0, :], m8s[:, 14:15], None, ALU.is_ge), reads=[scS.tok, m8s.tok], writes=[scS.tok])
        p = psum()
        tr(p[:, 0:32], scS[:, 2, :], ident[0:32, 0:32], [scS.tok, cgrp], [p.tok])
        k.op("dve", lambda e: e.tensor_copy(selTj[:], p[:, 0:32]), reads=[p.tok], writes=[selTj.tok])
        for pg in range(NPG):
            p = psum()
            mm(p[:, 0:32], efS[:, pg * 128:(pg + 1) * 128], selTj[:], True, True, [selTj.tok, cgS], [p.tok])
            k.op("act" if pg % 2 else "dve", (lambda e, p=p, pg=pg: e.copy(SEL2[:, pg, :], p[:, 0:32])) if pg % 2 else
                 (lambda e, p=p, pg=pg: e.tensor_copy(SEL2[:, pg, :], p[:, 0:32])), reads=[p.tok, SEL2.tok], writes=[SEL2.tok])
        KpT = [sb("KpT%d" % i, [66, 2, 128], BF16, es=esS) for i in range(4)]
        Vp = [sb("Vp%d" % i, [128, 2, 65], BF16, es=esS) for i in range(4)]
        for i in range(4):
            k.dma("pool", KpT[i][64:66, 0, :], cd["paugS"], KpT[i].tok, writes=[KpT[i].tok])
            k.dma("pool", KpT[i][64:66, 1, :], cd["paugS"], KpT[i].tok, writes=[KpT[i].tok])
            k.op("pool", lambda e, i=i: e.memset(Vp[i][:, :, 64:65], 1.0), reads=[Vp[i].tok], writes=[Vp[i].tok])
        it = 0
        for s in range(NSMP):
            for pg in range(NPG):
                pt_ = pgc[it % 8]
                kp = KpT[it % 4]
                vp = Vp[it % 4]
                it += 1
                c_ = s * NPAGE + pg
                k.idma(pt_[:], pools, idx[:, c_:c_ + 1], pt_.tok, reads=[idx.tok, pt_.tok], writes=[pt_.tok])
                for g in range(2):
                    p = psum()
                    tr(p[0:64, 0:128], pt_[:, g * 128:g * 128 + 64], ident[:], [pt_.tok, cgrp], [p.tok])
                    k.op("act", lambda e, p=p, g=g, kp=kp: e.copy(kp[0:64, g, :], p[0:64, 0:128]), reads=[p.tok, kp.tok], writes=[kp.tok])
                k.op("dve", lambda e, pt_=pt_, vp=vp: e.tensor_copy(vp[:, :, 0:64], pt_[:].rearrange("p (g k d) -> p g k d", g=2, k=2)[:, :, 1, :]),
                     reads=[pt_.tok, vp.tok], writes=[vp.tok])
                for g in range(2):
                    s_ = psum()
                    mm(s_[:, 0:4], kp[:, g, :], qsT[:, 4 * g:4 * g + 4, s], True, True, [kp.tok, qsT.tok], [s_.tok])
                    P = Pc[pci[0] % 8]
                    pci[0] += 1
                    k.op("act", lambda e, s_=s_, P=P: e.activation(out=P[:], in_=s_[:, 0:4], func=AF.Exp), reads=[s_.tok, P.tok], writes=[P.tok])
                    k.op("dve", lambda e, P=P, pg=pg, g=g, s=s: e.scalar_tensor_tensor(out=P[:], in0=P[:], scalar=SEL2[:, pg, 2 * s + g:2 * s + g + 1],
                                                                                       in1=pgw[:, pg, 4 * g:4 * g + 4], op0=ALU.mult, op1=ALU.mult),
                         reads=[P.tok, SEL2.tok, cgS], writes=[P.tok])
                    acc(6, pvsT[0:65, cols(g, s)], vp[:, g, :], P[:], [vp.tok, P.tok])
        wbt = sb("wbt", [128, 4, 256], es=esS)
        KwTs = sb("KwTs", [66, 2, 512], BF16, es=esS)
        Vws = sb("Vws", [128, 4, 2, 65], BF16, es=esS)
        k.dma("pool", KwTs[64:66, 0, :], cd["waugS"], KwTs.tok, writes=[KwTs.tok])
        k.dma("pool", KwTs[64:66, 1, :], cd["waugS"], KwTs.tok, writes=[KwTs.tok])
        k.op("pool", lambda e: e.memset(Vws[:, :, :, 64:65], 1.0), reads=[Vws.tok], writes=[Vws.tok])
        for s in range(NSMP):
            k.dma("sp", wbt[:], winb[s].rearrange("(t p) c -> p t c", p=128), wbt.tok, reads=[wbt.tok], writes=[wbt.tok])
            for t in range(4):
                for g in range(2):
                    p = psum()
                    tr(p[0:64, 0:128], wbt[:, t, g * 128:g * 128 + 64], ident[:], [wbt.tok, cgrp], [p.tok])
                    k.op("act", lambda e, p=p, g=g, t=t: e.copy(KwTs[0:64, g, t * 128:(t + 1) * 128], p[0:64, 0:128]), reads=[p.tok, KwTs.tok], writes=[KwTs.tok])
                k.op("dve", lambda e, t=t: e.tensor_copy(Vws[:, t, :, 0:64], wbt[:, t, :].rearrange("p (g k d) -> p g k d", g=2, k=2)[:, :, 1, :]),
                     reads=[wbt.tok, Vws.tok], writes=[Vws.tok])
            for g in range(2):
                for t in range(4):
                    s_ = psum()
                    mm(s_[:, 0:4], KwTs[:, g, t * 128:(t + 1) * 128], qsT[:, 4 * g:4 * g + 4, s], True, True, [KwTs.tok, qsT.tok], [s_.tok])
                    P = Pc[pci[0] % 8]
                    pci[0] += 1
                    k.op("act", lambda e, s_=s_, P=P: e.activation(out=P[:], in_=s_[:, 0:4], func=AF.Exp), reads=[s_.tok, P.tok], writes=[P.tok])
                    if t == 0:
                        k.op("dve", lambda e, P=P: e.tensor_scalar_mul(P[:], P[:], vm[:, 4:5]), reads=[P.tok, cgS], writes=[P.tok])
                    acc(7, pvwT[0:65, cols(g, s)], Vws[:, t, g, :], P[:], [Vws.tok, P.tok])
        Pn = sb("Pn", [16, 4, 16], BF16, es=esS)
        for br, bi, bank in ((0, 6, pvsT), (1, 7, pvwT)):
            for g in range(2):
                s_ = psum()
                mm(s_[0:16, 0:64], KnT[:, br, g, :], qsT[0:64, 4 * g:4 * g + 4, :], True, True, [KnT.tok, qsT.tok], [s_.tok])
                k.op("act", lambda e, s_=s_: e.activation(out=Pn[:], in_=s_[0:16, 0:64].rearrange("p (h s) -> p h s", h=4), func=AF.Exp),
                     reads=[s_.tok, Pn.tok], writes=[Pn.tok])
                k.op("dve", lambda e: e.tensor_mul(Pn[:], Pn[:], eyem[:]), reads=[Pn.tok, cgS], writes=[Pn.tok])
                acc(bi, bank[0:65, g * 64:(g + 1) * 64], Vn[:, br, g, :], Pn[:].rearrange("p h s -> p (h s)"), [Vn.tok, Pn.tok])
        pvt = sb("pvt", [16, 3, 8, 65], es=esS)
        xT = sb("xT_s", [65, 128], es=esS)
        for x, bank in ((0, pvcT), (1, pvsT), (2, pvwT)):
            if x == 0:
                src = cT
            else:
                k.op("act", lambda e, bank=bank: e.copy(xT[:], bank[0:65, 0:128]), reads=[bank.tok, xT.tok], writes=[xT.tok])
                src = xT
            for hh in range(2):
                p = psum()
                for h4 in range(4):
                    h8 = hh * 4 + h4
                    tr(p[0:16, h4 * 65:(h4 + 1) * 65], src[:, h8 * 16:(h8 + 1) * 16], ident[0:65, 0:65], [src.tok, cgrp], [p.tok])
                k.op("dve", lambda e, x=x, p=p, hh=hh: e.tensor_copy(pvt[:, x, hh * 4:hh * 4 + 4, :], p[0:16, 0:260].rearrange("p (h d) -> p h d", h=4)),
                     reads=[p.tok, pvt.tok], writes=[pvt.tok])
        sgs = sb("sgs", [16, 24], es=esS)
        rinS = sb("rinS", [16, 3, 8], es=esS)
        facS = sb("facS", [16, 3, 8], es=esS)
        onS = sb("onS", [16, 8, 64], es=esS)
        tmS = sb("tmS", [16, 8, 64], es=esS)
        mixS = sb("mixS", [16, D], es=esS)
        k.op("act", lambda e: e.activation(out=sgs[:], in_=projs[:, 1280:1304], func=AF.Exp, scale=-1.0), reads=[projs.tok], writes=[sgs.tok])
        k.op("dve", lambda e: e.tensor_scalar_add(sgs[:], sgs[:], 1.0), reads=[sgs.tok], writes=[sgs.tok])
        k.op("dve", lambda e: e.reciprocal(sgs[:], sgs[:]), reads=[sgs.tok], writes=[sgs.tok])
        k.op("dve", lambda e: e.tensor_scalar_max(rinS[:], pvt[:, :, :, 64], 1e-30), reads=[pvt.tok], writes=[rinS.tok])
        k.op("dve", lambda e: e.reciprocal(rinS[:], rinS[:]), reads=[rinS.tok], writes=[rinS.tok])
        k.op("dve", lambda e: e.tensor_mul(facS[:], rinS[:], sgs[:].rearrange("p (h x) -> p x h", x=3)), reads=[rinS.tok, sgs.tok], writes=[facS.tok])
        for x in range(3):
            fb = facS[:, x, :].unsqueeze(2).to_broadcast([16, 8, 64])
            if x == 0:
                k.op("dve", lambda e, fb=fb: e.tensor_mul(onS[:], pvt[:, 0, :, 0:64], fb), reads=[pvt.tok, facS.tok], writes=[onS.tok])
            else:
                k.op("dve", lambda e, fb=fb, x=x: e.tensor_mul(tmS[:], pvt[:, x, :, 0:64], fb), reads=[pvt.tok, facS.tok, tmS.tok], writes=[tmS.tok])
                k.op("dve", lambda e: e.tensor_add(onS[:], onS[:], tmS[:]), reads=[onS.tok, tmS.tok], writes=[onS.tok])
        onf = onS[:].rearrange("p h d -> p (h d)")
        k.op("act", lambda e: e.activation(out=tmS[:].rearrange("p h d -> p (h d)"), in_=onf, func=AF.Square, accum_out=sst[0:16, 4:5]),
             reads=[onS.tok, tmS.tok, sst.tok], writes=[tmS.tok, sst.tok])
        k.op("dve", lambda e: e.tensor_scalar(sst[0:16, 5:6], sst[0:16, 4:5], 1.0 / 512, EPS, ALU.mult, ALU.add), reads=[sst.tok], writes=[sst.tok])
        k.op("act", lambda e: e.activation(out=sst[0:16, 5:6], in_=sst[0:16, 5:6], func=AF.Ln), reads=[sst.tok], writes=[sst.tok])
        k.op("act", lambda e: e.activation(out=sst[0:16, 6:7], in_=sst[0:16, 5:6], func=AF.Exp, scale=-0.5), reads=[sst.tok], writes=[sst.tok])
        k.op("dve", lambda e: e.scalar_tensor_tensor(out=mixS[:, 0:512], in0=onf, scalar=sst[0:16, 6:7], in1=g5s[0:16, 0, :], op0=ALU.mult, op1=ALU.mult),
             reads=[onS.tok, sst.tok, cgS, mixS.tok], writes=[mixS.tok])
        e1 = sb("e1s", [16, 512], es=esS)
        qs_ = sb("qs_", [16, 512], es=esS)
        ks_ = sb("ks_", [16, 512], es=esS)
        fs_ = sb("fs_", [16, 512], es=esS)
        sos = sb("sos", [16, 512], es=esS)
        colT = sb("colT", [128, 3, 4, 16], es=esS)
        qTm = sb("qTm", [128, 4, 16, 16], es=esS)
        Sst = [sb("Sst%d" % i, [128, 4, 128], es=esS) for i in range(2)]
        Snw = [sb("Snw%d" % i, [128, 4, 128], es=esS) for i in range(2)]
        tmv = sb("tmv", [128, 128], es=esS)
        HQ, HF, HI, HO = 1304, 1816, 2328, 2840

        def sigm(dst, c0):
            k.op("act", lambda e: e.activation(out=e1[:], in_=projs[:, c0:c0 + 512], func=AF.Exp, scale=-1.0), reads=[projs.tok, e1.tok], writes=[e1.tok])
            k.op("dve", lambda e: e.tensor_scalar_add(dst[:], e1[:], 1.0), reads=[e1.tok, dst.tok], writes=[dst.tok])
            k.op("dve", lambda e: e.reciprocal(dst[:], dst[:]), reads=[dst.tok], writes=[dst.tok])
        sigm(qs_, HQ)
        k.op("dve", lambda e: e.tensor_mul(qs_[:], qs_[:], projs[:, HQ:HQ + 512]), reads=[qs_.tok, projs.tok], writes=[qs_.tok])
        sigm(ks_, HF)
        k.op("dve", lambda e: e.tensor_mul(ks_[:], ks_[:], e1[:]), reads=[ks_.tok, e1.tok], writes=[ks_.tok])
        k.op("dve", lambda e: e.tensor_mul(ks_[:], ks_[:], omls[:]), reads=[ks_.tok, omls.tok], writes=[ks_.tok])
        k.op("dve", lambda e: e.tensor_scalar(fs_[:], ks_[:], -1.0, 1.0, ALU.mult, ALU.add), reads=[ks_.tok, fs_.tok], writes=[fs_.tok])
        sigm(sos, HO)
        k.op("dve", lambda e: e.tensor_mul(sos[:], sos[:], projs[:, HO:HO + 512]), reads=[sos.tok, projs.tok], writes=[sos.tok])
        for qi, src in enumerate((qs_, ks_, fs_)):
            for h in range(4):
                p = psum()
                tr(p[:, 0:16], src[:, h * 128:(h + 1) * 128], ident[0:16, 0:16], [src.tok, cgrp], [p.tok])
                k.op("dve", lambda e, qi=qi, h=h, p=p: e.tensor_copy(colT[:, qi, h, :], p[:, 0:16]), reads=[p.tok, colT.tok], writes=[colT.tok])
        for h in range(4):
            k.op("dve", lambda e, h=h: e.tensor_mul(qTm[:, h, :, :], eye16[:], colT[:, 0, h, :].unsqueeze(2).to_broadcast([128, 16, 16])),
                 reads=[colT.tok, cgS, qTm.tok], writes=[qTm.tok])
        pO = PS[4]
        first = True
        for s in range(NS):
            Sin, Sout = Sst[s % 2], Snw[s % 2]
            k.dma("sp", Sin[:], sth[s].rearrange("h k v -> k h v"), Sin.tok, reads=[Sin.tok], writes=[Sin.tok])
            pv_ = psum()
            mm(pv_[:, :], selS[:, s, :], projs[:, HI:HI + 512], True, True, [cgS, projs.tok], [pv_.tok])
            for h in range(4):
                k.op("dve", lambda e, h=h, s=s, pv_=pv_: e.tensor_scalar_mul(tmv[:], pv_[:, h * 128:(h + 1) * 128], colT[:, 1, h, s:s + 1]),
                     reads=[pv_.tok, colT.tok, tmv.tok], writes=[tmv.tok])
                k.op("dve", lambda e, h=h, s=s, Sin=Sin, Sout=Sout: e.scalar_tensor_tensor(out=Sout[:, h, :], in0=Sin[:, h, :], scalar=colT[:, 2, h, s:s + 1],
                                                                                            in1=tmv[:], op0=ALU.mult, op1=ALU.add),
                     reads=[Sin.tok, colT.tok, tmv.tok, Sout.tok], writes=[Sout.tok])
            k.dma("sp", shg[s].rearrange("h k v -> k h v"), Sout[:], Sout.tok, reads=[Sout.tok])
            for h in range(4):
                mm(pO[0:16, h * 128:(h + 1) * 128], qTm[:, h, s, :], Sout[:, h, :], first, False, [qTm.tok, Sout.tok], [pO.tok], skip=True)
                first = False
        for h in range(4):
            k.op("act", lambda e, h=h: e.activation(out=e1[:, h * 128:(h + 1) * 128], in_=pO[0:16, h * 128:(h + 1) * 128], func=AF.Square,
                                                    accum_out=sst[0:16, 8 + h:9 + h]),
                 reads=[pO.tok, e1.tok, sst.tok], writes=[e1.tok, sst.tok])
        k.op("dve", lambda e: e.tensor_scalar(sst[0:16, 8:12], sst[0:16, 8:12], 1.0 / 128, EPS, ALU.mult, ALU.add), reads=[sst.tok], writes=[sst.tok])
        k.op("act", lambda e: e.activation(out=sst[0:16, 8:12], in_=sst[0:16, 8:12], func=AF.Ln), reads=[sst.tok], writes=[sst.tok])
        k.op("act", lambda e: e.activation(out=sst[0:16, 12:16], in_=sst[0:16, 8:12], func=AF.Exp, scale=-0.5), reads=[sst.tok], writes=[sst.tok])
        k.op("dve", lambda e: e.tensor_mul(e1[:].rearrange("p (h v) -> p h v", h=4), pO[0:16, :].rearrange("p (h v) -> p h v", h=4),
                                           sst[0:16, 12:16].unsqueeze(2).to_broadcast([16, 4, 128])),
             reads=[pO.tok, sst.tok, e1.tok], writes=[e1.tok])
        k.op("dve", lambda e: e.tensor_mul(e1[:], e1[:], g5s[0:16, 1, :]), reads=[e1.tok, cgS], writes=[e1.tok])
        k.op("dve", lambda e: e.tensor_mul(mixS[:, 512:1024], e1[:], sos[:]), reads=[e1.tok, sos.tok, mixS.tok], writes=[mixS.tok])
        mxS = sb("mxS", [128, 8, 128], BF16, es=esS)
        k.op("pool", lambda e: e.memset(mxS[:], 0.0), reads=[mxS.tok], writes=[mxS.tok])
        for kc in range(8):
            p = psum()
            tr(p[:, 0:16], mixS[:, kc * 128:(kc + 1) * 128], ident[0:16, 0:16], [mixS.tok, cgrp], [p.tok])
            k.op("dve", lambda e, kc=kc, p=p: e.tensor_copy(mxS[:, kc, 0:16], p[:, 0:16]), reads=[p.tok, mxS.tok], writes=[mxS.tok])
        k.dma("sp", mxs[:, :, NB * SEQ:NB * SEQ + 128].rearrange("c p t -> p c t"), mxS[:], mxS.tok, reads=[mxS.tok])
    k.barrier()
    esS.close()

    esB = ExitStack()
    cgB = k.tok()
    w_out_t = sb("w_out_t", [128, 8, D], BF16, es=esB)
    wrt = sb("wrt", [128, 8, 20], es=esB)
    brt = sb("brt", [128, 20], es=esB)
    selE = sb("selE", [16, 16, 128], es=esB)
    for kc in range(8):
        k.dma("pool", w_out_t[:, kc, :], w_out[kc * 128:(kc + 1) * 128, :], cgB, writes=[cgB])
    k.dma("sp", wrt[:], wroute.rearrange("(c p) n -> p c n", p=128), cgB, writes=[cgB])
    k.dma("sp", brt[:], broute, cgB, writes=[cgB])
    k.dma("sp", selE[:], cd["selE"], cgB, writes=[cgB])
    gv1t = sb("gv1t", [128, D], es=esB)
    gv2t = sb("gv2t", [128, D], es=esB)
    mcol2 = sb("mcol2", [128, 2, 8], es=esB)
    mT = sb("mT", [128, 8, 128], BF16, es=esB)
    xbt = sb("xbt", [128, D], es=esB)
    x1t = sb("x1t", [128, D], es=esB)
    xn2 = sb("xn2", [128, D], es=esB)
    h2Tf = sb("h2Tf", [128, 8, 128], es=esB)
    h2T = sb("h2T", [128, 8, MB], BF16, es=esB)
    combT = sb("combT", [16, MB], es=esB)
    yacc = sb("yacc", [128, MB // 128, D], es=esB)
    WGs = [sb("WG%d" % i, [128, 8, 4, 256], BF16, es=esB) for i in range(2)]
    WUs = [sb("WU%d" % i, [128, 8, 4, 256], BF16, es=esB) for i in range(2)]
    wsel = [0]
    WD = sb("WD", [128, 4, 2, D], BF16, es=esB)
    actT = sb("actT", [128, 4, 2, 512], BF16, es=esB)
    cbt = sb("cbt", [128, 512], es=esB)
    sil = sb("sil", [128, 512], es=esB)
    rt = sb("rt", [128, 16], es=esB)
    lg = sb("lg", [128, 20], es=esB)
    ohg = sb("ohg", [128, 4], es=esB)
    t16 = sb("t16", [128, 4, 4], es=esB)
    leg = sb("leg", [128, 3, 4], es=esB)
    oh12 = sb("oh12", [128, 2, 4], es=esB)
    c16 = sb("c16", [128, 4, 4], es=esB)
    ssb = sb("ssb", [128, 8], es=esB)
    print("phase B sbuf left:", nc.sbuf_bytes_remaining)

    def rstd_of(dst, src, n):
        k.op("dve", lambda e: e.tensor_scalar(dst, src, 1.0 / n, EPS, ALU.mult, ALU.add), reads=[ssb.tok], writes=[ssb.tok])
        k.op("act", lambda e: e.activation(out=dst, in_=dst, func=AF.Ln), reads=[ssb.tok], writes=[ssb.tok])
        k.op("act", lambda e: e.activation(out=dst, in_=dst, func=AF.Exp, scale=-0.5), reads=[ssb.tok], writes=[ssb.tok])

    def routing(t):
        pl = psum()
        for kc in range(8):
            mm(pl[:, 0:20], h2Tf[:, kc, :], wrt[:, kc, :], kc == 0, kc == 7, [h2Tf.tok, cgB], [pl.tok])
        k.op("dve", lambda e: e.tensor_add(lg[:], pl[:, 0:20], brt[:]), reads=[pl.tok, cgB, lg.tok], writes=[lg.tok])
        R = lambda i: rt[:, i:i + 1]
        k.op("dve", lambda e: e.reduce_max(R(0), lg[:, 0:4], axis=AX.X), reads=[lg.tok, rt.tok], writes=[rt.tok])
        k.op("dve", lambda e: e.tensor_scalar_mul(R(1), R(0), -1.0), reads=[rt.tok], writes=[rt.tok])
        k.op("act", lambda e: e.activation(out=ohg[:], in_=lg[:, 0:4], func=AF.Exp, bias=R(1), accum_out=R(2)),
             reads=[lg.tok, rt.tok, ohg.tok], writes=[ohg.tok, rt.tok])
        k.op("dve", lambda e: e.reciprocal(R(3), R(2)), reads=[rt.tok], writes=[rt.tok])
        k.op("dve", lambda e: e.tensor_scalar(ohg[:], lg[:, 0:4], R(0), None, ALU.is_equal), reads=[lg.tok, rt.tok, ohg.tok], writes=[ohg.tok])
        for g in range(4):
            k.op("dve", lambda e, g=g: e.tensor_scalar_mul(t16[:, g, :], lg[:, 4 + 4 * g:8 + 4 * g], ohg[:, g:g + 1]),
                 reads=[lg.tok, ohg.tok, t16.tok], writes=[t16.tok])
        k.op("dve", lambda e: e.tensor_add(leg[:, 0, :], t16[:, 0, :], t16[:, 1, :]), reads=[t16.tok, leg.tok], writes=[leg.tok])
        k.op("dve", lambda e: e.tensor_add(leg[:, 0, :], leg[:, 0, :], t16[:, 2, :]), reads=[t16.tok, leg.tok], writes=[leg.tok])
        k.op("dve", lambda e: e.tensor_add(leg[:, 0, :], leg[:, 0, :], t16[:, 3, :]), reads=[t16.tok, leg.tok], writes=[leg.tok])
        k.op("dve", lambda e: e.reduce_max(R(4), leg[:, 0, :], axis=AX.X), reads=[leg.tok, rt.tok], writes=[rt.tok])
        k.op("dve", lambda e: e.tensor_scalar(oh12[:, 0, :], leg[:, 0, :], R(4), None, ALU.is_equal), reads=[leg.tok, rt.tok, oh12.tok], writes=[oh12.tok])
        k.op("dve", lambda e: e.scalar_tensor_tensor(out=leg[:, 1, :], in0=oh12[:, 0, :], scalar=-1e9, in1=leg[:, 0, :], op0=ALU.mult, op1=ALU.add),
             reads=[oh12.tok, leg.tok], writes=[leg.tok])
        k.op("dve", lambda e: e.reduce_max(R(5), leg[:, 1, :], axis=AX.X), reads=[leg.tok, rt.tok], writes=[rt.tok])
        k.op("dve", lambda e: e.tensor_scalar(oh12[:, 1, :], leg[:, 1, :], R(5), None, ALU.is_equal), reads=[leg.tok, rt.tok, oh12.tok], writes=[oh12.tok])
        k.op("dve", lambda e: e.tensor_sub(R(6), R(5), R(4)), reads=[rt.tok], writes=[rt.tok])
        k.op("act", lambda e: e.activation(out=R(7), in_=R(6), func=AF.Exp), reads=[rt.tok], writes=[rt.tok])
        k.op("dve", lambda e: e.tensor_scalar_add(R(8), R(7), 1.0), reads=[rt.tok], writes=[rt.tok])
        k.op("dve", lambda e: e.reciprocal(R(9), R(8)), reads=[rt.tok], writes=[rt.tok])
        k.op("dve", lambda e: e.tensor_mul(R(10), R(9), R(3)), reads=[rt.tok], writes=[rt.tok])
        k.op("dve", lambda e: e.tensor_mul(R(11), R(10), R(7)), reads=[rt.tok], writes=[rt.tok])
        k.op("dve", lambda e: e.tensor_scalar_mul(leg[:, 2, :], oh12[:, 0, :], R(10)), reads=[oh12.tok, rt.tok, leg.tok], writes=[leg.tok])
        k.op("dve", lambda e: e.scalar_tensor_tensor(out=leg[:, 2, :], in0=oh12[:, 1, :], scalar=R(11), in1=leg[:, 2, :], op0=ALU.mult, op1=ALU.add),
             reads=[oh12.tok, rt.tok, leg.tok], writes=[leg.tok])
        for g in range(4):
            k.op("dve", lambda e, g=g: e.tensor_scalar_mul(c16[:, g, :], leg[:, 2, :], ohg[:, g:g + 1]),
                 reads=[leg.tok, ohg.tok, c16.tok], writes=[c16.tok])
        pc = psum()
        tr(pc[0:16, 0:128], c16[:].rearrange("p g e -> p (g e)"), ident[:], [c16.tok, cgrp], [pc.tok])
        k.op("act", lambda e: e.copy(combT[:, t * 128:(t + 1) * 128], pc[0:16, 0:128]), reads=[pc.tok, combT.tok], writes=[combT.tok])

    def batch_B(b, g0, xsrc, ydst, ntile, sample=False):
        for t in range(ntile):
            r0 = g0 + t * 128
            k.dma("sp", mT[:], mxs[:, :, r0:r0 + 128].rearrange("c p t -> p c t"), mT.tok, reads=[mT.tok], writes=[mT.tok])
            if sample:
                k.op("pool", lambda e: e.memset(xbt[:], 0.0), reads=[xbt.tok], writes=[xbt.tok])
                k.dma("sp", xbt[0:16, :], xsrc(t), xbt.tok, reads=[xbt.tok], writes=[xbt.tok])
            else:
                k.dma("sp", xbt[:], xsrc(t), xbt.tok, reads=[xbt.tok], writes=[xbt.tok])
            pm = (PS[6], PS[7])
            for half in range(2):
                for kc in range(8):
                    mm(pm[half][:, :], mT[:, kc, :], w_out_t[:, kc, half * 512:(half + 1) * 512], kc == 0, kc == 7, [mT.tok, cgB], [pm[half].tok])
                k.op("act", lambda e, half=half: e.activation(out=xn2[:, half * 512:(half + 1) * 512], in_=pm[half][:, :], func=AF.Square,
                                                              accum_out=ssb[:, half:half + 1]),
                     reads=[pm[half].tok, xn2.tok, ssb.tok], writes=[xn2.tok, ssb.tok])
            k.op("dve", lambda e: e.tensor_add(ssb[:, 2:3], ssb[:, 0:1], ssb[:, 1:2]), reads=[ssb.tok], writes=[ssb.tok])
            rstd_of(ssb[:, 3:4], ssb[:, 2:3], D)
            for half in range(2):
                hs = slice(half * 512, (half + 1) * 512)
                k.op("dve", lambda e, half=half, hs=hs: e.scalar_tensor_tensor(out=x1t[:, hs], in0=pm[half][:, :], scalar=ssb[:, 3:4], in1=gv1t[:, hs],
                                                                                op0=ALU.mult, op1=ALU.mult),
                     reads=[pm[half].tok, ssb.tok, gv1t.tok, x1t.tok], writes=[x1t.tok])
            k.op("dve", lambda e: e.tensor_add(x1t[:], x1t[:], xbt[:]), reads=[x1t.tok, xbt.tok], writes=[x1t.tok])
            k.dma("sp", x1s[r0:r0 + 128, :], x1t[:], x1t.tok, reads=[x1t.tok])
            k.op("act", lambda e: e.activation(out=xn2[:], in_=x1t[:], func=AF.Square, accum_out=ssb[:, 4:5]),
                 reads=[x1t.tok, xn2.tok, ssb.tok], writes=[xn2.tok, ssb.tok])
            rstd_of(ssb[:, 5:6], ssb[:, 4:5], D)
            k.op("dve", lambda e: e.tensor_scalar_mul(xn2[:], x1t[:], ssb[:, 5:6]), reads=[x1t.tok, ssb.tok, xn2.tok], writes=[xn2.tok])
            if sample:
                k.op("dve", lambda e: e.tensor_mul(xn2[0:16, :], xn2[0:16, :], yacc[0:16, 1, :]), reads=[xn2.tok, yacc.tok], writes=[xn2.tok])
                k.op("dve", lambda e: e.tensor_add(xn2[0:16, :], xn2[0:16, :], yacc[0:16, 2, :]), reads=[xn2.tok, yacc.tok], writes=[xn2.tok])
            for hh in range(2):
                p = psum()
                for c4 in range(4):
                    kc = hh * 4 + c4
                    tr(p[:, c4 * 128:(c4 + 1) * 128], xn2[:, kc * 128:(kc + 1) * 128], ident[:], [xn2.tok, cgrp], [p.tok])
                for c4 in range(4):
                    kc = hh * 4 + c4
                    k.op("act", lambda e, kc=kc, c4=c4, p=p: e.activation(out=h2Tf[:, kc, :], in_=p[:, c4 * 128:(c4 + 1) * 128], func=AF.Identity,
                                                                          scale=mcol2[:, 0, kc:kc + 1], bias=mcol2[:, 1, kc:kc + 1]),
                         reads=[p.tok, mcol2.tok, h2Tf.tok], writes=[h2Tf.tok])
            k.op("dve", lambda e, t=t: e.tensor_copy(h2T[:, :, t * 128:(t + 1) * 128], h2Tf[:]), reads=[h2Tf.tok, h2T.tok], writes=[h2T.tok])
            routing(t)
        nsub = (ntile + 3) // 4
        for g in range(4):
            WG = WGs[wsel[0] % 2]
            WU = WUs[wsel[0] % 2]
            wsel[0] += 1
            for e_ in range(4):
                E = 4 * g + e_
                k.dma("pool", WG[:, :, e_, :], wg_d[E].rearrange("(c p) f -> p c f", p=128), WG.tok, reads=[WG.tok], writes=[WG.tok])
                k.dma("pool", WU[:, :, e_, :], wu_d[E].rearrange("(c p) f -> p c f", p=128), WU.tok, reads=[WU.tok], writes=[WU.tok])
                k.dma("pool", WD[:, e_, :, :], wd_d[E].rearrange("(c p) n -> p c n", p=128), WD.tok, reads=[WD.tok], writes=[WD.tok])
            for sub in range(nsub):
                nt_sub = min(4, ntile - sub * 4)
                ncol = nt_sub * 128
                cs = slice(sub * 512, sub * 512 + ncol)
                for e_ in range(4):
                    E = 4 * g + e_
                    pc = psum()
                    mm(pc[:, 0:ncol], selE[:, E, :], combT[:, cs], True, True, [cgB, combT.tok], [pc.tok])
                    k.op("act", lambda e, pc=pc: e.copy(cbt[:, 0:ncol], pc[:, 0:ncol]), reads=[pc.tok, cbt.tok], writes=[cbt.tok])
                    for fc in range(2):
                        pg = psum()
                        for kc in range(8):
                            mm(pg[:, 0:ncol], WG[:, kc, e_, fc * 128:(fc + 1) * 128], h2T[:, kc, cs], kc == 0, kc == 7, [WG.tok, h2T.tok], [pg.tok])
                        pu = psum()
                        for kc in range(8):
                            mm(pu[:, 0:ncol], WU[:, kc, e_, fc * 128:(fc + 1) * 128], h2T[:, kc, cs], kc == 0, kc == 7, [WU.tok, h2T.tok], [pu.tok])
                        k.op("act", lambda e, pg=pg: e.activation(out=sil[:, 0:ncol], in_=pg[:, 0:ncol], func=AF.Silu), reads=[pg.tok, sil.tok], writes=[sil.tok])
                        k.op("dve", lambda e, pu=pu: e.tensor_mul(sil[:, 0:ncol], sil[:, 0:ncol], pu[:, 0:ncol]), reads=[pu.tok, sil.tok], writes=[sil.tok])
                        k.op("dve", lambda e, e_=e_, fc=fc: e.tensor_mul(actT[:, e_, fc, 0:ncol], sil[:, 0:ncol], cbt[:, 0:ncol]),
                             reads=[sil.tok, cbt.tok, actT.tok], writes=[actT.tok])
                for tl in range(nt_sub):
                    t = sub * 4 + tl
                    for half in range(2):
                        py = PS[4 + half]
                        i = 0
                        for e_ in range(4):
                            for fc in range(2):
                                mm(py[:, :], actT[:, e_, fc, tl * 128:(tl + 1) * 128], WD[:, e_, fc, half * 512:(half + 1) * 512], i == 0, i == 7,
                                   [actT.tok, WD.tok], [py.tok])
                                i += 1
                        hs = slice(half * 512, (half + 1) * 512)
                        if g == 0:
                            k.op("act", lambda e, py=py, t=t, hs=hs: e.copy(yacc[:, t, hs], py[:, :]), reads=[py.tok, yacc.tok], writes=[yacc.tok])
                        else:
                            k.op("dve", lambda e, py=py, t=t, hs=hs: e.tensor_add(yacc[:, t, hs], yacc[:, t, hs], py[:, :]), reads=[py.tok, yacc.tok], writes=[yacc.tok])
        for t in range(ntile):
            r0 = g0 + t * 128
            k.op("act", lambda e, t=t: e.activation(out=xn2[:], in_=yacc[:, t, :], func=AF.Square, accum_out=ssb[:, 6:7]),
                 reads=[yacc.tok, xn2.tok, ssb.tok], writes=[xn2.tok, ssb.tok])
            rstd_of(ssb[:, 7:8], ssb[:, 6:7], D)
            k.dma("sp", x1t[:], x1s[r0:r0 + 128, :], x1t.tok, reads=[x1t.tok], writes=[x1t.tok])
            k.op("dve", lambda e, t=t: e.scalar_tensor_tensor(out=xn2[:], in0=yacc[:, t, :], scalar=ssb[:, 7:8], in1=gv2t[:], op0=ALU.mult, op1=ALU.mult),
                 reads=[yacc.tok, ssb.tok, gv2t.tok, xn2.tok], writes=[xn2.tok])
            k.op("dve", lambda e: e.tensor_add(xn2[:], xn2[:], x1t[:]), reads=[xn2.tok, x1t.tok], writes=[xn2.tok])
            if sample:
                k.dma("sp", ydst(t), xn2[0:16, :], xn2.tok, reads=[xn2.tok])
            else:
                k.dma("sp", ydst(t), xn2[:], xn2.tok, reads=[xn2.tok])

    if stage >= 3:
        for b in range(NB):
            k.dma("sp", gv1t[:], gvs[b, 0], gv1t.tok, reads=[gv1t.tok], writes=[gv1t.tok])
            k.dma("sp", gv2t[:], gvs[b, 1], gv2t.tok, reads=[gv2t.tok], writes=[gv2t.tok])
            k.op("dve", lambda e, b=b: e.scalar_tensor_tensor(out=mcol2[:, 0, :], in0=aT[:, 32:40, 16 + b], scalar=1.0, in1=gcol[:, 2, :],
                                                               op0=ALU.add, op1=ALU.mult),
                 reads=[aT.tok, cgrp, mcol2.tok], writes=[mcol2.tok])
            k.op("dve", lambda e, b=b: e.tensor_copy(mcol2[:, 1, :], aT[:, 24:32, 16 + b]), reads=[aT.tok, mcol2.tok], writes=[mcol2.tok])
            nbat = int(os.environ.get("KNBAT", SEQ // MB))
            for bt in range(nbat):
                t00 = bt * MB
                batch_B(b, b * SEQ + t00,
                        lambda t, b=b, t00=t00: xp[b, t00 + t * 128:t00 + (t + 1) * 128, :],
                        lambda t, b=b, t00=t00: yp[b, t00 + t * 128:t00 + (t + 1) * 128, :], MB // 128)
    if stage >= 4:
        k.op("pool", lambda e: e.memset(gv1t[:], 0.0), reads=[gv1t.tok], writes=[gv1t.tok])
        k.op("pool", lambda e: e.memset(gv2t[:], 0.0), reads=[gv2t.tok], writes=[gv2t.tok])
        k.dma("sp", gv1t[0:16, :], sms[2], gv1t.tok, reads=[gv1t.tok], writes=[gv1t.tok])
        k.dma("sp", gv2t[0:16, :], sms[5], gv2t.tok, reads=[gv2t.tok], writes=[gv2t.tok])
        k.dma("sp", yacc[0:16, 1, :], sms[3], yacc.tok, reads=[yacc.tok], writes=[yacc.tok])
        k.dma("sp", yacc[0:16, 2, :], sms[4], yacc.tok, reads=[yacc.tok], writes=[yacc.tok])
        k.op("pool", lambda e: e.memset(mcol2[:, 0, :], 1.0), reads=[mcol2.tok], writes=[mcol2.tok])
        k.op("pool", lambda e: e.memset(mcol2[:, 1, :], 0.0), reads=[mcol2.tok], writes=[mcol2.tok])
        batch_B(None, NB * SEQ, lambda t: xs_d, lambda t: ys, 1, sample=True)
    k.barrier()
    esB.close()

    print("instructions:", k.nins, "dma sems:", len(k.dsems), "sbuf left:", nc.sbuf_bytes_remaining)
    return nc, cst


_CACHE = {}


def kernel(x_prompt, x_sample, c_prompt, c_sample, cache_cmp_kv, cache_slc_kv, cache_win_kv, state_hgrn, page_table,
           w_ada, b_ada, g_pre_mix, g_post_mix, g_pre_ffn, g_post_ffn, w_in, w_phi1, b_phi1, w_phi2, g_nsa_out,
           hgrn_lb_logits, g_hgrn_out, w_out, w_route_group, b_route_group, w_route_expert, b_route_expert,
           w_exp_gate, w_exp_up, w_exp_down):
    stage = STAGE
    if "nc" not in _CACHE:
        _CACHE["nc"] = build(stage)
    nc, cst = _CACHE["nc"]
    f = lambda a: np.ascontiguousarray(np.asarray(a, dtype=np.float32))
    bc = lambda v: np.ascontiguousarray(np.broadcast_to(np.asarray(v, np.float32).reshape(1, -1), (128, v.size)))
    common = {
        "w_ada": f(w_ada[0]), "b_ada": f(b_ada[0]).reshape(1, -1),
        "gcol": np.ascontiguousarray(np.stack([np.asarray(v[0], np.float32).reshape(8, 128).T for v in (g_pre_mix, g_post_mix, g_pre_ffn, g_post_ffn)], axis=1)),
        "gb": np.ascontiguousarray(np.stack([bc(g_pre_mix[0]), bc(g_post_mix[0]), bc(g_pre_ffn[0]), bc(g_post_ffn[0])], axis=1)),
        "g512": np.ascontiguousarray(np.stack([bc(g_nsa_out[0]), bc(np.asarray(g_hgrn_out[0]).reshape(-1)),
                                               bc(hgrn_lb_logits[0]), bc(hgrn_lb_logits[1])], axis=1)),
        "w_in": f(w_in[0]), "w_out": f(w_out[0]),
        "wroute": np.ascontiguousarray(np.concatenate([f(w_route_group[0]), f(w_route_expert[0])], axis=1)),
        "broute": bc(np.concatenate([np.asarray(b_route_group[0]), np.asarray(b_route_expert[0])])),
        **({"wg": f(w_exp_gate[0]), "wu": f(w_exp_up[0]), "wd": f(w_exp_down[0])} if stage >= 3 else {}),
        "bphi1": f(b_phi1[0]).reshape(128, 1),
        "wphi2": f(w_phi2[0]).reshape(128, 64),
    }
    w1 = f(w_phi1[0])
    bd = np.zeros((128, 32, 128), np.float32)
    bd[0:64, :, 0:64] = w1[0].transpose(1, 0, 2)
    bd[64:128, :, 64:128] = w1[1].transpose(1, 0, 2)
    common["wphi1"] = bd
    for n, a in cst.items():
        common["c_" + n] = a
    if stage >= 4:
        poolc_all = f(cache_cmp_kv[0]).reshape(NPOOL * 128, 256)
        pools_all = f(cache_slc_kv[0]).reshape(NPOOL * 128, 256)
    in_maps = []
    for c in range(NCORES):
        m = dict(common)
        m["xp"] = f(x_prompt[NB * c:NB * (c + 1)])
        if stage >= 4:
            m["xs"] = f(x_sample[NS * c:NS * (c + 1), 0])
            m["poolc"] = poolc_all
            m["pools"] = pools_all
            m["winb"] = f(cache_win_kv[0, NS * c:NS * (c + 1)]).reshape(NS, 512, 256)
            m["sth"] = f(state_hgrn[0, NS * c:NS * (c + 1)])
            m["ptab"] = np.ascontiguousarray(np.asarray(page_table[NS * c:NS * (c + 1)], np.int32).reshape(1, NS * NPAGE))
        m["call"] = np.ascontiguousarray(np.concatenate([f(c_sample[NS * c:NS * (c + 1)]), f(c_prompt[NB * c:NB * (c + 1)])], axis=0))
        in_maps.append(m)
    res = run_bass_kernel_spmd(nc, in_maps, core_ids=list(range(NCORES)))
    R = res.results
    cat = lambda n: np.concatenate([r[n] for r in R], axis=0)
    y_p = cat("yp")
    p_cmp = cat("pcmp").reshape(1, 16, SEQ, 2, 2, 64)
    p_slc = cat("pslc").reshape(1, 16, SEQ, 2, 2, 64)
    p_win = cat("pwin").reshape(1, 16, 512, 2, 2, 64)
    p_hg = cat("phg").reshape(1, 16, 4, 128, 128)
    z = lambda *s: np.zeros(s, np.float32)
    if stage < 4:
        return (y_p, z(128, 1, D), p_cmp, p_slc, p_win, p_hg, z(1, 128, 1, 2, 2, 64), z(1, 128, 1, 2, 2, 64),
                z(1, 128, 512, 2, 2, 64), z(1, 128, 4, 128, 128))
    y_s = cat("ys").reshape(128, 1, D)
    s_cmp = cat("scmp").reshape(1, 128, 1, 2, 2, 64)
    s_slc = cat("sslc").reshape(1, 128, 1, 2, 2, 64)
    s_win = cat("swin").reshape(1, 128, 512, 2, 2, 64)
    s_hg = cat("shg").reshape(1, 128, 4, 128, 128)
    return (y_p, y_s, p_cmp, p_slc, p_win, p_hg, s_cmp, s_slc, s_win, s_hg)
```
